# Optimizing a Trainium2 kernel written in Bass

```python
import jax, jax.numpy as jnp
from jax import lax
import numpy as np

D_MODEL = 1024
BATCH = 16
SEQ = 2048
DEPTH = 2

N_A_LAYERS = DEPTH // 2
N_B_LAYERS = DEPTH - N_A_LAYERS
N_DENSE = (DEPTH + 1) // 2
N_MOE = DEPTH // 2

ROPE_THETA = 10000.0
LN_EPS = 1e-5
DN_ALPHA = (2.0 * DEPTH) ** 0.25
DN_BETA = (8.0 * DEPTH) ** -0.25

RET_HEADS = 4
RET_DK = D_MODEL // RET_HEADS
RET_DV = 2 * RET_DK
RET_CHUNK = 128

NSA_HEADS = 16
NSA_GROUPS = 4
NSA_HD = D_MODEL // NSA_HEADS
CMP_LEN = 32
CMP_STRIDE = 16
CMP_HID = 4 * NSA_HD
SLC_LEN = 64
SLC_TOPK = 8
WIN = 512
NSA_QBLK = 32
FORCE_SCORE = 1e9

D_FF = 2816
N_EXPERTS = 8
TOP_K = 2
D_FF_E = 3584

kernel_name = 'yoco_retnet_nsa_moe_block'


def rotary(x, pos):
    d = x.shape[-1]
    inv = 1.0 / (ROPE_THETA ** (jnp.arange(0, d, 2, dtype=jnp.float32) / d))
    ang = pos.astype(jnp.float32)[:, None] * inv[None, :]
    cos, sin = jnp.cos(ang), jnp.sin(ang)
    xf = x.astype(jnp.float32)
    x1, x2 = xf[..., : d // 2], xf[..., d // 2:]
    return jnp.concatenate([x1 * cos - x2 * sin, x1 * sin + x2 * cos], axis=-1).astype(x.dtype)


def layer_norm(x, g, b):
    xf = x.astype(jnp.float32)
    mu = jnp.mean(xf, -1, keepdims=True)
    var = jnp.mean(jnp.square(xf - mu), -1, keepdims=True)
    return ((xf - mu) * lax.rsqrt(var + LN_EPS) * g + b).astype(x.dtype)


def masked_softmax(s, valid):
    s = jnp.where(valid, s.astype(jnp.float32), -jnp.inf)
    m = jnp.max(s, -1, keepdims=True)
    m = jnp.where(jnp.isfinite(m), m, 0.0)
    e = jnp.where(valid, jnp.exp(s - m), 0.0)
    return e / jnp.maximum(jnp.sum(e, -1, keepdims=True), 1e-30)


def swiglu(x, w_in, w_out):
    a, b = jnp.split(x @ w_in, 2, axis=-1)
    return (jax.nn.silu(a) * b) @ w_out


def retention(x, w_in, gn_g, w_out):
    B, S, _ = x.shape
    H, dk, dv, C = RET_HEADS, RET_DK, RET_DV, RET_CHUNK
    N = S // C
    proj = x @ w_in
    q, k, v, g = jnp.split(proj, [H * dk, 2 * H * dk, 2 * H * dk + H * dv], axis=-1)
    pos = jnp.arange(S)
    q = rotary(q.reshape(B, S, H, dk).transpose(0, 2, 1, 3), pos)
    k = rotary(k.reshape(B, S, H, dk).transpose(0, 2, 1, 3), pos) * dk ** -0.5
    v = v.reshape(B, S, H, dv).transpose(0, 2, 1, 3)
    log_gamma = jnp.log1p(-(2.0 ** (-5.0 - jnp.arange(H, dtype=jnp.float32))))
    i = jnp.arange(C, dtype=jnp.float32)
    rel = i[:, None] - i[None, :]
    intra_decay = jnp.where(rel >= 0, jnp.exp(log_gamma[:, None, None] * jnp.maximum(rel, 0.0)), 0.0)
    q_decay = jnp.exp(log_gamma[:, None] * (i + 1.0))[None, :, :, None]
    k_decay = jnp.exp(log_gamma[:, None] * (C - 1.0 - i))[None, :, :, None]
    chunk_decay = jnp.exp(log_gamma * C)[None, :, None, None]

    def to_chunks(t):
        return t.reshape(B, H, N, C, t.shape[-1]).transpose(2, 0, 1, 3, 4)

    def step(state, inp):
        qi, ki, vi = inp
        scores = jnp.einsum('bhqd,bhkd->bhqk', qi, ki) * intra_decay
        inner = jnp.einsum('bhqk,bhkv->bhqv', scores, vi)
        cross = jnp.einsum('bhqd,bhdv->bhqv', qi, state) * q_decay
        new_state = state * chunk_decay + jnp.einsum('bhkd,bhkv->bhdv', ki * k_decay, vi)
        return new_state, inner + cross

    state0 = jnp.zeros((B, H, dk, dv), jnp.float32)
    _, out = lax.scan(step, state0, (to_chunks(q), to_chunks(k), to_chunks(v)))
    out = out.transpose(1, 2, 0, 3, 4).reshape(B, H, S, dv).astype(jnp.float32)
    mu = jnp.mean(out, -1, keepdims=True)
    var = jnp.mean(jnp.square(out - mu), -1, keepdims=True)
    normed = (out - mu) * lax.rsqrt(var + LN_EPS) * gn_g.reshape(H, 1, dv)
    normed = normed.transpose(0, 2, 1, 3).reshape(B, S, H * dv)
    y = jax.nn.silu(g.astype(jnp.float32)) * normed
    return (y @ w_out).astype(x.dtype)


def nsa_shared_kv(h, w_kv, ck_pe, ck_w1, ck_w2, cv_pe, cv_w1, cv_w2):
    B, S, _ = h.shape
    G, dh = NSA_GROUPS, NSA_HD
    kv = (h @ w_kv).reshape(B, S, 6, G, dh).transpose(2, 0, 3, 1, 4)
    pos = jnp.arange(S)
    k_cmp, v_cmp = rotary(kv[0], pos), kv[1]
    k_slc, v_slc = rotary(kv[2], pos), kv[3]
    k_win, v_win = rotary(kv[4], pos), kv[5]
    n_cmp = (S - CMP_LEN) // CMP_STRIDE + 1
    idx = jnp.arange(n_cmp)[:, None] * CMP_STRIDE + jnp.arange(CMP_LEN)[None, :]

    def compress(t, pe, w1, w2):
        blocks = t[:, :, idx, :] + pe
        flat = blocks.reshape(B, G, n_cmp, CMP_LEN * dh)
        return jax.nn.gelu(flat @ w1) @ w2

    k_c = compress(k_cmp, ck_pe, ck_w1, ck_w2)
    v_c = compress(v_cmp, cv_pe, cv_w1, cv_w2)
    n_slc = S // SLC_LEN
    k_s = k_slc.reshape(B, G, n_slc, SLC_LEN, dh)
    v_s = v_slc.reshape(B, G, n_slc, SLC_LEN, dh)
    pad = ((0, 0), (0, 0), (WIN, 0), (0, 0))
    k_w = jnp.pad(k_win, pad)
    v_w = jnp.pad(v_win, pad)
    return (k_c, v_c, k_s, v_s, k_w, v_w)


def nsa_attention(x, w_q, w_out, k_c, v_c, k_s, v_s, k_w, v_w):
    B, S, _ = x.shape
    H, G, dh = NSA_HEADS, NSA_GROUPS, NSA_HD
    R = H // G
    Q = NSA_QBLK
    proj = x @ w_q
    q = proj[..., : H * dh].reshape(B, S, G, R, dh).transpose(0, 2, 3, 1, 4)
    q = rotary(q, jnp.arange(S)) * dh ** -0.5
    gates = jax.nn.sigmoid(proj[..., H * dh:].astype(jnp.float32))
    gates = gates.reshape(B, S, G, R, 3).transpose(0, 2, 3, 1, 4)
    n_cmp = k_c.shape[2]
    n_slc = k_s.shape[2]
    n_top = min(SLC_TOPK, n_slc)
    cmp_start = jnp.arange(n_cmp) * CMP_STRIDE
    cmp_end = cmp_start + CMP_LEN - 1
    slc_start = jnp.arange(n_slc) * SLC_LEN
    overlap = jnp.clip(jnp.minimum(cmp_start[:, None] + CMP_LEN, slc_start[None, :] + SLC_LEN)
                       - jnp.maximum(cmp_start[:, None], slc_start[None, :]), 0).astype(jnp.float32) / CMP_STRIDE
    j_idx = jnp.arange(n_slc)
    bi = jnp.arange(B)[:, None, None, None]
    gi = jnp.arange(G)[None, :, None, None]

    def block(i):
        s0 = i * Q
        t = s0 + jnp.arange(Q)
        qb = lax.dynamic_slice_in_dim(q, s0, Q, axis=3)
        gb = lax.dynamic_slice_in_dim(gates, s0, Q, axis=3)
        sc = jnp.einsum('bgrqd,bgnd->bgrqn', qb, k_c)
        p_cmp = masked_softmax(sc, cmp_end[None, :] <= t[:, None])
        o_cmp = jnp.einsum('bgrqn,bgnd->bgrqd', p_cmp, v_c)
        imp = jnp.einsum('bgrqn,nj->bgqj', p_cmp, overlap)
        cur = t // SLC_LEN
        forced = (j_idx[None, :] == 0) | (j_idx[None, :] == cur[:, None]) | (j_idx[None, :] == cur[:, None] - 1)
        imp = jnp.where(forced, FORCE_SCORE, imp)
        imp = jnp.where(j_idx[None, :] <= cur[:, None], imp, -jnp.inf)
        top_val, top_idx = lax.top_k(imp, n_top)
        sel_ok = jnp.isfinite(top_val)
        ksel = k_s[bi, gi, top_idx]
        vsel = v_s[bi, gi, top_idx]
        tok = top_idx[..., None] * SLC_LEN + jnp.arange(SLC_LEN)
        valid_s = sel_ok[..., None] & (tok <= t[None, None, :, None, None])
        ss = jnp.einsum('bgrqd,bgqkld->bgrqkl', qb, ksel).reshape(B, G, R, Q, n_top * SLC_LEN)
        p_s = masked_softmax(ss, valid_s.reshape(B, G, 1, Q, n_top * SLC_LEN))
        o_slc = jnp.einsum('bgrqkl,bgqkld->bgrqd', p_s.reshape(B, G, R, Q, n_top, SLC_LEN), vsel)
        kw = lax.dynamic_slice_in_dim(k_w, s0, Q + WIN, axis=2)
        vw = lax.dynamic_slice_in_dim(v_w, s0, Q + WIN, axis=2)
        kpos = s0 - WIN + jnp.arange(Q + WIN)
        valid_w = (kpos[None, :] <= t[:, None]) & (kpos[None, :] > t[:, None] - WIN) & (kpos[None, :] >= 0)
        sw = jnp.einsum('bgrqd,bgkd->bgrqk', qb, kw)
        o_win = jnp.einsum('bgrqk,bgkd->bgrqd', masked_softmax(sw, valid_w), vw)
        return gb[..., 0:1] * o_cmp + gb[..., 1:2] * o_slc + gb[..., 2:3] * o_win

    out = lax.map(block, jnp.arange(S // Q))
    out = out.transpose(1, 0, 4, 2, 3, 5).reshape(B, S, H * dh)
    return (out @ w_out).astype(x.dtype)


def moe_swiglu(x, w_router, w_in, w_out):
    B, S, D = x.shape
    xf = x.reshape(B * S, D)
    logits = (xf @ w_router).astype(jnp.float32)
    top_v, top_i = lax.top_k(logits, TOP_K)
    w = jax.nn.softmax(top_v, axis=-1)
    gate = jnp.sum(jax.nn.one_hot(top_i, N_EXPERTS, dtype=jnp.float32) * w[..., None], axis=1)
    out = jnp.zeros((B * S, D), jnp.float32)
    for e in range(N_EXPERTS):
        out = out + gate[:, e:e + 1] * swiglu(xf, w_in[e], w_out[e])
    return out.reshape(B, S, D).astype(x.dtype)


def setup_inputs(seed: int = 0) -> dict:
    key = jax.random.key(seed)
    ks = jax.random.split(key, 20)

    def nrm(k, shape, scale):
        return jax.random.normal(k, shape, jnp.float32) * scale

    ret_qk = RET_HEADS * RET_DK
    ret_v = RET_HEADS * RET_DV
    nsa_q = NSA_HEADS * NSA_HD
    return {
        'x': nrm(ks[0], (BATCH, SEQ, D_MODEL), 1.0),
        'ret_w_in': nrm(ks[1], (N_A_LAYERS, D_MODEL, 2 * ret_qk + 2 * ret_v), D_MODEL ** -0.5),
        'ret_gn_g': 1.0 + nrm(ks[2], (N_A_LAYERS, ret_v), 0.02),
        'ret_w_out': nrm(ks[3], (N_A_LAYERS, ret_v, D_MODEL), DN_BETA * ret_v ** -0.5),
        'nsa_w_kv': nrm(ks[4], (D_MODEL, 6 * NSA_GROUPS * NSA_HD), D_MODEL ** -0.5),
        'cmp_k_pe': nrm(ks[5], (CMP_LEN, NSA_HD), 0.1),
        'cmp_k_w1': nrm(ks[6], (CMP_LEN * NSA_HD, CMP_HID), (CMP_LEN * NSA_HD) ** -0.5),
        'cmp_k_w2': nrm(ks[7], (CMP_HID, NSA_HD), CMP_HID ** -0.5),
        'cmp_v_pe': nrm(ks[8], (CMP_LEN, NSA_HD), 0.1),
        'cmp_v_w1': nrm(ks[9], (CMP_LEN * NSA_HD, CMP_HID), (CMP_LEN * NSA_HD) ** -0.5),
        'cmp_v_w2': nrm(ks[10], (CMP_HID, NSA_HD), CMP_HID ** -0.5),
        'nsa_w_q': nrm(ks[11], (N_B_LAYERS, D_MODEL, nsa_q + 3 * NSA_HEADS), D_MODEL ** -0.5),
        'nsa_w_out': nrm(ks[12], (N_B_LAYERS, nsa_q, D_MODEL), DN_BETA * nsa_q ** -0.5),
        'ffn_w_in': nrm(ks[13], (N_DENSE, D_MODEL, 2 * D_FF), D_MODEL ** -0.5),
        'ffn_w_out': nrm(ks[14], (N_DENSE, D_FF, D_MODEL), DN_BETA * D_FF ** -0.5),
        'moe_router': nrm(ks[15], (N_MOE, D_MODEL, N_EXPERTS), D_MODEL ** -0.5),
        'moe_w_in': nrm(ks[16], (N_MOE, N_EXPERTS, D_MODEL, 2 * D_FF_E), D_MODEL ** -0.5),
        'moe_w_out': nrm(ks[17], (N_MOE, N_EXPERTS, D_FF_E, D_MODEL), DN_BETA * D_FF_E ** -0.5),
        'ln_g': 1.0 + nrm(ks[18], (DEPTH, 2, D_MODEL), 0.02),
        'ln_b': nrm(ks[19], (DEPTH, 2, D_MODEL), 0.02),
    }


def reference(x, ret_w_in, ret_gn_g, ret_w_out, nsa_w_kv, cmp_k_pe, cmp_k_w1, cmp_k_w2,
              cmp_v_pe, cmp_v_w1, cmp_v_w2, nsa_w_q, nsa_w_out, ffn_w_in, ffn_w_out,
              moe_router, moe_w_in, moe_w_out, ln_g, ln_b):
    shared = None
    for l in range(DEPTH):
        if l < N_A_LAYERS:
            mix = retention(x, ret_w_in[l], ret_gn_g[l], ret_w_out[l])
        else:
            if l == N_A_LAYERS:
                shared = nsa_shared_kv(x, nsa_w_kv, cmp_k_pe, cmp_k_w1, cmp_k_w2,
                                       cmp_v_pe, cmp_v_w1, cmp_v_w2)
            jb = l - N_A_LAYERS
            mix = nsa_attention(x, nsa_w_q[jb], nsa_w_out[jb], *shared)
        x = layer_norm(DN_ALPHA * x + mix, ln_g[l, 0], ln_b[l, 0])
        if l % 2 == 0:
            f = swiglu(x, ffn_w_in[l // 2], ffn_w_out[l // 2])
        else:
            f = moe_swiglu(x, moe_router[l // 2], moe_w_in[l // 2], moe_w_out[l // 2])
        x = layer_norm(DN_ALPHA * x + f, ln_g[l, 1], ln_b[l, 1])
    return x
```

```python
import math
import os
from contextlib import ExitStack

import numpy as np
import concourse.bass as bass
import concourse.mybir as mybir
from concourse.bass_utils import run_bass_kernel_spmd
from concourse.bass import OrderedEngineSet

F32 = mybir.dt.float32
BF16 = mybir.dt.bfloat16
I32 = mybir.dt.int32
AF = mybir.ActivationFunctionType
ALU = mybir.AluOpType

NCORES = 8
NB = 2
S = 2048
D = 1024
NT = S // 128
DN_ALPHA = 4.0 ** 0.25
LN_EPS = 1e-5
ROPE_THETA = 10000.0
RH, RDK, RDV = 4, 256, 512
NH, NG, DH = 16, 4, 64
NCMP = 127
DFF = 2816
NE = 8
DFFE = 3584
NEG = -30000.0
DBG_R = int(os.environ.get('DBG_R', 4))
NOSELF = bool(os.environ.get('DBG_NOSELF'))
DBG_ATT = float(os.environ.get('DBG_ATT', 9))


_UCNT = [0]


def _u(name):
    _UCNT[0] += 1
    return "%s_u%d" % (name, _UCNT[0])


class Buf:
    __slots__ = ("name", "w", "r")

    def __init__(self, name=""):
        self.name = name
        self.w = []
        self.r = []


class Sched:
    NDMA = 32

    def __init__(self, nc, stack):
        self.nc = nc
        self.eng = {"pe": nc.tensor, "dve": nc.vector, "act": nc.scalar, "pool": nc.gpsimd, "sp": nc.sync}
        self.sem = {}
        self.cnt = {}
        for k in ("pe", "dve", "act", "pool"):
            self.sem[k] = stack.enter_context(nc.semaphore("s_" + k))
            self.cnt[k] = 0
        self.dsem = [stack.enter_context(nc.semaphore("s_dma%d" % i)) for i in range(self.NDMA)]
        self.dcnt = [0] * self.NDMA
        self.dnext = 0
        self.seen = {}
        self.ninstr = 0

    def _sem(self, key):
        return self.sem[key] if isinstance(key, str) else self.dsem[key]

    def _wait(self, e, ev):
        key, val = ev
        if e == "pe" and key == "pe":
            return
        if NOSELF and e == key:
            return
        if self.seen.get((e, key), 0) >= val:
            return
        self.eng[e].wait_ge(self._sem(key), val)
        self.seen[(e, key)] = val
        self.ninstr += 1

    def _deps(self, e, reads, writes, add=False):
        for b in reads:
            for ev in b.w:
                self._wait(e, ev)
        for b in writes:
            if not add:
                for ev in b.w:
                    self._wait(e, ev)
            for ev in b.r:
                self._wait(e, ev)

    def op(self, e, fn, reads=(), writes=(), inc=True):
        self._deps(e, reads, writes)
        ins = fn()
        self.ninstr += 1
        ev = (e, self.cnt[e] + 1)
        if inc:
            self.cnt[e] += 1
            ins.then_inc(self.sem[e], 1)
        for b in writes:
            b.w = [ev]
            b.r = []
        for b in reads:
            if ev not in b.r:
                b.r.append(ev)
        return ins

    def dma(self, q, out, in_, reads=(), writes=(), add=False):
        i = self.dnext
        self.dnext = (self.dnext + 1) % self.NDMA
        if self.dcnt[i] > 0:
            self._wait(q, (i, self.dcnt[i]))
        self._deps(q, reads, writes, add=add)
        self.dcnt[i] += 16
        ev = (i, self.dcnt[i])
        self.eng[q].dma_start(out=out, in_=in_).then_inc(self.dsem[i], 16)
        self.ninstr += 1
        for b in writes:
            if add:
                b.w.append(ev)
            else:
                b.w = [ev]
            b.r = []
        for b in reads:
            b.r.append(ev)
        return ev

    def fence(self):
        for e in ("pe", "dve", "act", "pool", "sp"):
            for k in ("pe", "dve", "act", "pool"):
                if self.cnt[k] > 0 and k != e:
                    self._wait(e, (k, self.cnt[k]))
            if e != "pe" and e != "sp" and self.cnt[e] > 0:
                self._wait(e, (e, self.cnt[e]))
            for i in range(self.NDMA):
                if self.dcnt[i] > 0:
                    self._wait(e, (i, self.dcnt[i]))

    def finish(self, bufs, e="sp"):
        for b in bufs:
            for ev in b.w:
                self._wait(e, ev)


class Ring:
    def __init__(self, nc, st, name, shape, dtype, n):
        self.t = [st.enter_context(nc.sbuf_tensor(_u("%s_%d") % (name, i), list(shape), dtype)) for i in range(n)]
        self.b = [Buf("%s_%d" % (name, i)) for i in range(n)]
        self.i = -1

    def next(self):
        self.i = (self.i + 1) % len(self.t)
        return self.t[self.i], self.b[self.i]

    def cur(self):
        return self.t[self.i], self.b[self.i]


class Ctx:
    def __init__(self, nc, st):
        self.nc = nc
        self.S = Sched(nc, st)
        self.dbl = [st.enter_context(nc.psum_tensor("dbank%d" % i, [128, 1024], F32)) for i in range(4)]
        self.banks = [self.dbl[i // 2][:, (i % 2) * 512:(i % 2 + 1) * 512] for i in range(8)]
        self.pi = -1
        self.prot = [1, 2, 3]
        self.bbuf = [Buf("bank%d" % i) for i in range(8)]
        self.bi = -1
        self.rot = list(range(8))
        self.regs = {}

    def bank_pair(self):
        self.pi = (self.pi + 1) % len(self.prot)
        j = self.prot[self.pi]
        return (self.banks[2 * j], self.bbuf[2 * j]), (self.banks[2 * j + 1], self.bbuf[2 * j + 1]), self.dbl[j]

    def reg(self, val):
        if val not in self.regs:
            self.regs[val] = self.nc.gpsimd.to_reg(val)
        return self.regs[val]

    def bank(self):
        self.bi = (self.bi + 1) % len(self.rot)
        i = self.rot[self.bi]
        return self.banks[i], self.bbuf[i]


def _consts(cx, st, dr):
    nc, S_ = cx.nc, cx.S
    c = {}
    ones = st.enter_context(nc.sbuf_tensor(_u("c_ones"), [128, 128], BF16))
    ident = st.enter_context(nc.sbuf_tensor(_u("c_ident"), [128, 128], BF16))
    bo, bi = Buf(), Buf()
    S_.op("pool", lambda: nc.gpsimd.memset(ones[:], 1.0), writes=[bo])
    S_.op("pool", lambda: nc.gpsimd.affine_select(out=ident[:], in_=ones[:], pattern=[[-1, 128]],
                                                    compare_op=ALU.is_equal, fill=cx.reg(0.0), base=0,
                                                    channel_multiplier=1), reads=[bo], writes=[bi])
    c["ident"], c["ident_b"] = ident, bi
    c["ones"], c["ones_b"] = ones, bo
    epsb = st.enter_context(nc.sbuf_tensor(_u("c_eps"), [128, 1], F32))
    be = Buf()
    S_.op("pool", lambda: nc.gpsimd.memset(epsb[:], LN_EPS), writes=[be])
    c["eps"], c["eps_b"] = epsb, be
    return c


def _rope_tables(cx, c, dr_cos, dr_sin, nfreq, rows_fn_period, bc, sign_rows=False):
    nc, S_ = cx.nc, cx.S
    with ExitStack() as st:
        pidx = st.enter_context(nc.sbuf_tensor(_u("rt_pidx"), [128, 1], F32))
        inv = st.enter_context(nc.sbuf_tensor(_u("rt_inv"), [128, 1], F32))
        pos = st.enter_context(nc.sbuf_tensor(_u("rt_pos"), [128, S], F32))
        ang = st.enter_context(nc.sbuf_tensor(_u("rt_ang"), [128, S], F32))
        ki = st.enter_context(nc.sbuf_tensor(_u("rt_ki"), [128, S], I32))
        kf = st.enter_context(nc.sbuf_tensor(_u("rt_kf"), [128, S], F32))
        res = st.enter_context(nc.sbuf_tensor(_u("rt_res"), [128, S], F32))
        b1, b2, b3, b4, b5, b6, b7 = [Buf() for _ in range(7)]
        per = rows_fn_period
        for base in range(0, 128, per):
            S_.op("pool", lambda base=base: nc.gpsimd.iota(pidx[base:base + per, :], pattern=[[0, 1]], base=0,
                                                           channel_multiplier=1,
                                                           allow_small_or_imprecise_dtypes=True), writes=[b1])
        S_.op("act", lambda: nc.scalar.activation(out=inv[:], in_=pidx[:], func=AF.Exp,
                                                  scale=-math.log(ROPE_THETA) / nfreq), reads=[b1], writes=[b2])
        S_.op("pool", lambda: nc.gpsimd.iota(pos[:], pattern=[[1, S]], base=0, channel_multiplier=0,
                                             allow_small_or_imprecise_dtypes=True), writes=[b3])
        S_.op("dve", lambda: nc.vector.tensor_scalar(out=ang[:], in0=pos[:], scalar1=inv[:, 0:1], scalar2=None,
                                                     op0=ALU.mult), reads=[b3, b2], writes=[b4])
        for which, dr_t in ((0, dr_sin), (1, dr_cos)):
            shift = 0.0 if which == 0 else math.pi / 2
            S_.op("dve", lambda: nc.vector.tensor_scalar(out=ki[:], in0=ang[:], scalar1=shift,
                                                         scalar2=1.0 / (2 * math.pi), op0=ALU.add, op1=ALU.mult),
                  reads=[b4], writes=[b5])
            S_.op("dve", lambda: nc.vector.tensor_copy(out=kf[:], in_=ki[:]), reads=[b5], writes=[b6])
            S_.op("dve", lambda: nc.vector.scalar_tensor_tensor(out=res[:], in0=kf[:], scalar=-2 * math.pi,
                                                                in1=ang[:], op0=ALU.mult, op1=ALU.add),
                  reads=[b6, b4], writes=[b7])
            S_.op("dve", lambda: nc.vector.tensor_scalar(out=res[:], in0=res[:], scalar1=shift, scalar2=3.14159,
                                                         op0=ALU.add, op1=ALU.min), reads=[b7], writes=[b7])
            S_.op("dve", lambda: nc.vector.tensor_scalar(out=res[:], in0=res[:], scalar1=-3.14159, scalar2=None,
                                                         op0=ALU.max), reads=[b7], writes=[b7])
            S_.op("act", lambda: nc.scalar.activation(out=res[:], in_=res[:], func=AF.Sin), reads=[b7], writes=[b7])
            if sign_rows and which == 0:
                for p0 in (0, 64):
                    S_.op("act", lambda p0=p0: nc.scalar.mul(out=res[p0:p0 + 32, :], in_=res[p0:p0 + 32, :], mul=-1.0),
                          reads=[b7], writes=[b7])
            S_.dma("sp", dr_t[:, :], res[:], reads=[b7], writes=[bc])


def _load_w(cx, Wt, Wb, w_ap, kchunks, c0, c1, dst_c0=None, q="pool"):
    if dst_c0 is None:
        dst_c0 = c0
    n = c1 - c0
    for kc in range(kchunks):
        cx.S.dma(q, Wt[:, kc, dst_c0:dst_c0 + n], w_ap[kc * 128:(kc + 1) * 128, c0:c1], writes=[Wb], add=(kc > 0))


def _transpose_to(cx, c, src_t, src_b, nchunk, dst_ap_fn, dst_b, evac="act"):
    nc, S_ = cx.nc, cx.S
    for k0 in range(0, nchunk, 8):
        k1 = min(nchunk, k0 + 8)
        bk, bb = cx.bank()
        pb = bk.bitcast(BF16)
        for k in range(k0, k1):
            S_.op("pe", lambda k=k: nc.tensor.transpose(out=pb[:, (k - k0) * 128:(k - k0 + 1) * 128],
                                                        in_=src_t[:, k * 128:(k + 1) * 128],
                                                        identity=c["ident"][:]),
                  reads=[src_b, c["ident_b"]], writes=[bb], inc=(k == k1 - 1))
        dst = dst_ap_fn(k0, k1)
        if evac == "act" or (evac == "alt" and (k0 // 8) % 2 == 0):
            S_.op("act", lambda: nc.scalar.copy(out=dst, in_=pb[:, 0:(k1 - k0) * 128]), reads=[bb], writes=[dst_b])
        else:
            S_.op("dve", lambda: nc.vector.tensor_copy(out=dst, in_=pb[:, 0:(k1 - k0) * 128]), reads=[bb],
                  writes=[dst_b])


def _layernorm_store(cx, c, z_t, z_b, lng, lnb, lnpb, dst_ap, dst_b, st6, mv, rs, sb):
    nc, S_ = cx.nc, cx.S
    for hh in range(2):
        S_.op("dve", lambda hh=hh: nc.vector.bn_stats(out=st6[:, hh, :], in_=z_t[:, hh * 512:(hh + 1) * 512]),
              reads=[z_b], writes=[sb])
    S_.op("dve", lambda: nc.vector.bn_aggr(out=mv[:], in_=st6[:].rearrange("p a b -> p (a b)")), reads=[sb], writes=[sb])
    S_.op("act", lambda: nc.scalar.activation(out=rs[:], in_=mv[:, 1:2], func=AF.Sqrt, bias=c["eps"][:], scale=1.0),
          reads=[sb, c["eps_b"]], writes=[sb])
    S_.op("dve", lambda: nc.vector.reciprocal(out=rs[:], in_=rs[:]), reads=[sb], writes=[sb])
    S_.op("dve", lambda: nc.vector.scalar_tensor_tensor(out=z_t[:], in0=z_t[:], scalar=mv[:, 0:1], in1=lng[:],
                                                        op0=ALU.subtract, op1=ALU.mult),
          reads=[z_b, sb, lnpb], writes=[z_b])
    S_.op("dve", lambda: nc.vector.scalar_tensor_tensor(out=z_t[:], in0=z_t[:], scalar=rs[:, 0:1], in1=lnb[:],
                                                        op0=ALU.mult, op1=ALU.add),
          reads=[z_b, sb, lnpb], writes=[z_b])
    S_.dma("sp", dst_ap, z_t[:], reads=[z_b], writes=[dst_b], add=True)


def phase_ret1(cx, c, x_in, x_b, w_in, cos_d, sin_d, tab_b, yn_d, yn_b):
    nc, S_ = cx.nc, cx.S
    lg = [math.log1p(-(2.0 ** (-5.0 - h))) for h in range(RH)]
    with ExitStack() as st:
        W = st.enter_context(nc.sbuf_tensor(_u("r1_W"), [128, 8, 4096], BF16))
        Wb = [Buf() for _ in range(4)]
        for j in range(4):
            _load_w(cx, W, Wb[j], w_in, 8, j * 1024, (j + 1) * 1024)
        dec = st.enter_context(nc.sbuf_tensor(_u("r1_dec"), [128, RH, 128], F32))
        qdec = st.enter_context(nc.sbuf_tensor(_u("r1_qdec"), [128, RH, 128], F32))
        kdec = st.enter_context(nc.sbuf_tensor(_u("r1_kdec"), [128, RH], F32))
        io = st.enter_context(nc.sbuf_tensor(_u("r1_io"), [128, 128], F32))
        io2 = st.enter_context(nc.sbuf_tensor(_u("r1_io2"), [128, 128], F32))
        io3 = st.enter_context(nc.sbuf_tensor(_u("r1_io3"), [128, 1], F32))
        lnsc = st.enter_context(nc.sbuf_tensor(_u("r1_lnsc"), [128, 1], F32))
        bt = Buf()
        S_.op("pool", lambda: nc.gpsimd.iota(io[:], pattern=[[1, 128]], base=0, channel_multiplier=-1,
                                             allow_small_or_imprecise_dtypes=True), writes=[bt])
        S_.op("pool", lambda: nc.gpsimd.iota(io2[:], pattern=[[1, 128]], base=1, channel_multiplier=0,
                                             allow_small_or_imprecise_dtypes=True), writes=[bt])
        S_.op("pool", lambda: nc.gpsimd.iota(io3[:], pattern=[[0, 1]], base=127, channel_multiplier=-1,
                                             allow_small_or_imprecise_dtypes=True), writes=[bt])
        S_.op("pool", lambda: nc.gpsimd.memset(lnsc[:], math.log(1.0 / 16.0)), writes=[bt])
        bdec = Buf()
        for h in range(RH):
            S_.op("act", lambda h=h: nc.scalar.activation(out=dec[:, h, :], in_=io[:], func=AF.Exp, scale=lg[h],
                                                          bias=lnsc[:]), reads=[bt], writes=[bdec])
            S_.op("pool", lambda h=h: nc.gpsimd.affine_select(out=dec[:, h, :], in_=dec[:, h, :],
                                                              pattern=[[1, 128]], compare_op=ALU.is_ge, fill=cx.reg(0.0),
                                                              base=0, channel_multiplier=-1),
                  reads=[bdec], writes=[bdec])
            S_.op("act", lambda h=h: nc.scalar.activation(out=qdec[:, h, :], in_=io2[:], func=AF.Exp, scale=lg[h]),
                  reads=[bt], writes=[bdec])
            S_.op("act", lambda h=h: nc.scalar.activation(out=kdec[:, h:h + 1], in_=io3[:], func=AF.Exp, scale=lg[h],
                                                          bias=lnsc[:]), reads=[bt], writes=[bdec])
        states = [st.enter_context(nc.sbuf_tensor(_u("r1_state"), [128, RH, 2, 512], F32)) for _ in range(NB)]
        states_bf = [st.enter_context(nc.sbuf_tensor(_u("r1_statebf"), [128, RH, 2, 512], BF16)) for _ in range(NB)]
        stbs = [[[Buf() for _ in range(2)] for _ in range(RH)] for _ in range(NB)]
        stbbs = [[[Buf() for _ in range(2)] for _ in range(RH)] for _ in range(NB)]
        xtok = Ring(nc, st, "r1_xtok", [128, D], F32, 4)
        xbr = Ring(nc, st, "r1_xb", [128, D], BF16, 1)
        xTr = Ring(nc, st, "r1_xT", [128, 8, 128], BF16, 2)
        csr = Ring(nc, st, "r1_cs", [128, 2, 128], F32, 4)
        qkr = Ring(nc, st, "r1_qk", [128, RH, 2, 2, 128], BF16, 3)
        t1r = Ring(nc, st, "r1_t1", [128, 2, 128], F32, 2)
        t2r = Ring(nc, st, "r1_t2", [128, 2, 128], F32, 2)
        vr = Ring(nc, st, "r1_v", [128, 2048], BF16, 3)
        kdr = Ring(nc, st, "r1_kd", [128, RH, 256], BF16, 2)
        sTr = Ring(nc, st, "r1_sT", [128, RH, 128], BF16, 2)
        qdr = Ring(nc, st, "r1_qd", [128, RH, 2, 128], BF16, 2)
        tmpr = Ring(nc, st, "r1_tmp", [128, 512], F32, 2)
        ynr = Ring(nc, st, "r1_yn", [128, 2048], BF16, 2)
        st6r = Ring(nc, st, "r1_st6", [128, RH, 6], F32, 2)
        mvr = Ring(nc, st, "r1_mv", [128, RH, 2], F32, 2)
        rsr = Ring(nc, st, "r1_rs", [128, RH], F32, 2)

        items = [(b, n) for n in range(NT) for b in range(NB)]
        pre = {}

        def loads(k):
            b, n = items[k]
            t0 = n * 128
            xt, xtb = xtok.next()
            S_.dma("sp", xt[:], x_in[b, t0:t0 + 128, :], reads=[x_b], writes=[xtb])
            cs, csb = csr.next()
            S_.dma("sp", cs[:, 0, :], cos_d[:, t0:t0 + 128], reads=[tab_b], writes=[csb])
            S_.dma("sp", cs[:, 1, :], sin_d[:, t0:t0 + 128], reads=[tab_b], writes=[csb], add=True)
            pre[k] = (xt, xtb, cs, csb)

        def stageA(k):
            b, n = items[k]
            t0 = n * 128
            xt, xtb, cs, csb = pre.pop(k)
            xb, xbb = xbr.next()
            S_.op("pool", lambda: nc.gpsimd.tensor_copy(out=xb[:], in_=xt[:]), reads=[xtb], writes=[xbb])
            xT, xTb = xTr.next()
            _transpose_to(cx, c, xb, xbb, 8,
                          lambda k0, k1: xT[:, k0:k1, :].rearrange("p a b -> p (a b)"), xTb, evac="dve")
            qk, qkb = qkr.next()
            for h in range(RH):
                bk, bb = cx.bank()
                bv = bk[:].rearrange("p (a b c) -> p a b c", a=2, b=2)
                for qi in range(2):
                    for half in range(2):
                        col = qi * 1024 + h * 256 + half * 128
                        for kc in range(8):
                            S_.op("pe", lambda kc=kc, col=col, qi=qi, half=half: nc.tensor.matmul(
                                bv[:, qi, half, :], lhsT=W[:, kc, col:col + 128], rhs=xT[:, kc, :],
                                start=(kc == 0), stop=(kc == 7)),
                                reads=[Wb[qi], xTb], writes=[bb], inc=(kc == 7 and qi == 1 and half == 1))
                cosb = cs[:, 0:1, :].broadcast_to([128, 2, 128])
                sinb = cs[:, 1:2, :].broadcast_to([128, 2, 128])
                A = bv[:, :, 0, :]
                Bh = bv[:, :, 1, :]
                t1, t1b = t1r.next()
                t2, t2b = t2r.next()
                S_.op("dve", lambda: nc.vector.tensor_tensor(out=t1[:], in0=A, in1=cosb, op=ALU.mult),
                      reads=[bb, csb], writes=[t1b])
                S_.op("dve", lambda: nc.vector.tensor_tensor(out=t2[:], in0=Bh, in1=sinb, op=ALU.mult),
                      reads=[bb, csb], writes=[t2b])
                S_.op("pool", lambda h=h: nc.gpsimd.tensor_tensor(out=qk[:, h, :, 0, :], in0=t1[:], in1=t2[:],
                                                                   op=ALU.subtract),
                      reads=[t1b, t2b], writes=[qkb])
                t1, t1b = t1r.next()
                t2, t2b = t2r.next()
                S_.op("dve", lambda: nc.vector.tensor_tensor(out=t1[:], in0=A, in1=sinb, op=ALU.mult),
                      reads=[bb, csb], writes=[t1b])
                S_.op("dve", lambda: nc.vector.tensor_tensor(out=t2[:], in0=Bh, in1=cosb, op=ALU.mult),
                      reads=[bb, csb], writes=[t2b])
                S_.op("pool", lambda h=h: nc.gpsimd.tensor_tensor(out=qk[:, h, :, 1, :], in0=t1[:], in1=t2[:],
                                                                   op=ALU.add),
                      reads=[t1b, t2b], writes=[qkb])
            v, vb = vr.next()
            for j in range(4):
                bk, bb = cx.bank()
                for kc in range(8):
                    S_.op("pe", lambda kc=kc, j=j: nc.tensor.matmul(
                        bk[:], lhsT=xT[:, kc, :], rhs=W[:, kc, 2048 + j * 512:2048 + (j + 1) * 512],
                        start=(kc == 0), stop=(kc == 7)), reads=[Wb[2 + j // 2], xTb], writes=[bb],
                        inc=(kc == 7))
                S_.op("act", lambda j=j: nc.scalar.copy(out=v[:, j * 512:(j + 1) * 512], in_=bk[:]),
                      reads=[bb], writes=[vb])
            return (qk, qkb, v, vb)

        def stageB(k, stt):
            b, n = items[k]
            t0 = n * 128
            qk, qkb, v, vb = stt
            state, state_bf, stb, stbb = states[b], states_bf[b], stbs[b], stbbs[b]
            bk, bb = cx.bank()
            bs = bk[:].rearrange("p (h i) -> p h i", h=RH)
            for h in range(RH):
                for half in range(2):
                    S_.op("pe", lambda h=h, half=half: nc.tensor.matmul(
                        bs[:, h, :], lhsT=qk[:, h, 1, half, :], rhs=qk[:, h, 0, half, :],
                        start=(half == 0), stop=(half == 1)), reads=[qkb], writes=[bb],
                        inc=(h == RH - 1 and half == 1))
            sT, sTb = sTr.next()
            S_.op("dve", lambda: nc.vector.tensor_tensor(out=sT[:], in0=bs, in1=dec[:], op=ALU.mult),
                  reads=[bb, bdec], writes=[sTb])
            kd, kdb = kdr.next()
            if n < NT - 1:
                bk, bb = cx.bank()
                pb = bk.bitcast(BF16)
                for h in range(RH):
                    for half in range(2):
                        o = (h * 2 + half) * 128
                        S_.op("pe", lambda h=h, half=half, o=o: nc.tensor.transpose(
                            out=pb[:, o:o + 128], in_=qk[:, h, 1, half, :], identity=c["ident"][:]),
                            reads=[qkb, c["ident_b"]], writes=[bb], inc=(h == RH - 1 and half == 1))
                S_.op("dve", lambda: nc.vector.tensor_tensor(
                    out=kd[:], in0=pb[:, 0:1024].rearrange("p (h d) -> p h d", h=RH),
                    in1=kdec[:].unsqueeze(2).broadcast_to([128, RH, 256]), op=ALU.mult),
                    reads=[bb, bdec], writes=[kdb])
            qd, qdb = qdr.next()
            if n > 0:
                S_.op("pool", lambda: nc.gpsimd.tensor_tensor(
                    out=qd[:], in0=qk[:, :, 0, :, :],
                    in1=qdec[:].unsqueeze(2).broadcast_to([128, RH, 2, 128]), op=ALU.mult),
                    reads=[qkb, bdec], writes=[qdb])
            yn, ynb = ynr.next()
            st6, st6b = st6r.next()
            mv, mvb = mvr.next()
            rs, rsb = rsr.next()
            pos = []
            for h in range(RH):
                po, pob = cx.bank()
                pos.append((po, pob))
                S_.op("pe", lambda h=h, po=po: nc.tensor.matmul(po[:], lhsT=sT[:, h, :],
                                                                rhs=v[:, h * 512:(h + 1) * 512],
                                                                start=True, stop=(n == 0)),
                      reads=[sTb, vb], writes=[pob], inc=(n == 0))
                if n > 0:
                    for half in range(2):
                        S_.op("pe", lambda h=h, half=half, po=po: nc.tensor.matmul(
                            po[:], lhsT=qd[:, h, half, :], rhs=state_bf[:, h, half, :],
                            start=False, stop=(half == 1)),
                            reads=[qdb, stbb[h][half]], writes=[pob], inc=(half == 1))
                S_.op("dve", lambda h=h, po=po: nc.vector.bn_stats(out=st6[:, h, :], in_=po[:]),
                      reads=[pob], writes=[st6b])
                S_.op("dve", lambda h=h: nc.vector.bn_aggr(out=mv[:, h, :], in_=st6[:, h, :]),
                      reads=[st6b], writes=[mvb])
                S_.op("act", lambda h=h: nc.scalar.activation(out=rs[:, h:h + 1], in_=mv[:, h, 1:2], func=AF.Sqrt,
                                                              bias=c["eps"][:], scale=1.0),
                      reads=[mvb, c["eps_b"]], writes=[rsb])
                S_.op("dve", lambda h=h: nc.vector.reciprocal(out=rs[:, h:h + 1], in_=rs[:, h:h + 1]),
                      reads=[rsb], writes=[rsb])
                S_.op("dve", lambda h=h, po=po: nc.vector.tensor_scalar(
                    out=yn[:, h * 512:(h + 1) * 512], in0=po[:], scalar1=mv[:, h, 0:1], scalar2=rs[:, h:h + 1],
                    op0=ALU.subtract, op1=ALU.mult), reads=[pob, mvb, rsb], writes=[ynb])
                if n < NT - 1:
                    for half in range(2):
                        pu, pub = cx.bank()
                        S_.op("pe", lambda h=h, half=half, pu=pu: nc.tensor.matmul(
                            pu[:], lhsT=kd[:, h, half * 128:(half + 1) * 128], rhs=v[:, h * 512:(h + 1) * 512],
                            start=True, stop=True), reads=[kdb, vb], writes=[pub])
                        if n == 0:
                            S_.op("dve", lambda h=h, half=half, pu=pu: nc.vector.tensor_copy(
                                out=state[:, h, half, :], in_=pu[:]), reads=[pub], writes=[stb[h][half]])
                        else:
                            S_.op("dve", lambda h=h, half=half, pu=pu: nc.vector.scalar_tensor_tensor(
                                out=state[:, h, half, :], in0=state[:, h, half, :],
                                scalar=math.exp(lg[h] * 128), in1=pu[:], op0=ALU.mult, op1=ALU.add),
                                reads=[pub, stb[h][half]], writes=[stb[h][half]])
                        S_.op("act", lambda h=h, half=half: nc.scalar.copy(
                            out=state_bf[:, h, half, :], in_=state[:, h, half, :]),
                            reads=[stb[h][half]], writes=[stbb[h][half]])
            S_.dma("sp", yn_d[b, t0:t0 + 128, :], yn[:], reads=[ynb], writes=[yn_b], add=True)


        loads(0)
        loads(1)
        nxt = stageA(0)
        for k in range(len(items)):
            cur = nxt
            if k + 2 < len(items):
                loads(k + 2)
            if k + 1 < len(items):
                nxt = stageA(k + 1)
            stageB(k, cur)


def phase_ret2(cx, c, x_in, x_b, w_in, gn_g, w_out, ln_g, ln_b, yn_d, yn_b, x1_d, x1_b, side=None):
    nc, S_ = cx.nc, cx.S
    with ExitStack() as st:
        Wg = st.enter_context(nc.sbuf_tensor(_u("r2_Wg"), [128, 8, 2048], BF16))
        Wo = st.enter_context(nc.sbuf_tensor(_u("r2_Wo"), [128, 16, 1024], BF16))
        Wgb, Wob = [Buf(), Buf()], [Buf(), Buf()]
        for j in range(2):
            _load_w(cx, Wg, Wgb[j], w_in, 8, 4096 + j * 1024, 4096 + (j + 1) * 1024, dst_c0=j * 1024)
        for j in range(2):
            _load_w(cx, Wo, Wob[j], w_out, 16, j * 512, (j + 1) * 512)
        gng = st.enter_context(nc.sbuf_tensor(_u("r2_gng"), [128, 2048], F32))
        lng = st.enter_context(nc.sbuf_tensor(_u("r2_lng"), [128, D], F32))
        lnb = st.enter_context(nc.sbuf_tensor(_u("r2_lnb"), [128, D], F32))
        pb_ = Buf()
        S_.dma("sp", gng[:], gn_g[0:1, :].partition_broadcast(128), writes=[pb_])
        S_.dma("sp", lng[:], ln_g[0:1, :].partition_broadcast(128), writes=[pb_], add=True)
        S_.dma("sp", lnb[:], ln_b[0:1, :].partition_broadcast(128), writes=[pb_], add=True)
        xtok = Ring(nc, st, "r2_xtok", [128, D], F32, 4)
        xbr = Ring(nc, st, "r2_xb", [128, D], BF16, 2)
        xTr = Ring(nc, st, "r2_xT", [128, 8, 128], BF16, 2)
        ynr = Ring(nc, st, "r2_yn", [128, 2048], BF16, 4)
        sgr = Ring(nc, st, "r2_sg", [128, 2048], F32, 2)
        yr = Ring(nc, st, "r2_y", [128, 2048], BF16, 3)
        yTr = Ring(nc, st, "r2_yT", [128, 16, 128], BF16, 2)
        zr = Ring(nc, st, "r2_z", [128, D], F32, 2)
        st6r = Ring(nc, st, "r2_st6", [128, 2, 6], F32, 2)
        mvr = Ring(nc, st, "r2_mv", [128, 2], F32, 2)
        rsr = Ring(nc, st, "r2_rs", [128, 1], F32, 2)
        items = [(b, n) for b in range(NB) for n in range(NT)]
        pre = {}

        def loads(k):
            b, n = items[k]
            t0 = n * 128
            xt, xtb = xtok.next()
            S_.dma("sp", xt[:], x_in[b, t0:t0 + 128, :], reads=[x_b], writes=[xtb])
            yn, ynb = ynr.next()
            S_.dma("sp", yn[:], yn_d[b, t0:t0 + 128, :], reads=[yn_b], writes=[ynb])
            pre[k] = (xt, xtb, yn, ynb)

        def stage1(k):
            xt, xtb, yn, ynb = pre.pop(k)
            xb, xbb = xbr.next()
            S_.op("pool", lambda: nc.gpsimd.tensor_copy(out=xb[:], in_=xt[:]), reads=[xtb], writes=[xbb])
            xT, xTb = xTr.next()
            _transpose_to(cx, c, xb, xbb, 8,
                          lambda k0, k1: xT[:, k0:k1, :].rearrange("p a b -> p (a b)"), xTb, evac="dve")
            sg, sgb = sgr.next()
            for j in range(4):
                bk, bb = cx.bank()
                for kc in range(8):
                    S_.op("pe", lambda kc=kc, j=j, bk=bk: nc.tensor.matmul(
                        bk[:], lhsT=xT[:, kc, :], rhs=Wg[:, kc, j * 512:(j + 1) * 512],
                        start=(kc == 0), stop=(kc == 7)), reads=[Wgb[j // 2], xTb], writes=[bb], inc=(kc == 7))
                S_.op("act", lambda j=j, bk=bk: nc.scalar.activation(out=sg[:, j * 512:(j + 1) * 512], in_=bk[:],
                                                                     func=AF.Silu), reads=[bb], writes=[sgb])
            S_.op("pool", lambda: nc.gpsimd.tensor_tensor(out=sg[:], in0=sg[:], in1=gng[:], op=ALU.mult),
                  reads=[sgb, pb_], writes=[sgb])
            y, yb = yr.next()
            S_.op("dve", lambda: nc.vector.tensor_tensor(out=y[:], in0=sg[:], in1=yn[:], op=ALU.mult),
                  reads=[sgb, ynb], writes=[yb])
            return (xt, xtb, y, yb)

        def stage2(k, stt):
            b, n = items[k]
            t0 = n * 128
            xt, xtb, y, yb = stt
            yT, yTb = yTr.next()
            _transpose_to(cx, c, y, yb, 16,
                          lambda k0, k1: yT[:, k0:k1, :].rearrange("p a b -> p (a b)"), yTb, evac="alt")
            z, zb = zr.next()
            for hh in range(2):
                bk, bb = cx.bank()
                for kc in range(16):
                    S_.op("pe", lambda kc=kc, hh=hh, bk=bk: nc.tensor.matmul(
                        bk[:], lhsT=yT[:, kc, :], rhs=Wo[:, kc, hh * 512:(hh + 1) * 512],
                        start=(kc == 0), stop=(kc == 15)), reads=[Wob[hh], yTb], writes=[bb], inc=(kc == 15))
                S_.op("dve", lambda hh=hh, bk=bk: nc.vector.scalar_tensor_tensor(
                    out=z[:, hh * 512:(hh + 1) * 512], in0=xt[:, hh * 512:(hh + 1) * 512], scalar=DN_ALPHA,
                    in1=bk[:], op0=ALU.mult, op1=ALU.add), reads=[bb, xtb], writes=[zb])
            st6, sb = st6r.next()
            mv, _ = mvr.next()
            rs, _ = rsr.next()
            _layernorm_store(cx, c, z, zb, lng, lnb, pb_, x1_d[b, t0:t0 + 128, :], x1_b, st6, mv, rs, sb)

        loads(0)
        loads(1)
        nxt = stage1(0)
        for k in range(len(items)):
            cur = nxt
            if k + 2 < len(items):
                loads(k + 2)
            if k + 1 < len(items):
                nxt = stage1(k + 1)
            stage2(k, cur)
            if side is not None:
                side(k, st)


def _load_w3(cx, dst_ap, w_ap, r0, nk, c0, c1, wb, add=False, q="pool"):
    src = w_ap[r0:r0 + nk * 128, c0:c1].rearrange("(k p) c -> p k c", p=128)
    cx.S.dma(q, dst_ap, src, writes=[wb], add=add)


def phase_ffn(cx, c, x_d, x_b, w_ins, w_outs, F, router, ln_g, ln_b, dst_d, dst_b, T=1024):
    nc, S_ = cx.nc, cx.S
    ne = int(os.environ.get('DBG_NE', len(w_ins)))
    moe = router is not None and not os.environ.get('DBG_NOROUTER')
    ntt = T // 128
    groups = []
    f0 = 0
    while f0 < F:
        g = min(512, F - f0)
        groups.append((f0, g))
        f0 += g
    with ExitStack() as st:
        lng = st.enter_context(nc.sbuf_tensor(_u("f_lng"), [128, D], F32))
        lnb = st.enter_context(nc.sbuf_tensor(_u("f_lnb"), [128, D], F32))
        pb_ = Buf()
        S_.dma("sp", lng[:], ln_g.partition_broadcast(128), writes=[pb_])
        S_.dma("sp", lnb[:], ln_b.partition_broadcast(128), writes=[pb_], add=True)
        if moe:
            wr = st.enter_context(nc.sbuf_tensor(_u("f_wr"), [128, 8, NE], F32))
            wrb = Buf()
            S_.dma("sp", wr[:], router.rearrange("(k p) e -> p k e", p=128), writes=[wrb])
            ident32 = st.enter_context(nc.sbuf_tensor(_u("f_id32"), [128, 128], F32))
            idb = Buf()
            S_.op("pool", lambda: nc.gpsimd.tensor_copy(out=ident32[:], in_=c["ident"][:]), reads=[c["ident_b"]],
                  writes=[idb])
            gate = st.enter_context(nc.sbuf_tensor(_u("f_gate"), [128, ntt, NE], F32))
            gateb = Buf()
            xT32r = Ring(nc, st, "f_xT32", [128, 8, 128], F32, 2)
            lgr = Ring(nc, st, "f_lg", [128, NE], F32, 2)
            topr = Ring(nc, st, "f_top", [128, 8], F32, 2)
            ssr = Ring(nc, st, "f_ss", [128, 1], F32, 2)
            sgr = Ring(nc, st, "f_sg", [128, NE], F32, 2)
        acc = st.enter_context(nc.sbuf_tensor(_u("f_acc"), [128, ntt, D], F32))
        accb = [Buf() for _ in range(ntt)]
        nxT = 1 if moe else 2
        xTs = [st.enter_context(nc.sbuf_tensor(_u("f_xT"), [128, 8, T], BF16)) for _ in range(nxT)]
        xTbs = [[Buf() for _ in range(ntt)] for _ in range(nxT)]
        xtok = Ring(nc, st, "f_xtok", [128, D], F32, 4)
        xbr = Ring(nc, st, "f_xb", [128, D], BF16, 2)
        Wir = Ring(nc, st, "f_Wi", [128, 8, 2, 512], BF16, 2)
        Wor = Ring(nc, st, "f_Wo", [128, 4, D], BF16, 2)
        hTr = Ring(nc, st, "f_hT", [128, 4, T], BF16, 2)
        sar = Ring(nc, st, "f_sa", [128, 512], F32, 3)
        st6r = Ring(nc, st, "f_st6", [128, 2, 6], F32, 2)
        mvr = Ring(nc, st, "f_mv", [128, 2], F32, 2)
        rsr = Ring(nc, st, "f_rs", [128, 1], F32, 2)

        work = [(e, gi) for e in range(ne) for gi in range(len(groups))]
        wpre = {}

        def wload(k):
            e, gi = work[k % len(work)]
            f0, g = groups[gi]
            Wi, Wib = Wir.next()
            Wo, Wob = Wor.next()
            _load_w3(cx, Wi[:, :, 0, 0:g], w_ins[e], 0, 8, f0, f0 + g, Wib)
            _load_w3(cx, Wi[:, :, 1, 0:g], w_ins[e], 0, 8, F + f0, F + f0 + g, Wib, add=True)
            _load_w3(cx, Wo[:, 0:g // 128, :], w_outs[e], f0, g // 128, 0, D, Wob)
            wpre[k] = (Wi, Wib, Wo, Wob)

        nmac = NB * S // T

        def early(m):
            b = (m * T) // S
            s0 = (m * T) % S
            xT, xTb = xTs[m % nxT], xTbs[m % nxT]
            for tt in range(ntt):
                xt, xtb = xtok.next()
                S_.dma("sp", xt[:], x_d[b, s0 + tt * 128:s0 + (tt + 1) * 128, :], reads=[x_b], writes=[xtb])
                xb, xbb = xbr.next()
                S_.op("act", lambda xb=xb, xt=xt: nc.scalar.copy(out=xb[:], in_=xt[:]), reads=[xtb], writes=[xbb])
                _transpose_to(cx, c, xb, xbb, 8,
                              lambda k0, k1, tt=tt, xT=xT: xT[:, k0:k1, tt * 128:(tt + 1) * 128], xTb[tt])

        wk = 0
        wload(0)
        if not moe:
            early(0)
        for m in range(nmac):
            b = (m * T) // S
            s0 = (m * T) % S
            xT, xTb = xTs[m % nxT], xTbs[m % nxT]
            for tt in range(ntt):
                xt, xtb = xtok.next()
                S_.dma("sp", xt[:], x_d[b, s0 + tt * 128:s0 + (tt + 1) * 128, :], reads=[x_b], writes=[xtb])
                S_.op("act", lambda tt=tt, xt=xt: nc.scalar.mul(out=acc[:, tt, :], in_=xt[:], mul=DN_ALPHA),
                      reads=[xtb], writes=[accb[tt]])
                if not moe:
                    pass
                else:
                    xT32, xT32b = xT32r.next()
                    for k0 in (0, 4):
                        bk, bb = cx.bank()
                        for k in range(k0, k0 + 4):
                            S_.op("pe", lambda k=k, k0=k0, bk=bk, xt=xt: nc.tensor.transpose(
                                out=bk[:, (k - k0) * 128:(k - k0 + 1) * 128], in_=xt[:, k * 128:(k + 1) * 128],
                                identity=ident32[:]), reads=[xtb, idb], writes=[bb], inc=(k == k0 + 3))
                        S_.op("act", lambda k0=k0, bk=bk, xT32=xT32: nc.scalar.copy(
                            out=xT32[:, k0:k0 + 4, :], in_=bk[:].rearrange("p (a b) -> p a b", a=4)),
                            reads=[bb], writes=[xT32b])
                        S_.op("pool", lambda k0=k0, xT32=xT32, tt=tt: nc.gpsimd.tensor_copy(
                            out=xT[:, k0:k0 + 4, tt * 128:(tt + 1) * 128],
                            in_=xT32[:, k0:k0 + 4, :]), reads=[xT32b], writes=[xTb[tt]])
                    if DBG_R < 2:
                        continue
                    bk, bb = cx.bank()
                    for kc in range(8):
                        S_.op("pe", lambda kc=kc, bk=bk, xT32=xT32: nc.tensor.matmul(
                            bk[:, 0:NE], lhsT=xT32[:, kc, :], rhs=wr[:, kc, :], start=(kc == 0), stop=(kc == 7)),
                            reads=[xT32b, wrb], writes=[bb], inc=(kc == 7))
                    lg_, lgb = lgr.next()
                    top, topb = topr.next()
                    ss, ssb = ssr.next()
                    sg, sgb = sgr.next()
                    S_.op("dve", lambda bk=bk, lg_=lg_: nc.vector.tensor_copy(out=lg_[:], in_=bk[:, 0:NE]),
                          reads=[bb], writes=[lgb])
                    if DBG_R < 3:
                        continue
                    S_.op("dve", lambda lg_=lg_, top=top: nc.vector.max(out=top[:], in_=lg_[:]), reads=[lgb],
                          writes=[topb])
                    S_.op("dve", lambda top=top, ss=ss: nc.vector.tensor_tensor(out=ss[:], in0=top[:, 0:1],
                                                                               in1=top[:, 1:2], op=ALU.add),
                          reads=[topb], writes=[ssb])
                    S_.op("dve", lambda lg_=lg_, ss=ss, sg=sg: nc.vector.tensor_scalar(
                        out=sg[:], in0=lg_[:], scalar1=2.0, scalar2=ss[:, 0:1], op0=ALU.mult, op1=ALU.subtract),
                        reads=[lgb, ssb], writes=[sgb])
                    S_.op("act", lambda sg=sg: nc.scalar.activation(out=sg[:], in_=sg[:], func=AF.Sigmoid),
                          reads=[sgb], writes=[sgb])
                    S_.op("dve", lambda lg_=lg_, top=top, sg=sg, tt=tt: nc.vector.scalar_tensor_tensor(
                        out=gate[:, tt, :], in0=lg_[:], scalar=top[:, 1:2], in1=sg[:], op0=ALU.is_ge, op1=ALU.mult),
                        reads=[lgb, topb, sgb], writes=[gateb])
            for e in range(ne):
                for gi, (f0, g) in enumerate(groups):
                    nfc = g // 128
                    if (not moe) and e == 0 and gi == 1 and m + 1 < nmac:
                        early(m + 1)
                    if wk + 1 < nmac * len(work):
                        wload(wk + 1)
                    Wi, Wib, Wo, Wob = wpre.pop(wk)
                    wk += 1
                    hT, hTb = hTr.next()
                    for ts in range(T // 512):
                        tsl = slice(ts * 512, (ts + 1) * 512)
                        xdeps = xTb[ts * 4:(ts + 1) * 4]
                        for fc in range(nfc):
                            pa, pab = cx.bank()
                            pbk, pbb = cx.bank()
                            for ab, (pp, ppb) in enumerate(((pa, pab), (pbk, pbb))):
                                for kc in range(8):
                                    S_.op("pe", lambda kc=kc, ab=ab, pp=pp, fc=fc, Wi=Wi: nc.tensor.matmul(
                                        pp[:], lhsT=Wi[:, kc, ab, fc * 128:(fc + 1) * 128], rhs=xT[:, kc, tsl],
                                        start=(kc == 0), stop=(kc == 7)), reads=[Wib] + xdeps, writes=[ppb],
                                        inc=(kc == 7))
                            sa, sab = sar.next()
                            S_.op("act", lambda sa=sa, pa=pa: nc.scalar.activation(out=sa[:], in_=pa[:],
                                                                                   func=AF.Silu),
                                  reads=[pab], writes=[sab])
                            S_.op("dve", lambda sa=sa, pbk=pbk, hT=hT, fc=fc: nc.vector.tensor_tensor(
                                out=hT[:, fc, tsl], in0=sa[:], in1=pbk[:], op=ALU.mult), reads=[sab, pbb],
                                writes=[hTb])
                    for tt in range(ntt):
                        for hh in range(2):
                            po, pob = cx.bank()
                            for fc in range(nfc):
                                S_.op("pe", lambda fc=fc, po=po, hT=hT, Wo=Wo, tt=tt, hh=hh: nc.tensor.matmul(
                                    po[:], lhsT=hT[:, fc, tt * 128:(tt + 1) * 128],
                                    rhs=Wo[:, fc, hh * 512:(hh + 1) * 512], start=(fc == 0), stop=(fc == nfc - 1)),
                                    reads=[hTb, Wob], writes=[pob], inc=(fc == nfc - 1))
                            asl = acc[:, tt, hh * 512:(hh + 1) * 512]
                            if moe and DBG_R >= 4:
                                S_.op("dve", lambda po=po, asl=asl, tt=tt, e=e: nc.vector.scalar_tensor_tensor(
                                    out=asl, in0=po[:], scalar=gate[:, tt, e:e + 1], in1=asl, op0=ALU.mult,
                                    op1=ALU.add), reads=[pob, gateb, accb[tt]], writes=[accb[tt]])
                            else:
                                S_.op("dve", lambda po=po, asl=asl: nc.vector.tensor_tensor(
                                    out=asl, in0=po[:], in1=asl, op=ALU.add), reads=[pob, accb[tt]],
                                    writes=[accb[tt]])
            for tt in range(ntt):
                st6, sb = st6r.next()
                mv, _ = mvr.next()
                rs, _ = rsr.next()
                _layernorm_store(cx, c, acc[:, tt, :], accb[tt], lng, lnb, pb_,
                                 dst_d[b, s0 + tt * 128:s0 + (tt + 1) * 128, :], dst_b, st6, mv, rs, sb)


def _nsa_pieces(w_q, w_kv):
    pieces = []
    for i in range(4):
        pieces.append(("q", w_q, i * 256, 256, False, True, i))
    pieces.append(("kc", w_kv, 0, 256, False, True, 0))
    pieces.append(("vc", w_kv, 256, 256, False, False, 0))
    for i in range(2):
        pieces.append(("ks", w_kv, 512 + i * 128, 128, True, True, i))
    for i in range(2):
        pieces.append(("kw", w_kv, 1024 + i * 128, 128, True, True, i))
    return pieces


def _nsa_prep_rings(nc, st):
    return (Ring(nc, st, "n0_Wr", [128, 8, 256], BF16, 2), Ring(nc, st, "n0_Wp", [128, 8, 256], BF16, 2),
            Ring(nc, st, "n0_Ws", [128, 8, 256], BF16, 2))


def _nsa_prep_piece(cx, pieces, pi, rings, wsw_d, wswb):
    nc, S_ = cx.nc, cx.S
    Wrr, Wpr, Wsr = rings
    kind, wsrc, c0, ncr, dup, rot, idx = pieces[pi]
    Wr, Wrb = Wrr.next()
    _load_w3(cx, Wr[:, :, 0:ncr], wsrc, 0, 8, c0, c0 + ncr, Wrb)
    if dup:
        Wp, Wpb = Wpr.next()
        for gg in range(2):
            S_.op("pool", lambda Wp=Wp, Wr=Wr, gg=gg: nc.gpsimd.tensor_copy(
                out=Wp[:, :, gg * 128:(gg + 1) * 128].rearrange("p k (t d) -> p k t d", t=2),
                in_=Wr[:, :, gg * 64:(gg + 1) * 64].unsqueeze(2).broadcast_to([128, 8, 2, 64])),
                reads=[Wrb], writes=[Wpb])
    else:
        Wp, Wpb = Wr, Wrb
    S_.dma("sp", wsw_d[2 * pi], Wp[:], reads=[Wpb], writes=[wswb], add=True)
    if rot:
        Ws, Wsb = Wsr.next()
        wp5 = Wp[:].rearrange("p k (h t d) -> p k h t d", t=2, d=32)
        ws5 = Ws[:].rearrange("p k (h t d) -> p k h t d", t=2, d=32)
        for k0 in (0, 4):
            S_.op("pool", lambda ws5=ws5, wp5=wp5, k0=k0: nc.gpsimd.tensor_copy(
                out=ws5[:, k0:k0 + 4, :, 0, :], in_=wp5[:, k0:k0 + 4, :, 1, :]), reads=[Wpb], writes=[Wsb])
            S_.op("pool", lambda ws5=ws5, wp5=wp5, k0=k0: nc.gpsimd.tensor_copy(
                out=ws5[:, k0:k0 + 4, :, 1, :], in_=wp5[:, k0:k0 + 4, :, 0, :]), reads=[Wpb], writes=[Wsb])
        S_.dma("sp", wsw_d[2 * pi + 1], Ws[:], reads=[Wsb], writes=[wswb], add=True)


SLOT_R = [0, 2, 1, 3]


def phase_nsa(cx, c, x2_d, x2_b, w_kv, ck_pe, ck_w1, ck_w2, cv_pe, cv_w1, cv_w2, w_q, w_out, ln_g, ln_b,
              cos_d, sin_d, tab_b, x3_d, x3_b, wsw_d, dbg_kc=None, dbg_vc=None, dbg_b=None, wswb=None):
    nc, S_ = cx.nc, cx.S
    ident = c["ident"]
    idb = c["ident_b"]
    with ExitStack() as st:
        QT2 = st.enter_context(nc.sbuf_tensor(_u("n_QT2"), [128, 8, S], BF16))
        KS = st.enter_context(nc.sbuf_tensor(_u("n_KS"), [128, NG, S], BF16))
        KW = st.enter_context(nc.sbuf_tensor(_u("n_KW"), [128, NG, S], BF16))
        Vaug = st.enter_context(nc.sbuf_tensor(_u("n_Vaug"), [128, NT, 2, NG, 65], BF16))
        G3 = st.enter_context(nc.sbuf_tensor(_u("n_G3"), [128, NT, NG, 4, 3], F32))
        kcT = st.enter_context(nc.sbuf_tensor(_u("n_kcT"), [128, NG, 128], BF16))
        vcaug = st.enter_context(nc.sbuf_tensor(_u("n_vcaug"), [128, NG, 97], BF16))
        Emat = st.enter_context(nc.sbuf_tensor(_u("n_E"), [96, S], BF16))
        tri4 = st.enter_context(nc.sbuf_tensor(_u("n_tri4"), [128, 2, 128], BF16))
        tri4b = st.enter_context(nc.sbuf_tensor(_u("n_tri4b"), [128, 2, 128], BF16))
        cmask = st.enter_context(nc.sbuf_tensor(_u("n_cmask"), [128, NT, 128], BF16))
        M1 = st.enter_context(nc.sbuf_tensor(_u("n_M1"), [128, NT, 32], F32))
        M2 = st.enter_context(nc.sbuf_tensor(_u("n_M2"), [128, NT, 32], F32))
        VAL = st.enter_context(nc.sbuf_tensor(_u("n_VAL"), [128, NT, 32], F32))
        ovt = st.enter_context(nc.sbuf_tensor(_u("n_ov"), [128, 32], F32))
        QTb, KSb, KWb, Vb, G3b, kcTb, vcb, cb, Wob, pb_ = [Buf() for _ in range(10)]
        P = "pool"
        S_.op(P, lambda: nc.gpsimd.memset(Emat[:], 1.0), writes=[cb])
        for p0 in (0, 64):
            S_.op(P, lambda p0=p0: nc.gpsimd.affine_select(out=Emat[p0:p0 + 32], in_=Emat[p0:p0 + 32], pattern=[[1, S]],
                                                           compare_op=ALU.is_ge, fill=cx.reg(0.0), base=0,
                                                           channel_multiplier=-64), reads=[cb], writes=[cb])
            S_.op(P, lambda p0=p0: nc.gpsimd.affine_select(out=Emat[p0:p0 + 32], in_=Emat[p0:p0 + 32], pattern=[[-1, S]],
                                                           compare_op=ALU.is_ge, fill=cx.reg(0.0), base=63,
                                                           channel_multiplier=64), reads=[cb], writes=[cb])
        S_.op(P, lambda: nc.gpsimd.memset(tri4[:], 0.0), writes=[cb])
        S_.op(P, lambda: nc.gpsimd.affine_select(out=tri4[:], in_=tri4[:], pattern=[[0, 2], [1, 128]],
                                                 compare_op=ALU.is_ge, fill=cx.reg(NEG), base=0, channel_multiplier=-1),
              reads=[cb], writes=[cb])
        S_.op(P, lambda: nc.gpsimd.memset(tri4b[:], 0.0), writes=[cb])
        S_.op(P, lambda: nc.gpsimd.affine_select(out=tri4b[:], in_=tri4b[:], pattern=[[0, 2], [-1, 128]],
                                                 compare_op=ALU.is_ge, fill=cx.reg(NEG), base=-1, channel_multiplier=1),
              reads=[cb], writes=[cb])
        S_.op(P, lambda: nc.gpsimd.memset(cmask[:], 1.0), writes=[cb])
        S_.op(P, lambda: nc.gpsimd.affine_select(out=cmask[:], in_=cmask[:], pattern=[[128, NT], [1, 128]],
                                                 compare_op=ALU.is_ge, fill=cx.reg(0.0), base=-31, channel_multiplier=-16),
              reads=[cb], writes=[cb])
        S_.op(P, lambda: nc.gpsimd.memset(VAL[:], 1.0), writes=[cb])
        S_.op(P, lambda: nc.gpsimd.memset(M1[:], 0.0), writes=[cb])
        for half in range(2):
            ps = slice(half * 64, (half + 1) * 64)
            S_.op(P, lambda ps=ps, half=half: nc.gpsimd.affine_select(
                out=VAL[ps], in_=VAL[ps], pattern=[[2, NT], [-1, 32]], compare_op=ALU.is_ge, fill=cx.reg(0.0), base=half,
                channel_multiplier=0), reads=[cb], writes=[cb])
            for off in (0, 1):
                S_.op(P, lambda ps=ps, half=half, off=off: nc.gpsimd.affine_select(
                    out=M1[ps], in_=M1[ps], pattern=[[-2, NT], [1, 32]], compare_op=ALU.not_equal, fill=cx.reg(1.0),
                    base=-half + off, channel_multiplier=0), reads=[cb], writes=[cb])
        S_.op(P, lambda: nc.gpsimd.memset(M1[:, :, 0:1], 1.0), reads=[cb], writes=[cb])
        S_.op(P, lambda: nc.gpsimd.tensor_tensor(out=M1[:], in0=M1[:], in1=VAL[:], op=ALU.mult), reads=[cb], writes=[cb])
        S_.op("dve", lambda: nc.vector.tensor_scalar(out=M2[:], in0=VAL[:], scalar1=-1.0, scalar2=1e30, op0=ALU.add,
                                                     op1=ALU.mult), reads=[cb], writes=[cb])
        S_.op("dve", lambda: nc.vector.scalar_tensor_tensor(out=M2[:], in0=M1[:], scalar=1e9, in1=M2[:], op0=ALU.mult,
                                                            op1=ALU.add), reads=[cb], writes=[cb])
        S_.op("dve", lambda: nc.vector.tensor_tensor(out=M1[:], in0=VAL[:], in1=M1[:], op=ALU.subtract), reads=[cb],
              writes=[cb])
        S_.op(P, lambda: nc.gpsimd.iota(ovt[:], pattern=[[-64, 32]], base=0, channel_multiplier=16,
                                        allow_small_or_imprecise_dtypes=True), reads=[cb], writes=[cb])
        with ExitStack() as st2:
            lo = st2.enter_context(nc.sbuf_tensor(_u("n_lo"), [128, 32], F32))
            S_.op("dve", lambda: nc.vector.tensor_scalar(out=lo[:], in0=ovt[:], scalar1=0.0, scalar2=None, op0=ALU.max),
                  reads=[cb], writes=[cb])
            S_.op("dve", lambda: nc.vector.tensor_scalar(out=ovt[:], in0=ovt[:], scalar1=32.0, scalar2=64.0,
                                                         op0=ALU.add, op1=ALU.min), reads=[cb], writes=[cb])
            S_.op("dve", lambda: nc.vector.tensor_tensor(out=ovt[:], in0=ovt[:], in1=lo[:], op=ALU.subtract),
                  reads=[cb], writes=[cb])
            S_.op("dve", lambda: nc.vector.tensor_scalar(out=ovt[:], in0=ovt[:], scalar1=0.0, scalar2=1.0 / 16.0,
                                                         op0=ALU.max, op1=ALU.mult), reads=[cb], writes=[cb])
            for g in range(NG):
                S_.op("dve", lambda g=g: nc.vector.tensor_copy(out=vcaug[:, g, 64:96], in_=ovt[:]), reads=[cb],
                      writes=[vcb])
            S_.op("dve", lambda: nc.vector.memset(vcaug[:, :, 96:97], 1.0), reads=[cb], writes=[vcb])
            S_.op("dve", lambda: nc.vector.memset(Vaug[:, :, :, :, 64:65], 1.0), writes=[Vb])
            S_.fence()

        pieces = _nsa_pieces(w_q, w_kv)
        if wswb is None:
            wswb = Buf()
            with ExitStack() as s0:
                rings = _nsa_prep_rings(nc, s0)
                for pi in range(len(pieces)):
                    _nsa_prep_piece(cx, pieces, pi, rings, wsw_d, wswb)
                S_.fence()

        for b in range(NB):
            with ExitStack() as sa:
                kcmpT = sa.enter_context(nc.sbuf_tensor(_u("n_kcmpT"), [128, 2, S], BF16))
                vcmpT = sa.enter_context(nc.sbuf_tensor(_u("n_vcmpT"), [128, 2, S], BF16))
                kcb, vcmb = Buf(), Buf()
                with ExitStack() as s1:
                    xtok = Ring(nc, s1, "n1_xtok", [128, D], F32, 2)
                    xbr = Ring(nc, s1, "n1_xb", [128, D], BF16, 2)
                    xTr = Ring(nc, s1, "n1_xT", [128, 8, 512], BF16, 2)
                    csr = Ring(nc, s1, "n1_cs", [128, 2, 512], F32, 1)
                    Wpr = Ring(nc, s1, "n1_Wp", [128, 8, 256], BF16, 3)
                    Wsr = Ring(nc, s1, "n1_Ws", [128, 8, 256], BF16, 3)
                    Wvr = Ring(nc, s1, "n1_Wv", [128, 8, 560], BF16, 1)
                    t1r = Ring(nc, s1, "n1_t1", [128, 512], F32, 2)
                    t2r = Ring(nc, s1, "n1_t2", [128, 512], F32, 2)
                    for blk in range(S // 512):
                        tb0 = blk * 512
                        cs, csb = csr.next()
                        S_.dma("sp", cs[:, 0, :], cos_d[:, tb0:tb0 + 512], reads=[tab_b], writes=[csb])
                        S_.dma("sp", cs[:, 1, :], sin_d[:, tb0:tb0 + 512], reads=[tab_b], writes=[csb], add=True)
                        xT, xTb = xTr.next()
                        for tt in range(4):
                            xt, xtb = xtok.next()
                            S_.dma("sp", xt[:], x2_d[b, tb0 + tt * 128:tb0 + (tt + 1) * 128, :], reads=[x2_b],
                                   writes=[xtb])
                            xb, xbb = xbr.next()
                            S_.op("pool", lambda xb=xb, xt=xt: nc.gpsimd.tensor_copy(out=xb[:], in_=xt[:]),
                                  reads=[xtb], writes=[xbb])
                            _transpose_to(cx, c, xb, xbb, 8,
                                          lambda k0, k1, tt=tt, xT=xT: xT[:, k0:k1, tt * 128:(tt + 1) * 128], xTb)
                        for pi, (kind, wsrc, c0, ncr, dup, rot, idx) in enumerate(pieces):
                            Wp, Wpb = Wpr.next()
                            S_.dma("sp", Wp[:], wsw_d[2 * pi], reads=[wswb], writes=[Wpb])
                            if rot:
                                Ws, Wsb = Wsr.next()
                                S_.dma("sp", Ws[:], wsw_d[2 * pi + 1], reads=[wswb], writes=[Wsb])
                            for ch in range(2):
                                pa, pab = cx.bank()
                                for kc in range(8):
                                    S_.op("pe", lambda kc=kc, ch=ch, Wp=Wp, pa=pa, xT=xT: nc.tensor.matmul(
                                        pa[:], lhsT=Wp[:, kc, ch * 128:(ch + 1) * 128], rhs=xT[:, kc, :],
                                        start=(kc == 0), stop=(kc == 7)), reads=[Wpb, xTb], writes=[pab],
                                        inc=(kc == 7))
                                if kind == "q":
                                    dst, dstb = QT2[:, idx * 2 + ch, tb0:tb0 + 512], QTb
                                elif kind == "kc":
                                    dst, dstb = kcmpT[:, ch, tb0:tb0 + 512], kcb
                                elif kind == "vc":
                                    dst, dstb = vcmpT[:, ch, tb0:tb0 + 512], vcmb
                                elif kind == "ks":
                                    dst, dstb = KS[:, idx * 2 + ch, tb0:tb0 + 512], KSb
                                else:
                                    dst, dstb = KW[:, idx * 2 + ch, tb0:tb0 + 512], KWb
                                if not rot:
                                    S_.op("act", lambda dst=dst, pa=pa: nc.scalar.copy(out=dst, in_=pa[:]),
                                          reads=[pab], writes=[dstb])
                                    continue
                                pb2, pbb2 = cx.bank()
                                for kc in range(8):
                                    S_.op("pe", lambda kc=kc, ch=ch, Ws=Ws, pb2=pb2, xT=xT: nc.tensor.matmul(
                                        pb2[:], lhsT=Ws[:, kc, ch * 128:(ch + 1) * 128], rhs=xT[:, kc, :],
                                        start=(kc == 0), stop=(kc == 7)), reads=[Wsb, xTb], writes=[pbb2],
                                        inc=(kc == 7))
                                t1, t1b = t1r.next()
                                t2, t2b = t2r.next()
                                S_.op("dve", lambda t1=t1, pa=pa, cs=cs: nc.vector.tensor_tensor(
                                    out=t1[:], in0=pa[:], in1=cs[:, 0, :], op=ALU.mult), reads=[pab, csb],
                                    writes=[t1b])
                                S_.op("dve", lambda t2=t2, pb2=pb2, cs=cs: nc.vector.tensor_tensor(
                                    out=t2[:], in0=pb2[:], in1=cs[:, 1, :], op=ALU.mult), reads=[pbb2, csb],
                                    writes=[t2b])
                                S_.op("pool", lambda dst=dst, t1=t1, t2=t2: nc.gpsimd.tensor_tensor(
                                    out=dst, in0=t1[:], in1=t2[:], op=ALU.add), reads=[t1b, t2b], writes=[dstb])
                        Wv, Wvb = Wvr.next()
                        _load_w3(cx, Wv[:, :, 0:256], w_kv, 0, 8, 768, 1024, Wvb)
                        _load_w3(cx, Wv[:, :, 256:512], w_kv, 0, 8, 1280, 1536, Wvb, add=True)
                        _load_w3(cx, Wv[:, :, 512:560], w_q, 0, 8, 1024, 1072, Wvb, add=True)
                        for tt in range(4):
                            ti = blk * 4 + tt
                            pv, pvb = cx.bank()
                            pg, pgb = cx.bank()
                            for kc in range(8):
                                S_.op("pe", lambda kc=kc, tt=tt, pv=pv, xT=xT, Wv=Wv: nc.tensor.matmul(
                                    pv[:], lhsT=xT[:, kc, tt * 128:(tt + 1) * 128], rhs=Wv[:, kc, 0:512],
                                    start=(kc == 0), stop=(kc == 7)), reads=[Wvb, xTb], writes=[pvb], inc=(kc == 7))
                            for kc in range(8):
                                S_.op("pe", lambda kc=kc, tt=tt, pg=pg, xT=xT, Wv=Wv: nc.tensor.matmul(
                                    pg[:, 0:48], lhsT=xT[:, kc, tt * 128:(tt + 1) * 128], rhs=Wv[:, kc, 512:560],
                                    start=(kc == 0), stop=(kc == 7)), reads=[Wvb, xTb], writes=[pgb], inc=(kc == 7))
                            S_.op("act", lambda ti=ti, pv=pv: nc.scalar.copy(
                                out=Vaug[:, ti, :, :, 0:64],
                                in_=pv[:].rearrange("p (a g d) -> p a g d", a=2, g=NG)), reads=[pvb], writes=[Vb])
                            for gg in range(NG):
                                S_.op("act", lambda ti=ti, pg=pg, gg=gg: nc.scalar.activation(
                                    out=G3[:, ti, gg].rearrange("p (a b) c -> p a b c", a=2),
                                    in_=pg[:, gg * 12:(gg + 1) * 12].rearrange("p (b a c) -> p a b c", b=2, a=2),
                                    func=AF.Sigmoid), reads=[pgb], writes=[G3b])
                S_.fence()
                with ExitStack() as s2:
                    W1 = [s2.enter_context(nc.sbuf_tensor(_u("n2_W1_%d" % i), [128, 32, 256], BF16)) for i in range(2)]
                    W2 = [s2.enter_context(nc.sbuf_tensor(_u("n2_W2_%d" % i), [128, 2, 128], BF16)) for i in range(2)]
                    pet = [s2.enter_context(nc.sbuf_tensor(_u("n2_pe_%d" % i), [32, 64], BF16)) for i in range(2)]
                    peT = [s2.enter_context(nc.sbuf_tensor(_u("n2_peT_%d" % i), [128, 32], BF16)) for i in range(2)]
                    bias = [s2.enter_context(nc.sbuf_tensor(_u("n2_bias_%d" % i), [128, 2], F32)) for i in range(2)]
                    wb2 = [Buf(), Buf()]
                    hx = Ring(nc, s2, "n2_hx", [128, 2, 127], F32, 2)
                    hu = Ring(nc, s2, "n2_hu", [128, 2, 127], F32, 2)
                    hs = Ring(nc, s2, "n2_hs", [128, 2, 127], F32, 2)
                    hb = Ring(nc, s2, "n2_hb", [128, 2, 127], BF16, 2)
                    for i, (pe_, w1_, w2_) in enumerate(((ck_pe, ck_w1, ck_w2), (cv_pe, cv_w1, cv_w2))):
                        src = w1_.rearrange("(l d) h -> d l h", d=64)
                        S_.dma("pool", W1[i][0:64], src, writes=[wb2[i]])
                        S_.dma("pool", W1[i][64:128], src, writes=[wb2[i]], add=True)
                        src2 = w2_.rearrange("(k p) d -> p k d", p=128)
                        S_.dma("pool", W2[i][:, :, 0:64], src2, writes=[wb2[i]], add=True)
                        S_.dma("pool", W2[i][:, :, 64:128], src2, writes=[wb2[i]], add=True)
                        S_.dma("pool", pet[i][:], pe_, writes=[wb2[i]], add=True)
                        bk, bb = cx.bank()
                        pb = bk.bitcast(BF16)
                        S_.op("pe", lambda i=i, pb=pb: nc.tensor.transpose(out=pb[0:64, 0:32], in_=pet[i][:],
                                                                           identity=ident[0:32, 0:32]),
                              reads=[wb2[i], idb], writes=[bb])
                        S_.op("act", lambda i=i, pb=pb: nc.scalar.copy(out=peT[i][0:64, :], in_=pb[0:64, 0:32]),
                              reads=[bb], writes=[wb2[i]])
                        bk, bb = cx.bank()
                        for hc in range(2):
                            for l in range(32):
                                S_.op("pe", lambda i=i, hc=hc, l=l, bk=bk: nc.tensor.matmul(
                                    bk[:, hc:hc + 1], lhsT=W1[i][0:64, l, hc * 128:(hc + 1) * 128],
                                    rhs=peT[i][0:64, l:l + 1], start=(l == 0), stop=(l == 31)),
                                    reads=[wb2[i]], writes=[bb], inc=(l == 31))
                        S_.op("act", lambda i=i, bk=bk: nc.scalar.copy(out=bias[i][:], in_=bk[:, 0:2]), reads=[bb],
                              writes=[wb2[i]])
                    for g in range(NG):
                        p0 = (g % 2) * 64
                        cc = g // 2
                        for i, (srcT, srcb) in enumerate(((kcmpT, kcb), (vcmpT, vcmb))):
                            bk, bb = cx.bank()
                            bv = bk[:, 0:256].rearrange("p (a n) -> p a n", a=2)
                            for hc in range(2):
                                for l in range(32):
                                    S_.op("pe", lambda i=i, hc=hc, l=l, bv=bv, srcT=srcT: nc.tensor.matmul(
                                        bv[:, hc, 0:127], lhsT=W1[i][p0:p0 + 64, l, hc * 128:(hc + 1) * 128],
                                        rhs=srcT[p0:p0 + 64, cc, l:l + 16 * 126 + 1:16], start=(l == 0),
                                        stop=(l == 31)), reads=[wb2[i], srcb], writes=[bb], inc=(l == 31))
                            x_, xb_ = hx.next()
                            u_, ub_ = hu.next()
                            s_, sb_ = hs.next()
                            h_, hb_ = hb.next()
                            for hc in range(2):
                                S_.op("act", lambda hc=hc, x_=x_, bv=bv, i=i: nc.scalar.activation(
                                    out=x_[:, hc, :], in_=bv[:, hc, 0:127], func=AF.Identity,
                                    bias=bias[i][:, hc:hc + 1], scale=1.0), reads=[bb, wb2[i]], writes=[xb_])
                            S_.op("dve", lambda x_=x_, u_=u_: nc.vector.tensor_tensor(out=u_[:], in0=x_[:], in1=x_[:],
                                                                                      op=ALU.mult),
                                  reads=[xb_], writes=[ub_])
                            S_.op("dve", lambda u_=u_: nc.vector.tensor_scalar(out=u_[:], in0=u_[:], scalar1=0.044715,
                                                                               scalar2=1.0, op0=ALU.mult,
                                                                               op1=ALU.add), reads=[ub_], writes=[ub_])
                            S_.op("dve", lambda x_=x_, u_=u_: nc.vector.tensor_tensor(out=u_[:], in0=u_[:], in1=x_[:],
                                                                                      op=ALU.mult),
                                  reads=[xb_, ub_], writes=[ub_])
                            S_.op("act", lambda u_=u_, s_=s_: nc.scalar.activation(
                                out=s_[:], in_=u_[:], func=AF.Sigmoid, scale=2.0 * math.sqrt(2.0 / math.pi)),
                                reads=[ub_], writes=[sb_])
                            S_.op("dve", lambda x_=x_, s_=s_, h_=h_: nc.vector.tensor_tensor(
                                out=h_[:], in0=x_[:], in1=s_[:], op=ALU.mult), reads=[xb_, sb_], writes=[hb_])
                            bk2, bb2 = cx.bank()
                            if i == 0:
                                for hc in range(2):
                                    S_.op("pe", lambda hc=hc, bk2=bk2, h_=h_: nc.tensor.matmul(
                                        bk2[:, 0:127], lhsT=W2[0][:, hc, :], rhs=h_[:, hc, :], start=(hc == 0),
                                        stop=(hc == 1)), reads=[wb2[0], hb_], writes=[bb2], inc=(hc == 1))
                                S_.op("act", lambda g=g, bk2=bk2: nc.scalar.copy(out=kcT[:, g, 0:127],
                                                                                 in_=bk2[:, 0:127]),
                                      reads=[bb2], writes=[kcTb])
                            else:
                                for hc in range(2):
                                    S_.op("pe", lambda hc=hc, bk2=bk2, h_=h_: nc.tensor.matmul(
                                        bk2[0:127, 0:64], lhsT=h_[:, hc, :], rhs=W2[1][:, hc, 0:64], start=(hc == 0),
                                        stop=(hc == 1)), reads=[wb2[1], hb_], writes=[bb2], inc=(hc == 1))
                                S_.op("act", lambda g=g, bk2=bk2: nc.scalar.copy(out=vcaug[0:127, g, 0:64],
                                                                                 in_=bk2[0:127, 0:64]),
                                      reads=[bb2], writes=[vcb])
                    if dbg_kc is not None:
                        S_.dma("sp", dbg_kc[b], kcT[:], reads=[kcTb], writes=[dbg_b], add=True)
                        S_.dma("sp", dbg_vc[b], vcaug[:], reads=[vcb], writes=[dbg_b], add=True)
                S_.fence()
            if os.environ.get("DBG_NOATT"):
                continue
            with ExitStack() as sb_:
                Wo = sb_.enter_context(nc.sbuf_tensor(_u("n_Wo"), [128, 8, D], BF16))
                lng = sb_.enter_context(nc.sbuf_tensor(_u("n_lng"), [128, D], F32))
                lnb = sb_.enter_context(nc.sbuf_tensor(_u("n_lnb"), [128, D], F32))
                Wob, pb_ = Buf(), Buf()
                S_.dma("sp", lng[:], ln_g.partition_broadcast(128), writes=[pb_])
                S_.dma("sp", lnb[:], ln_b.partition_broadcast(128), writes=[pb_], add=True)
                _load_w3(cx, Wo[:, :, :], w_out, 0, 8, 0, D, Wob)
                xtok = Ring(nc, sb_, "nb_xtok", [128, D], F32, 3)
                Or = Ring(nc, sb_, "nb_O", [128, D], F32, 4)
                Obr = Ring(nc, sb_, "nb_Ob", [128, D], BF16, 2)
                OTr = Ring(nc, sb_, "nb_OT", [128, 8, 128], BF16, 2)
                zr = Ring(nc, sb_, "nb_z", [128, D], F32, 2)
                ecr = Ring(nc, sb_, "nb_ec", [128, 4, 128], BF16, 3)
                basr = Ring(nc, sb_, "nb_bas", [128, 4 * 97], F32, 3)
                pTr = Ring(nc, sb_, "nb_pT", [128, 4, 128], BF16, 5)
                rzr = Ring(nc, sb_, "nb_rz", [128, 4], F32, 10)
                facr = Ring(nc, sb_, "nb_fac", [128, 4], F32, 10)
                impr = Ring(nc, sb_, "nb_imp", [128, 32], F32, 3)
                topr = Ring(nc, sb_, "nb_top", [128, 8], F32, 2)
                selr = Ring(nc, sb_, "nb_sel", [128, 32], F32, 2)
                selbr = Ring(nc, sb_, "nb_selb", [128, 96], BF16, 2)
                sT4r = Ring(nc, sb_, "nb_sT4", [96, 2, 128], BF16, 4)
                st6r = Ring(nc, sb_, "nb_st6", [128, 2, 6], F32, 2)
                mvr = Ring(nc, sb_, "nb_mv", [128, 2], F32, 2)
                rsr = Ring(nc, sb_, "nb_rs", [128, 1], F32, 2)
                old_rot = cx.rot
                cx.rot = [2, 3]
                cx.bi = -1
                cx.pi = -1
                it = 0
                pre = {}

                def xload(qt):
                    xt, xtb = xtok.next()
                    S_.dma("sp", xt[:], x2_d[b, qt * 128:(qt + 1) * 128, :], reads=[x2_b], writes=[xtb])
                    pre[qt] = (xt, xtb)

                accs = ((cx.banks[0], cx.bbuf[0]), (cx.banks[1], cx.bbuf[1]))

                def scores(bks, rq, g, Kt, Kb, ksl, nk, last):
                    for hb2 in range(2):
                        p0 = hb2 * 64
                        bk, bb = bks[hb2]
                        S_.op("pe", lambda hb2=hb2, p0=p0, bk=bk: nc.tensor.matmul(
                            bk[0:nk, 0:256], lhsT=Kt[p0:p0 + 64, g, ksl], rhs=rq[hb2],
                            start=True, stop=last), reads=[Kb, QTb], writes=[bb], inc=last)

                def bias_mm(bks, lhs_fn, rhs_fn, rd, last):
                    for hb2 in range(2):
                        bk, bb = bks[hb2]
                        S_.op("pe", lambda hb2=hb2, bk=bk: nc.tensor.matmul(
                            bk[:, 0:256], lhsT=lhs_fn(hb2), rhs=rhs_fn(hb2), start=False, stop=last),
                            reads=rd, writes=[bb], inc=last)

                def exp_to(dst, dstb, bks, nk):
                    dbl = bks[2]
                    S_.op("act", lambda: nc.scalar.activation(
                        out=dst[0:nk].rearrange("p (b s) i -> p b (s i)", b=2),
                        in_=dbl[0:nk, :].rearrange("p (b c) -> p b c", b=2)[:, :, 0:256], func=AF.Exp, scale=0.125),
                        reads=[bks[0][1], bks[1][1]], writes=[dstb])

                def combine(qt, g, Og, Ob, acc, accb, ncol, br, first):
                    av = acc[:, 0:4 * ncol].rearrange("p (s c) -> p s c", s=4)
                    rz, rzb = rzr.next()
                    fac, facb = facr.next()
                    S_.op("dve", lambda: nc.vector.tensor_scalar(out=rz[:], in0=av[:, :, ncol - 1],
                                                                 scalar1=1e-30, scalar2=None, op0=ALU.max),
                          reads=[accb], writes=[rzb])
                    S_.op("dve", lambda: nc.vector.reciprocal(out=rz[:], in_=rz[:]), reads=[rzb], writes=[rzb])
                    S_.op("dve", lambda: nc.vector.tensor_tensor(out=fac[:], in0=rz[:],
                                                                 in1=G3[:, qt, g, :, br], op=ALU.mult),
                          reads=[rzb, G3b], writes=[facb])
                    for s_i in range(4):
                        osl = Og[:, s_i // 2, s_i % 2, :]
                        if first:
                            S_.op("dve", lambda s_i=s_i, osl=osl: nc.vector.tensor_scalar(
                                out=osl, in0=av[:, s_i, 0:64], scalar1=fac[:, s_i:s_i + 1], scalar2=None,
                                op0=ALU.mult), reads=[accb, facb], writes=[Ob])
                        else:
                            S_.op("dve", lambda s_i=s_i, osl=osl: nc.vector.scalar_tensor_tensor(
                                out=osl, in0=av[:, s_i, 0:64], scalar=fac[:, s_i:s_i + 1], in1=osl,
                                op0=ALU.mult, op1=ALU.add), reads=[accb, facb, Ob], writes=[Ob])
                    return rz, rzb, av

                def stageA1(qt, g, O, Ob):
                    qsl = slice(qt * 128, (qt + 1) * 128)
                    rq = [QT2[0:64, 2 * g:2 * g + 2, qsl], QT2[64:128, 2 * g:2 * g + 2, qsl]]
                    Og = O[:, g * 256:(g + 1) * 256].rearrange("p (a b d) -> p b a d", a=2, b=2)
                    bks = cx.bank_pair()
                    scores(bks, rq, g, kcT, kcTb, slice(0, 127), 127, True)
                    ec, ecb = ecr.next()
                    exp_to(ec, ecb, bks, 127)
                    S_.op("dve", lambda: nc.vector.tensor_tensor(
                        out=ec[0:127], in0=ec[0:127],
                        in1=cmask[0:127, qt:qt + 1, :].broadcast_to([127, 4, 128]), op=ALU.mult),
                        reads=[ecb, cb], writes=[ecb])
                    return dict(qt=qt, g=g, rq=rq, Og=Og, Ob=Ob, ec=ec, ecb=ecb)

                def stageA2(stt):
                    qt, g, Og, Ob, ec, ecb = (stt[k_] for k_ in ("qt", "g", "Og", "Ob", "ec", "ecb"))
                    pr2 = cx.bank_pair()
                    ba, bab = pr2[0]
                    for s_i in range(4):
                        S_.op("pe", lambda s_i=s_i: nc.tensor.matmul(
                            ba[:, s_i * 97:(s_i + 1) * 97], lhsT=ec[0:127, s_i, :], rhs=vcaug[0:127, g, :],
                            start=True, stop=True), reads=[ecb, vcb], writes=[bab], inc=(s_i == 3))
                    bas, basb = basr.next()
                    S_.op("dve", lambda: nc.vector.tensor_copy(out=bas[:], in_=ba[:, 0:4 * 97]), reads=[bab],
                          writes=[basb])
                    ba, bab = bas, basb
                    rz, rzb, av = combine(qt, g, Og, Ob, ba, bab, 97, 0, True)
                    imp, impb = impr.next()
                    for s_i in range(4):
                        if s_i == 0:
                            S_.op("dve", lambda: nc.vector.tensor_scalar(
                                out=imp[:], in0=av[:, 0, 64:96], scalar1=rz[:, 0:1], scalar2=None, op0=ALU.mult),
                                reads=[bab, rzb], writes=[impb])
                        else:
                            S_.op("dve", lambda s_i=s_i: nc.vector.scalar_tensor_tensor(
                                out=imp[:], in0=av[:, s_i, 64:96], scalar=rz[:, s_i:s_i + 1], in1=imp[:],
                                op0=ALU.mult, op1=ALU.add), reads=[bab, rzb, impb], writes=[impb])
                    S_.op("dve", lambda: nc.vector.tensor_tensor(out=imp[:], in0=imp[:], in1=M1[:, qt, :],
                                                                 op=ALU.mult), reads=[impb, cb], writes=[impb])
                    S_.op("dve", lambda: nc.vector.tensor_tensor(out=imp[:], in0=imp[:], in1=M2[:, qt, :],
                                                                 op=ALU.add), reads=[impb, cb], writes=[impb])
                    top, topb = topr.next()
                    S_.op("dve", lambda: nc.vector.max(out=top[:], in_=imp[:]), reads=[impb], writes=[topb])
                    sel, selb_ = selr.next()
                    S_.op("dve", lambda: nc.vector.scalar_tensor_tensor(
                        out=sel[:], in0=imp[:], scalar=top[:, 7:8], in1=VAL[:, qt, :], op0=ALU.is_ge,
                        op1=ALU.mult), reads=[impb, topb, cb], writes=[selb_])
                    selbf, selbfb = selbr.next()
                    S_.op("pool", lambda: nc.gpsimd.memset(selbf[:, 32:64], 0.0), writes=[selbfb])
                    for c0 in (0, 64):
                        S_.op("dve", lambda c0=c0: nc.vector.tensor_scalar(
                            out=selbf[:, c0:c0 + 32], in0=sel[:], scalar1=-1.0, scalar2=-NEG, op0=ALU.add,
                            op1=ALU.mult), reads=[selb_], writes=[selbfb])
                    stt["selbf"], stt["selbfb"] = selbf, selbfb

                def stageA3(stt):
                    selbf, selbfb = stt["selbf"], stt["selbfb"]
                    pr2 = cx.bank_pair()
                    bt, btb = pr2[0]
                    ptb = bt.bitcast(BF16)
                    S_.op("pe", lambda: nc.tensor.transpose(out=ptb[0:96, 0:128], in_=selbf[:], identity=ident[:]),
                          reads=[selbfb, idb], writes=[btb])
                    sT4, sT4b = sT4r.next()
                    S_.op("act", lambda: nc.scalar.copy(
                        out=sT4[:], in_=ptb[0:96, 0:128].unsqueeze(1).broadcast_to([96, 2, 128])),
                        reads=[btb], writes=[sT4b])
                    stt["sT4"], stt["sT4b"] = sT4, sT4b

                def stageB(stt, hooks):
                    qt, g, rq, Og, Ob, sT4, sT4b = (stt[k] for k in ("qt", "g", "rq", "Og", "Ob", "sT4", "sT4b"))
                    kts_w = [kt for kt in range(qt - 4, qt + 1) if kt >= 0]
                    steps = [("w", kt) for kt in kts_w] + [("s", kt) for kt in range(qt + 1)]

                    def emit_scores(step):
                        kind, kt = step
                        ksl = slice(kt * 128, (kt + 1) * 128)
                        bks = cx.bank_pair()
                        if kind == "w":
                            edge = (kt == qt) or (kt == qt - 4)
                            scores(bks, rq, g, KW, KWb, ksl, 128, not edge)
                            if edge:
                                tri = tri4 if kt == qt else tri4b
                                bias_mm(bks, lambda hb2: ident[:],
                                        lambda hb2, tri=tri: tri[:].rearrange("p s i -> p (s i)"), [cb, idb], True)
                        else:
                            scores(bks, rq, g, KS, KSb, ksl, 128, False)
                            bias_mm(bks, lambda hb2, ksl=ksl: Emat[hb2 * 64:hb2 * 64 + 32, ksl],
                                    lambda hb2: sT4[hb2 * 64:hb2 * 64 + 32].rearrange("p s i -> p (s i)"),
                                    [cb, sT4b], kt != qt)
                            if kt == qt:
                                bias_mm(bks, lambda hb2: ident[:], lambda hb2: tri4[:].rearrange("p s i -> p (s i)"),
                                        [cb, idb], True)
                        return bks

                    def emit_rest(step, bks):
                        kind, kt = step
                        acc, accb = accs[1] if kind == "w" else accs[0]
                        vi = 1 if kind == "w" else 0
                        first_kt = kts_w[0] if kind == "w" else 0
                        pT, pTb = pTr.next()
                        exp_to(pT, pTb, bks, 128)
                        for s_i in range(4):
                            S_.op("pe", lambda s_i=s_i, pT=pT, kt=kt, acc=acc: nc.tensor.matmul(
                                acc[:, s_i * 65:(s_i + 1) * 65], lhsT=pT[:, s_i, :], rhs=Vaug[:, kt, vi, g, :],
                                start=(kt == first_kt and s_i == 0), stop=(kt == qt), skip_group_check=True),
                                reads=[pTb, Vb], writes=[accb], inc=(s_i == 3))
                        if kt == qt:
                            combine(qt, g, Og, Ob, acc, accb, 65, 2 if kind == "w" else 1, False)

                    LOOK = 1
                    n = len(steps)
                    if len(hooks) == 4:
                        cand = [0, 1, max(2, n // 3), max(3, (2 * n) // 3)]
                    else:
                        cand = [0, max(1, n // 3), max(2, (2 * n) // 3)]
                    for ci in range(1, len(cand)):
                        cand[ci] = max(cand[ci], cand[ci - 1] + 1)
                    hookpos = {p: hi for hi, p in enumerate(cand)}
                    done = 0
                    pend = [emit_scores(steps[i]) for i in range(min(LOOK, n))]
                    for i, step in enumerate(steps):
                        if i + LOOK < n:
                            pend.append(emit_scores(steps[i + LOOK]))
                        emit_rest(step, pend.pop(0))
                        if i in hookpos and hookpos[i] == done and done < len(hooks):
                            hooks[done]()
                            done += 1
                    while done < len(hooks):
                        hooks[done]()
                        done += 1

                def tail(qt, O, Ob, xt, xtb):
                    cx.pi = (cx.pi + 1) % len(cx.prot)
                    cx.rot = [2 * cx.prot[cx.pi], 2 * cx.prot[cx.pi] + 1]
                    cx.bi = -1
                    Obf, Obfb = Obr.next()
                    S_.op("pool", lambda Obf=Obf, O=O: nc.gpsimd.tensor_copy(out=Obf[:], in_=O[:]), reads=[Ob],
                          writes=[Obfb])
                    OT, OTb = OTr.next()
                    _transpose_to(cx, c, Obf, Obfb, 8, lambda k0, k1, OT=OT: OT[:, k0:k1, :].rearrange("p a b -> p (a b)"),
                                  OTb)
                    z, zb = zr.next()
                    for hh in range(2):
                        bk, bb = cx.bank()
                        for kc in range(8):
                            S_.op("pe", lambda kc=kc, hh=hh, bk=bk, OT=OT: nc.tensor.matmul(
                                bk[:], lhsT=OT[:, kc, :], rhs=Wo[:, kc, hh * 512:(hh + 1) * 512], start=(kc == 0),
                                stop=(kc == 7)), reads=[Wob, OTb], writes=[bb], inc=(kc == 7))
                        S_.op("dve", lambda hh=hh, bk=bk, z=z, xt=xt: nc.vector.scalar_tensor_tensor(
                            out=z[:, hh * 512:(hh + 1) * 512], in0=xt[:, hh * 512:(hh + 1) * 512], scalar=DN_ALPHA,
                            in1=bk[:], op0=ALU.mult, op1=ALU.add), reads=[bb, xtb], writes=[zb])
                    st6, sb6 = st6r.next()
                    mv, _ = mvr.next()
                    rs, _ = rsr.next()
                    _layernorm_store(cx, c, z, zb, lng, lnb, pb_, x3_d[b, qt * 128:(qt + 1) * 128, :], x3_b, st6, mv,
                                     rs, sb6)

                items = [(qt, g) for qt in range(NT) for g in range(NG)]
                Otiles = {}

                def get_O(qt):
                    if qt not in Otiles:
                        Otiles[qt] = Or.next()
                    return Otiles[qt]

                xload(0)
                xload(1)
                ALOOK = 2

                def fullA(q2, g2):
                    stt = stageA1(q2, g2, *get_O(q2))
                    stageA2(stt)
                    stageA3(stt)
                    return stt

                stq = [fullA(q2, g2) for (q2, g2) in items[:ALOOK]]
                pending_tail = [None]
                for k, (qt, g) in enumerate(items):
                    hooks = []
                    if k + ALOOK < len(items):
                        q2, g2 = items[k + ALOOK]
                        box = {}

                        def h1(q2=q2, g2=g2, box=box):
                            box["s"] = stageA1(q2, g2, *get_O(q2))

                        def h2(box=box):
                            stageA2(box["s"])

                        def h3(box=box):
                            stageA3(box["s"])
                            stq.append(box["s"])

                        hooks = [h1, h2, h3]
                    if pending_tail[0] is not None:
                        if len(hooks) == 3:
                            hooks = [hooks[0], pending_tail[0], hooks[1], hooks[2]]
                        else:
                            hooks = hooks + [pending_tail[0]]
                        pending_tail[0] = None
                    stageB(stq.pop(0), hooks)
                    if g == NG - 1:
                        def tl(qt=qt):
                            if qt + 2 < NT:
                                xload(qt + 2)
                            xt, xtb = pre.pop(qt)
                            O, Ob = Otiles.pop(qt)
                            tail(qt, O, Ob, xt, xtb)
                        pending_tail[0] = tl
                if pending_tail[0] is not None:
                    pending_tail[0]()
                    pending_tail[0] = None
                cx.rot = old_rot
                cx.bi = -1
            S_.fence()


CAP = int(os.environ.get('DBG_CAP', 1152))
NSLOT = NE * CAP


def _dma_ind(S_, nc, breg, out, out_off, in_, in_off, reads, writes, add=True):
    i = S_.dnext
    S_.dnext = (S_.dnext + 1) % S_.NDMA
    if S_.dcnt[i] > 0:
        S_._wait("pool", (i, S_.dcnt[i]))
    S_._deps("pool", reads, writes, add=add)
    S_.dcnt[i] += 16
    ev = (i, S_.dcnt[i])
    nc.gpsimd.indirect_dma_start(out=out, out_offset=out_off, in_=in_, in_offset=in_off, bounds_check=breg,
                                 oob_is_err=False).then_inc(S_.dsem[i], 16)
    S_.ninstr += 1
    for b in writes:
        if add:
            b.w.append(ev)
        else:
            b.w = [ev]
        b.r = []
    for b in reads:
        b.r.append(ev)


def phase_moe_sparse(cx, c, x_d, x_b, w_ins, w_outs, router, ln_g, ln_b, dst_d, dst_b, xe_d, ye_d, flag_d,
                     dbg_sl=None):
    nc, S_ = cx.nc, cx.S
    F = DFFE
    NTT = NB * S // 128
    xe_b, ye_b, flag_b = Buf(), Buf(), Buf()
    with ExitStack() as st:
        G12 = st.enter_context(nc.sbuf_tensor(_u("ms_G12"), [128, NTT, 2], F32))
        SL = st.enter_context(nc.sbuf_tensor(_u("ms_SL"), [128, NTT, 2], I32))
        SLt = [[st.enter_context(nc.sbuf_tensor(_u("ms_SLt"), [128, 1], I32)) for _ in range(2)] for _ in range(NTT)]
        G12b, SLb = Buf(), Buf()
        with ExitStack() as s1:
            wr = s1.enter_context(nc.sbuf_tensor(_u("ms_wr"), [128, 8, NE], F32))
            ident32 = s1.enter_context(nc.sbuf_tensor(_u("ms_id32"), [128, 128], F32))
            Ltri = s1.enter_context(nc.sbuf_tensor(_u("ms_Ltri"), [128, 128], BF16))
            eC = s1.enter_context(nc.sbuf_tensor(_u("ms_eC"), [128, NE], F32))
            cum = s1.enter_context(nc.sbuf_tensor(_u("ms_cum"), [128, NE], F32))
            ovfacc = s1.enter_context(nc.sbuf_tensor(_u("ms_ovf"), [128, 1], F32))
            ovfb16 = s1.enter_context(nc.sbuf_tensor(_u("ms_ovfb"), [128, 1], BF16))
            flag_sb = s1.enter_context(nc.sbuf_tensor(_u("ms_flag"), [1, 1], I32))
            wrb, idb32, cb, cumb, ovfb = Buf(), Buf(), Buf(), Buf(), Buf()
            S_.dma("sp", wr[:], router.rearrange("(k p) e -> p k e", p=128), writes=[wrb])
            S_.op("pool", lambda: nc.gpsimd.tensor_copy(out=ident32[:], in_=c["ident"][:]), reads=[c["ident_b"]],
                  writes=[idb32])
            S_.op("pool", lambda: nc.gpsimd.affine_select(out=Ltri[:], in_=c["ones"][:], pattern=[[1, 128]],
                                                          compare_op=ALU.is_ge, fill=cx.reg(0.0), base=-1,
                                                          channel_multiplier=-1), reads=[c["ones_b"]], writes=[cb])
            S_.op("pool", lambda: nc.gpsimd.iota(eC[:], pattern=[[CAP, NE]], base=0, channel_multiplier=0,
                                                 allow_small_or_imprecise_dtypes=True), writes=[cb])
            S_.op("pool", lambda: nc.gpsimd.memset(cum[:], 0.0), writes=[cumb])
            S_.op("pool", lambda: nc.gpsimd.memset(ovfacc[:], 0.0), writes=[ovfb])
            xtok = Ring(nc, s1, "ms_xtok", [128, D], F32, 3)
            xbr = Ring(nc, s1, "ms_xb", [128, D], BF16, 3)
            xT32r = Ring(nc, s1, "ms_xT32", [128, 8, 128], F32, 2)
            lgr = Ring(nc, s1, "ms_lg", [128, NE], F32, 2)
            topr = Ring(nc, s1, "ms_top", [128, 8], F32, 2)
            mkr = Ring(nc, s1, "ms_mk", [128, NE], F32, 2)
            mkbr = Ring(nc, s1, "ms_mkb", [128, NE], BF16, 2)
            cumbr = Ring(nc, s1, "ms_cumb", [128, NE], BF16, 2)
            p2r = Ring(nc, s1, "ms_p2", [128, NE], F32, 2)
            ger = Ring(nc, s1, "ms_ge", [128, NE], F32, 2)
            tmr = Ring(nc, s1, "ms_tm", [128, NE], F32, 4)
            slfr = Ring(nc, s1, "ms_slf", [128, 2], F32, 2)
            dr = Ring(nc, s1, "ms_d", [128, 1], F32, 2)
            pre = {}

            def xload(j):
                b, t0 = (j * 128) // S, (j * 128) % S
                xt, xtb = xtok.next()
                S_.dma("sp", xt[:], x_d[b, t0:t0 + 128, :], reads=[x_b], writes=[xtb])
                pre[j] = (xt, xtb)

            xload(0)
            for j in range(NTT):
                if j + 1 < NTT:
                    xload(j + 1)
                xt, xtb = pre.pop(j)
                xb, xbb = xbr.next()
                S_.op("act", lambda xb=xb, xt=xt: nc.scalar.copy(out=xb[:], in_=xt[:]), reads=[xtb], writes=[xbb])
                xT32, xT32b = xT32r.next()
                for k0 in (0, 4):
                    bk, bb = cx.bank()
                    for k in range(k0, k0 + 4):
                        S_.op("pe", lambda k=k, k0=k0, bk=bk, xt=xt: nc.tensor.transpose(
                            out=bk[:, (k - k0) * 128:(k - k0 + 1) * 128], in_=xt[:, k * 128:(k + 1) * 128],
                            identity=ident32[:]), reads=[xtb, idb32], writes=[bb], inc=(k == k0 + 3))
                    S_.op("act", lambda k0=k0, bk=bk, xT32=xT32: nc.scalar.copy(
                        out=xT32[:, k0:k0 + 4, :], in_=bk[:].rearrange("p (a b) -> p a b", a=4)),
                        reads=[bb], writes=[xT32b])
                bk, bb = cx.bank()
                for kc in range(8):
                    S_.op("pe", lambda kc=kc, bk=bk, xT32=xT32: nc.tensor.matmul(
                        bk[:, 0:NE], lhsT=xT32[:, kc, :], rhs=wr[:, kc, :], start=(kc == 0), stop=(kc == 7)),
                        reads=[xT32b, wrb], writes=[bb], inc=(kc == 7))
                lg_, lgb = lgr.next()
                top, topb = topr.next()
                S_.op("dve", lambda bk=bk, lg_=lg_: nc.vector.tensor_copy(out=lg_[:], in_=bk[:, 0:NE]), reads=[bb],
                      writes=[lgb])
                S_.op("dve", lambda lg_=lg_, top=top: nc.vector.max(out=top[:], in_=lg_[:]), reads=[lgb],
                      writes=[topb])
                d_, db_ = dr.next()
                S_.op("dve", lambda top=top, d_=d_: nc.vector.tensor_tensor(out=d_[:], in0=top[:, 0:1],
                                                                           in1=top[:, 1:2], op=ALU.subtract),
                      reads=[topb], writes=[db_])
                S_.op("act", lambda d_=d_, j=j: nc.scalar.activation(out=G12[:, j, 0:1], in_=d_[:], func=AF.Sigmoid),
                      reads=[db_], writes=[G12b])
                S_.op("act", lambda d_=d_, j=j: nc.scalar.activation(out=G12[:, j, 1:2], in_=d_[:], func=AF.Sigmoid,
                                                                    scale=-1.0), reads=[db_], writes=[G12b])
                mk, mkb = mkr.next()
                S_.op("dve", lambda mk=mk, lg_=lg_, top=top: nc.vector.tensor_scalar(
                    out=mk[:], in0=lg_[:], scalar1=top[:, 1:2], scalar2=None, op0=ALU.is_ge), reads=[lgb, topb],
                    writes=[mkb])
                mkb16, mkb16b = mkbr.next()
                S_.op("dve", lambda mk=mk, mkb16=mkb16: nc.vector.tensor_copy(out=mkb16[:], in_=mk[:]), reads=[mkb],
                      writes=[mkb16b])
                cumb16, cumb16b = cumbr.next()
                S_.op("dve", lambda cumb16=cumb16: nc.vector.tensor_copy(out=cumb16[:], in_=cum[:]), reads=[cumb],
                      writes=[cumb16b])
                bp, bpb = cx.bank()
                S_.op("pe", lambda bp=bp, mkb16=mkb16: nc.tensor.matmul(bp[:, 0:NE], lhsT=Ltri[:], rhs=mkb16[:],
                                                                         start=True, stop=False),
                      reads=[cb, mkb16b], writes=[bpb], inc=False)
                S_.op("pe", lambda bp=bp, cumb16=cumb16: nc.tensor.matmul(bp[:, 0:NE], lhsT=c["ones"][:],
                                                                           rhs=cumb16[:], start=False, stop=True),
                      reads=[c["ones_b"], cumb16b], writes=[bpb])
                S_.op("dve", lambda mk=mk: nc.vector.tensor_tensor(out=cum[:], in0=cum[:], in1=mk[:], op=ALU.add),
                      reads=[mkb, cumb, cumb16b], writes=[cumb])
                ge, geb = ger.next()
                p2, p2b = p2r.next()
                S_.op("dve", lambda ge=ge, bp=bp: nc.vector.tensor_scalar(out=ge[:], in0=bp[:, 0:NE],
                                                                          scalar1=float(CAP), scalar2=None,
                                                                          op0=ALU.is_ge), reads=[bpb], writes=[geb])
                S_.op("dve", lambda p2=p2, bp=bp: nc.vector.tensor_tensor(out=p2[:], in0=bp[:, 0:NE], in1=eC[:],
                                                                          op=ALU.add), reads=[bpb, cb], writes=[p2b])
                S_.op("dve", lambda p2=p2, ge=ge: nc.vector.scalar_tensor_tensor(
                    out=p2[:], in0=ge[:], scalar=1.0e6, in1=p2[:], op0=ALU.mult, op1=ALU.add), reads=[geb, p2b],
                    writes=[p2b])
                tm, tmb = tmr.next()
                S_.op("dve", lambda tm=tm, ge=ge, mk=mk: nc.vector.tensor_tensor(out=tm[:], in0=ge[:], in1=mk[:],
                                                                                 op=ALU.mult), reads=[geb, mkb],
                      writes=[tmb])
                tm2, tm2b = tmr.next()
                S_.op("dve", lambda tm=tm, tm2=tm2: nc.vector.tensor_reduce(out=tm2[:, 0:1], in_=tm[:],
                                                                           axis=mybir.AxisListType.X, op=ALU.add),
                      reads=[tmb], writes=[tm2b])
                S_.op("dve", lambda tm2=tm2: nc.vector.tensor_tensor(out=ovfacc[:], in0=ovfacc[:], in1=tm2[:, 0:1],
                                                                     op=ALU.add), reads=[tm2b, ovfb], writes=[ovfb])
                slf, slfb = slfr.next()
                for ci in range(2):
                    tm, tmb = tmr.next()
                    S_.op("dve", lambda tm=tm, lg_=lg_, top=top, p2=p2, ci=ci: nc.vector.scalar_tensor_tensor(
                        out=tm[:], in0=lg_[:], scalar=top[:, ci:ci + 1], in1=p2[:], op0=ALU.is_equal, op1=ALU.mult),
                        reads=[lgb, topb, p2b], writes=[tmb])
                    S_.op("dve", lambda tm=tm, slf=slf, ci=ci: nc.vector.tensor_reduce(
                        out=slf[:, ci:ci + 1], in_=tm[:], axis=mybir.AxisListType.X, op=ALU.add), reads=[tmb],
                        writes=[slfb])
                S_.op("dve", lambda slf=slf, j=j: nc.vector.tensor_copy(out=SL[:, j, :], in_=slf[:]), reads=[slfb],
                      writes=[SLb])
                for ci in range(2):
                    S_.op("dve", lambda slf=slf, j=j, ci=ci: nc.vector.tensor_copy(out=SLt[j][ci][:],
                                                                                  in_=slf[:, ci:ci + 1]),
                          reads=[slfb], writes=[SLb])
                for ci in range(2):
                    _dma_ind(S_, nc, cx.reg(NSLOT - 1), xe_d[:, :], bass.IndirectOffsetOnAxis(ap=SLt[j][ci][:, :], axis=0), xb[:, :],
                             None, reads=[xbb, SLb], writes=[xe_b])
            S_.op("dve", lambda: nc.vector.tensor_copy(out=ovfb16[:], in_=ovfacc[:]), reads=[ovfb], writes=[ovfb])
            bk, bb = cx.bank()
            S_.op("pe", lambda: nc.tensor.matmul(bk[0:1, 0:1], lhsT=ovfb16[:, 0:1], rhs=c["ones"][:, 0:1], start=True,
                                                 stop=True), reads=[ovfb, c["ones_b"]], writes=[bb])
            S_.op("dve", lambda: nc.vector.tensor_copy(out=flag_sb[:], in_=bk[0:1, 0:1]), reads=[bb], writes=[ovfb])
            S_.dma("sp", flag_d[:, :], flag_sb[:], reads=[ovfb], writes=[flag_b])
            if dbg_sl is not None:
                S_.dma("sp", dbg_sl[:, :, :], SL[:], reads=[SLb], writes=[flag_b], add=True)
            S_.fence()
        with ExitStack() as s2:
            T = CAP
            ntt = T // 128
            tblocks = []
            t0 = 0
            while t0 < T:
                tn = min(512, T - t0)
                tblocks.append((t0, tn))
                t0 += tn
            groups = [(f0, 512) for f0 in range(0, F, 512)]
            acc = s2.enter_context(nc.sbuf_tensor(_u("me_acc"), [128, ntt, D], F32))
            accb = [Buf() for _ in range(ntt)]
            xTs = [s2.enter_context(nc.sbuf_tensor(_u("me_xT"), [128, 8, T], BF16)) for _ in range(2)]
            xTbs = [[Buf() for _ in range(ntt)] for _ in range(2)]
            xer = Ring(nc, s2, "me_xe", [128, D], BF16, 3)
            Wir = Ring(nc, s2, "me_Wi", [128, 8, 2, 512], BF16, 2)
            Wor = Ring(nc, s2, "me_Wo", [128, 4, D], BF16, 2)
            hTr = Ring(nc, s2, "me_hT", [128, 4, T], BF16, 2)
            sar = Ring(nc, s2, "me_sa", [128, 512], F32, 3)
            work = [(e, gi) for e in range(NE) for gi in range(len(groups))]
            wpre = {}

            def wload(k):
                e, gi = work[k]
                f0, g = groups[gi]
                Wi, Wib = Wir.next()
                Wo, Wob = Wor.next()
                _load_w3(cx, Wi[:, :, 0, 0:g], w_ins[e], 0, 8, f0, f0 + g, Wib)
                _load_w3(cx, Wi[:, :, 1, 0:g], w_ins[e], 0, 8, F + f0, F + f0 + g, Wib, add=True)
                _load_w3(cx, Wo[:, 0:g // 128, :], w_outs[e], f0, g // 128, 0, D, Wob)
                wpre[k] = (Wi, Wib, Wo, Wob)

            def load_xT(e):
                xT, xTb = xTs[e % 2], xTbs[e % 2]
                for tt in range(ntt):
                    xe, xeb = xer.next()
                    S_.dma("sp", xe[:], xe_d[e * CAP + tt * 128:e * CAP + (tt + 1) * 128, :], reads=[xe_b],
                           writes=[xeb])
                    _transpose_to(cx, c, xe, xeb, 8,
                                  lambda k0, k1, tt=tt, xT=xT: xT[:, k0:k1, tt * 128:(tt + 1) * 128], xTb[tt])

            wk = 0
            wload(0)
            load_xT(0)
            for e in range(NE):
                xT, xTb = xTs[e % 2], xTbs[e % 2]
                for gi, (f0, g) in enumerate(groups):
                    nfc = g // 128
                    if gi == 1 and e + 1 < NE:
                        load_xT(e + 1)
                    if wk + 1 < len(work):
                        wload(wk + 1)
                    Wi, Wib, Wo, Wob = wpre.pop(wk)
                    wk += 1
                    hT, hTb = hTr.next()
                    for (tb0, tn) in tblocks:
                        tsl = slice(tb0, tb0 + tn)
                        xdeps = xTb[tb0 // 128:(tb0 + tn) // 128]
                        for fc in range(nfc):
                            pa, pab = cx.bank()
                            pbk, pbb = cx.bank()
                            for ab, (pp, ppb) in enumerate(((pa, pab), (pbk, pbb))):
                                for kc in range(8):
                                    S_.op("pe", lambda kc=kc, ab=ab, pp=pp, fc=fc, Wi=Wi: nc.tensor.matmul(
                                        pp[:, 0:tn], lhsT=Wi[:, kc, ab, fc * 128:(fc + 1) * 128], rhs=xT[:, kc, tsl],
                                        start=(kc == 0), stop=(kc == 7)), reads=[Wib] + xdeps, writes=[ppb],
                                        inc=(kc == 7))
                            sa, sab = sar.next()
                            S_.op("act", lambda sa=sa, pa=pa: nc.scalar.activation(out=sa[:, 0:tn], in_=pa[:, 0:tn],
                                                                                   func=AF.Silu),
                                  reads=[pab], writes=[sab])
                            S_.op("dve", lambda sa=sa, pbk=pbk, hT=hT, fc=fc: nc.vector.tensor_tensor(
                                out=hT[:, fc, tsl], in0=sa[:, 0:tn], in1=pbk[:, 0:tn], op=ALU.mult), reads=[sab, pbb],
                                writes=[hTb])
                    for tt in range(ntt):
                        for hh in range(2):
                            po, pob = cx.bank()
                            for fc in range(nfc):
                                S_.op("pe", lambda fc=fc, po=po, hT=hT, Wo=Wo, tt=tt, hh=hh: nc.tensor.matmul(
                                    po[:], lhsT=hT[:, fc, tt * 128:(tt + 1) * 128],
                                    rhs=Wo[:, fc, hh * 512:(hh + 1) * 512], start=(fc == 0), stop=(fc == nfc - 1)),
                                    reads=[hTb, Wob], writes=[pob], inc=(fc == nfc - 1))
                            asl = acc[:, tt, hh * 512:(hh + 1) * 512]
                            if gi == 0:
                                S_.op("act", lambda po=po, asl=asl: nc.scalar.copy(out=asl, in_=po[:]),
                                      reads=[pob], writes=[accb[tt]])
                            else:
                                S_.op("dve", lambda po=po, asl=asl: nc.vector.tensor_tensor(
                                    out=asl, in0=po[:], in1=asl, op=ALU.add), reads=[pob, accb[tt]],
                                    writes=[accb[tt]])
                for tt in range(ntt):
                    S_.dma("sp", ye_d[e * CAP + tt * 128:e * CAP + (tt + 1) * 128, :], acc[:, tt, :],
                           reads=[accb[tt]], writes=[ye_b], add=True)
            S_.fence()
        with ExitStack() as s3:
            lng = s3.enter_context(nc.sbuf_tensor(_u("mc_lng"), [128, D], F32))
            lnb = s3.enter_context(nc.sbuf_tensor(_u("mc_lnb"), [128, D], F32))
            pb_ = Buf()
            S_.dma("sp", lng[:], ln_g.partition_broadcast(128), writes=[pb_])
            S_.dma("sp", lnb[:], ln_b.partition_broadcast(128), writes=[pb_], add=True)
            xtok = Ring(nc, s3, "mc_xtok", [128, D], F32, 3)
            y1r = Ring(nc, s3, "mc_y1", [128, D], F32, 3)
            y2r = Ring(nc, s3, "mc_y2", [128, D], F32, 3)
            st6r = Ring(nc, s3, "mc_st6", [128, 2, 6], F32, 2)
            mvr = Ring(nc, s3, "mc_mv", [128, 2], F32, 2)
            rsr = Ring(nc, s3, "mc_rs", [128, 1], F32, 2)
            pre = {}

            def cload(j):
                b, t0 = (j * 128) // S, (j * 128) % S
                xt, xtb = xtok.next()
                S_.dma("sp", xt[:], x_d[b, t0:t0 + 128, :], reads=[x_b], writes=[xtb])
                y1, y1b = y1r.next()
                y2, y2b = y2r.next()
                _dma_ind(S_, nc, cx.reg(NSLOT - 1), y1[:, :], None, ye_d[:, :], bass.IndirectOffsetOnAxis(ap=SLt[j][0][:, :], axis=0),
                         reads=[ye_b, SLb], writes=[y1b], add=False)
                _dma_ind(S_, nc, cx.reg(NSLOT - 1), y2[:, :], None, ye_d[:, :], bass.IndirectOffsetOnAxis(ap=SLt[j][1][:, :], axis=0),
                         reads=[ye_b, SLb], writes=[y2b], add=False)
                pre[j] = (xt, xtb, y1, y1b, y2, y2b)

            cload(0)
            for j in range(NTT):
                if j + 1 < NTT:
                    cload(j + 1)
                xt, xtb, y1, y1b, y2, y2b = pre.pop(j)
                b, t0 = (j * 128) // S, (j * 128) % S
                S_.op("act", lambda xt=xt: nc.scalar.mul(out=xt[:], in_=xt[:], mul=DN_ALPHA), reads=[xtb],
                      writes=[xtb])
                S_.op("dve", lambda xt=xt, y1=y1, j=j: nc.vector.scalar_tensor_tensor(
                    out=xt[:], in0=y1[:], scalar=G12[:, j, 0:1], in1=xt[:], op0=ALU.mult, op1=ALU.add),
                    reads=[y1b, G12b, xtb], writes=[xtb])
                S_.op("dve", lambda xt=xt, y2=y2, j=j: nc.vector.scalar_tensor_tensor(
                    out=xt[:], in0=y2[:], scalar=G12[:, j, 1:2], in1=xt[:], op0=ALU.mult, op1=ALU.add),
                    reads=[y2b, G12b, xtb], writes=[xtb])
                st6, sb = st6r.next()
                mv, _ = mvr.next()
                rs, _ = rsr.next()
                _layernorm_store(cx, c, xt, xtb, lng, lnb, pb_, dst_d[b, t0:t0 + 128, :], dst_b, st6, mv, rs, sb)
    return flag_b


LAST_INPUTS = []


def build_program(phases=("ret1", "ret2", "ffn", "moe"), dbg=False):
    nc = bass.Bass("TRN2", target_bir_lowering=False)
    dt = {}
    del LAST_INPUTS[:]

    def inp(name, shape):
        dt[name] = nc.dram_tensor(name, list(shape), F32, kind="ExternalInput").ap()
        LAST_INPUTS.append(name)
        return dt[name]

    x = inp("x", [NB, S, D])
    ret_w_in = inp("ret_w_in", [1, D, 6144])
    ret_gn_g = inp("ret_gn_g", [1, 2048])
    ret_w_out = inp("ret_w_out", [1, 2048, D])
    nsa_w_kv = inp("nsa_w_kv", [D, 1536])
    cmp_k_pe = inp("cmp_k_pe", [32, 64])
    cmp_k_w1 = inp("cmp_k_w1", [2048, 256])
    cmp_k_w2 = inp("cmp_k_w2", [256, 64])
    cmp_v_pe = inp("cmp_v_pe", [32, 64])
    cmp_v_w1 = inp("cmp_v_w1", [2048, 256])
    cmp_v_w2 = inp("cmp_v_w2", [256, 64])
    nsa_w_q = inp("nsa_w_q", [1, D, 1072])
    nsa_w_out = inp("nsa_w_out", [1, D, D])
    ffn_w_in = inp("ffn_w_in", [1, D, 2 * DFF])
    ffn_w_out = inp("ffn_w_out", [1, DFF, D])
    moe_router = inp("moe_router", [1, D, NE])
    moe_w_in = inp("moe_w_in", [1, NE, D, 2 * DFFE])
    moe_w_out = inp("moe_w_out", [1, NE, DFFE, D])
    ln_g = inp("ln_g", [2, 2, D])
    ln_b = inp("ln_b", [2, 2, D])
    okind = "ExternalOutput" if dbg else "Internal"

    def scratch(name, shape, dtype=F32, src_phase=None):
        if dbg and src_phase is not None and src_phase not in phases:
            LAST_INPUTS.append(name)
            return nc.dram_tensor(name, list(shape), dtype, kind="ExternalInput").ap()
        return nc.dram_tensor(name, list(shape), dtype, kind=okind).ap()

    cos0 = scratch("cos0", [128, S])
    sin0 = scratch("sin0", [128, S])
    yn_d = scratch("yn_d", [NB, S, 2048], BF16)
    cos1 = scratch("cos1", [128, S])
    sin1 = scratch("sin1", [128, S])
    wsw_d = nc.dram_tensor("wsw_d", [20, 128, 8, 256], BF16, kind="Internal").ap()
    dbg_kc = dbg_vc = None
    if dbg:
        dbg_kc = nc.dram_tensor("dbg_kc", [NB, 128, NG, 128], BF16, kind="ExternalOutput").ap()
        dbg_vc = nc.dram_tensor("dbg_vc", [NB, 128, NG, 97], BF16, kind="ExternalOutput").ap()
    x1_d = scratch("x1", [NB, S, D], src_phase="ret2")
    x2_d = scratch("x2", [NB, S, D], src_phase="ffn")
    x3_d = scratch("x3", [NB, S, D], src_phase="nsa")
    out_d = nc.dram_tensor("out", [NB, S, D], F32, kind="ExternalOutput").ap()
    with ExitStack() as st:
        cx = Ctx(nc, st)
        c = _consts(cx, st, None)
        xb_, tab0_b, yn_b, x1_b, x2_b, x3_b, out_b = [Buf() for _ in range(7)]
        nsa_wswb = None
        tab1_b = Buf()
        tab1_done = False
        if "ret1" in phases:
            _rope_tables(cx, c, cos0, sin0, 128, 128, tab0_b)
            if "nsa" in phases:
                _rope_tables(cx, c, cos1, sin1, 32, 32, tab1_b, sign_rows=True)
                tab1_done = True
            cx.S.fence()
            phase_ret1(cx, c, x, xb_, ret_w_in[0], cos0, sin0, tab0_b, yn_d, yn_b)
            cx.S.fence()
        if "ret2" in phases:
            side = None
            if "nsa" in phases:
                pcs = _nsa_pieces(nsa_w_q[0], nsa_w_kv)
                prep = {"rings": None, "next": 0}
                wswb_pre = Buf()

                def side(k, st_):
                    if prep["rings"] is None:
                        prep["rings"] = _nsa_prep_rings(nc, st_)
                    if k % 3 == 2 and prep["next"] < len(pcs):
                        _nsa_prep_piece(cx, pcs, prep["next"], prep["rings"], wsw_d, wswb_pre)
                        prep["next"] += 1
            phase_ret2(cx, c, x, xb_, ret_w_in[0], ret_gn_g, ret_w_out[0], ln_g[0, 0:1, :], ln_b[0, 0:1, :],
                       yn_d, yn_b, x1_d, x1_b, side=side)
            if side is not None:
                assert prep["next"] == len(pcs)
                nsa_wswb = wswb_pre
            cx.S.fence()
        if "ffn" in phases:
            phase_ffn(cx, c, x1_d, x1_b, [ffn_w_in[0]], [ffn_w_out[0]], DFF, None, ln_g[0, 1:2, :], ln_b[0, 1:2, :],
                      x2_d, x2_b)
            cx.S.fence()
        if "nsa" in phases:
            dbgb = Buf()
            if not tab1_done:
                _rope_tables(cx, c, cos1, sin1, 32, 32, tab1_b, sign_rows=True)
                cx.S.fence()
            phase_nsa(cx, c, x2_d, x2_b, nsa_w_kv, cmp_k_pe, cmp_k_w1, cmp_k_w2, cmp_v_pe, cmp_v_w1, cmp_v_w2,
                      nsa_w_q[0], nsa_w_out[0], ln_g[1, 0:1, :], ln_b[1, 0:1, :], cos1, sin1, tab1_b, x3_d, x3_b,
                      wsw_d, dbg_kc, dbg_vc, dbgb, wswb=nsa_wswb)
            cx.S.fence()
        if "moe" in phases and os.environ.get("DBG_DENSE_MOE"):
            phase_ffn(cx, c, x3_d, x3_b, [moe_w_in[0, e] for e in range(NE)], [moe_w_out[0, e] for e in range(NE)],
                      DFFE, moe_router[0], ln_g[1, 1:2, :], ln_b[1, 1:2, :], out_d, out_b)
            cx.S.fence()
        elif "moe" in phases:
            xe_d = nc.dram_tensor("xe_d", [NSLOT, D], BF16, kind="Internal").ap()
            ye_d = nc.dram_tensor("ye_d", [NSLOT, D], F32, kind="Internal").ap()
            flag_d = nc.dram_tensor("flag_d", [1, 1], I32, kind=okind).ap()
            dbg_sl = nc.dram_tensor("dbg_sl", [128, NB * S // 128, 2], I32, kind="ExternalOutput").ap() if dbg else None
            flag_b = phase_moe_sparse(cx, c, x3_d, x3_b, [moe_w_in[0, e] for e in range(NE)],
                                      [moe_w_out[0, e] for e in range(NE)], moe_router[0], ln_g[1, 1:2, :],
                                      ln_b[1, 1:2, :], out_d, out_b, xe_d, ye_d, flag_d, dbg_sl)
            cx.S.fence()
            Sd = cx.S
            engs = OrderedEngineSet([mybir.EngineType.PE, mybir.EngineType.DVE, mybir.EngineType.Activation,
                                     mybir.EngineType.Pool, mybir.EngineType.SP])
            regs = nc.alloc_registers("ovf_flag", engs)
            for reg in regs:
                nc.reg_load(reg, flag_d[0:1, 0:1])
            cnt0 = dict(Sd.cnt)
            dcnt0 = list(Sd.dcnt)
            with nc.If(nc.snap(regs) > 0):
                phase_ffn(cx, c, x3_d, x3_b, [moe_w_in[0, e] for e in range(NE)],
                          [moe_w_out[0, e] for e in range(NE)], DFFE, moe_router[0], ln_g[1, 1:2, :],
                          ln_b[1, 1:2, :], out_d, out_b)
                Sd.fence()
            with nc.Else():
                for k in ("pe", "dve", "act", "pool"):
                    dlt = Sd.cnt[k] - cnt0[k]
                    if dlt > 0:
                        Sd.eng[k].sem_inc(Sd.sem[k], dlt)
                for i in range(Sd.NDMA):
                    dlt = Sd.dcnt[i] - dcnt0[i]
                    if dlt > 0:
                        nc.sync.sem_inc(Sd.dsem[i], dlt)
            cx.S.fence()
        cx.S.finish([x1_b, x2_b, x3_b, out_b, yn_b, tab0_b], "sp")
        print("instructions:", cx.S.ninstr)
    return nc


_PROG = {}


def kernel(**inputs):
    if "nc" not in _PROG:
        _PROG["nc"] = build_program(phases=("ret1", "ret2", "ffn", "nsa", "moe"), dbg=False)
        _PROG["names"] = list(LAST_INPUTS)
    nc = _PROG["nc"]
    names = _PROG["names"]
    x = np.ascontiguousarray(np.asarray(inputs["x"], dtype=np.float32))
    shared = {k: np.ascontiguousarray(np.asarray(inputs[k], dtype=np.float32)) for k in names if k != "x"}
    in_maps = []
    for ci in range(NCORES):
        m = dict(shared)
        m["x"] = np.ascontiguousarray(x[ci * NB:(ci + 1) * NB])
        in_maps.append(m)
    res = run_bass_kernel_spmd(nc, in_maps, core_ids=list(range(NCORES)))
    out = np.concatenate([np.asarray(r["out"], dtype=np.float32) for r in res.results], axis=0)
    return out
```

```python
import math
import os
from contextlib import ExitStack

import numpy as np
import concourse.bass as bass
import concourse.mybir as mybir
from concourse.bass_utils import run_bass_kernel_spmd
from concourse.bass import OrderedEngineSet

F32 = mybir.dt.float32
BF16 = mybir.dt.bfloat16
I32 = mybir.dt.int32
AF = mybir.ActivationFunctionType
ALU = mybir.AluOpType

NCORES = 8
NB = 2
S = 2048
D = 1024
NT = S // 128
DN_ALPHA = 4.0 ** 0.25
LN_EPS = 1e-5
ROPE_THETA = 10000.0
RH, RDK, RDV = 4, 256, 512
NH, NG, DH = 16, 4, 64
NCMP = 127
DFF = 2816
NE = 8
DFFE = 3584
NEG = -30000.0
DBG_R = int(os.environ.get('DBG_R', 4))
NOSELF = bool(os.environ.get('DBG_NOSELF'))
DBG_ATT = float(os.environ.get('DBG_ATT', 9))


_UCNT = [0]


def _u(name):
    _UCNT[0] += 1
    return "%s_u%d" % (name, _UCNT[0])


class Buf:
    __slots__ = ("name", "w", "r")

    def __init__(self, name=""):
        self.name = name
        self.w = []
        self.r = []


class Sched:
    NDMA = 32

    def __init__(self, nc, stack):
        self.nc = nc
        self.eng = {"pe": nc.tensor, "dve": nc.vector, "act": nc.scalar, "pool": nc.gpsimd, "sp": nc.sync}
        self.sem = {}
        self.cnt = {}
        for k in ("pe", "dve", "act", "pool"):
            self.sem[k] = stack.enter_context(nc.semaphore("s_" + k))
            self.cnt[k] = 0
        self.dsem = [stack.enter_context(nc.semaphore("s_dma%d" % i)) for i in range(self.NDMA)]
        self.dcnt = [0] * self.NDMA
        self.dnext = 0
        self.seen = {}
        self.ninstr = 0

    def _sem(self, key):
        return self.sem[key] if isinstance(key, str) else self.dsem[key]

    def _wait(self, e, ev):
        key, val = ev
        if e == "pe" and key == "pe":
            return
        if NOSELF and e == key:
            return
        if self.seen.get((e, key), 0) >= val:
            return
        self.eng[e].wait_ge(self._sem(key), val)
        self.seen[(e, key)] = val
        self.ninstr += 1

    def _deps(self, e, reads, writes, add=False):
        for b in reads:
            for ev in b.w:
                self._wait(e, ev)
        for b in writes:
            if not add:
                for ev in b.w:
                    self._wait(e, ev)
            for ev in b.r:
                self._wait(e, ev)

    def op(self, e, fn, reads=(), writes=(), inc=True):
        self._deps(e, reads, writes)
        ins = fn()
        self.ninstr += 1
        ev = (e, self.cnt[e] + 1)
        if inc:
            self.cnt[e] += 1
            ins.then_inc(self.sem[e], 1)
        for b in writes:
            b.w = [ev]
            b.r = []
        for b in reads:
            if ev not in b.r:
                b.r.append(ev)
        return ins

    def dma(self, q, out, in_, reads=(), writes=(), add=False):
        i = self.dnext
        self.dnext = (self.dnext + 1) % self.NDMA
        if self.dcnt[i] > 0:
            self._wait(q, (i, self.dcnt[i]))
        self._deps(q, reads, writes, add=add)
        self.dcnt[i] += 16
        ev = (i, self.dcnt[i])
        self.eng[q].dma_start(out=out, in_=in_).then_inc(self.dsem[i], 16)
        self.ninstr += 1
        for b in writes:
            if add:
                b.w.append(ev)
            else:
                b.w = [ev]
            b.r = []
        for b in reads:
            b.r.append(ev)
        return ev

    def fence(self):
        for e in ("pe", "dve", "act", "pool", "sp"):
            for k in ("pe", "dve", "act", "pool"):
                if self.cnt[k] > 0 and k != e:
                    self._wait(e, (k, self.cnt[k]))
            if e != "pe" and e != "sp" and self.cnt[e] > 0:
                self._wait(e, (e, self.cnt[e]))
            for i in range(self.NDMA):
                if self.dcnt[i] > 0:
                    self._wait(e, (i, self.dcnt[i]))

    def finish(self, bufs, e="sp"):
        for b in bufs:
            for ev in b.w:
                self._wait(e, ev)


class Ring:
    def __init__(self, nc, st, name, shape, dtype, n):
        self.t = [st.enter_context(nc.sbuf_tensor(_u("%s_%d") % (name, i), list(shape), dtype)) for i in range(n)]
        self.b = [Buf("%s_%d" % (name, i)) for i in range(n)]
        self.i = -1

    def next(self):
        self.i = (self.i + 1) % len(self.t)
        return self.t[self.i], self.b[self.i]

    def cur(self):
        return self.t[self.i], self.b[self.i]


class Ctx:
    def __init__(self, nc, st):
        self.nc = nc
        self.S = Sched(nc, st)
        self.dbl = [st.enter_context(nc.psum_tensor("dbank%d" % i, [128, 1024], F32)) for i in range(4)]
        self.banks = [self.dbl[i // 2][:, (i % 2) * 512:(i % 2 + 1) * 512] for i in range(8)]
        self.pi = -1
        self.prot = [1, 2, 3]
        self.bbuf = [Buf("bank%d" % i) for i in range(8)]
        self.bi = -1
        self.rot = list(range(8))
        self.regs = {}

    def bank_pair(self):
        self.pi = (self.pi + 1) % len(self.prot)
        j = self.prot[self.pi]
        return (self.banks[2 * j], self.bbuf[2 * j]), (self.banks[2 * j + 1], self.bbuf[2 * j + 1]), self.dbl[j]

    def reg(self, val):
        if val not in self.regs:
            self.regs[val] = self.nc.gpsimd.to_reg(val)
        return self.regs[val]

    def bank(self):
        self.bi = (self.bi + 1) % len(self.rot)
        i = self.rot[self.bi]
        return self.banks[i], self.bbuf[i]


def _consts(cx, st, dr):
    nc, S_ = cx.nc, cx.S
    c = {}
    ones = st.enter_context(nc.sbuf_tensor(_u("c_ones"), [128, 128], BF16))
    ident = st.enter_context(nc.sbuf_tensor(_u("c_ident"), [128, 128], BF16))
    bo, bi = Buf(), Buf()
    S_.op("pool", lambda: nc.gpsimd.memset(ones[:], 1.0), writes=[bo])
    S_.op("pool", lambda: nc.gpsimd.affine_select(out=ident[:], in_=ones[:], pattern=[[-1, 128]],
                                                    compare_op=ALU.is_equal, fill=cx.reg(0.0), base=0,
                                                    channel_multiplier=1), reads=[bo], writes=[bi])
    c["ident"], c["ident_b"] = ident, bi
    c["ones"], c["ones_b"] = ones, bo
    epsb = st.enter_context(nc.sbuf_tensor(_u("c_eps"), [128, 1], F32))
    be = Buf()
    S_.op("pool", lambda: nc.gpsimd.memset(epsb[:], LN_EPS), writes=[be])
    c["eps"], c["eps_b"] = epsb, be
    return c


def _rope_tables(cx, c, dr_cos, dr_sin, nfreq, rows_fn_period, bc, sign_rows=False):
    nc, S_ = cx.nc, cx.S
    with ExitStack() as st:
        pidx = st.enter_context(nc.sbuf_tensor(_u("rt_pidx"), [128, 1], F32))
        inv = st.enter_context(nc.sbuf_tensor(_u("rt_inv"), [128, 1], F32))
        pos = st.enter_context(nc.sbuf_tensor(_u("rt_pos"), [128, S], F32))
        ang = st.enter_context(nc.sbuf_tensor(_u("rt_ang"), [128, S], F32))
        ki = st.enter_context(nc.sbuf_tensor(_u("rt_ki"), [128, S], I32))
        kf = st.enter_context(nc.sbuf_tensor(_u("rt_kf"), [128, S], F32))
        res = st.enter_context(nc.sbuf_tensor(_u("rt_res"), [128, S], F32))
        b1, b2, b3, b4, b5, b6, b7 = [Buf() for _ in range(7)]
        per = rows_fn_period
        for base in range(0, 128, per):
            S_.op("pool", lambda base=base: nc.gpsimd.iota(pidx[base:base + per, :], pattern=[[0, 1]], base=0,
                                                           channel_multiplier=1,
                                                           allow_small_or_imprecise_dtypes=True), writes=[b1])
        S_.op("act", lambda: nc.scalar.activation(out=inv[:], in_=pidx[:], func=AF.Exp,
                                                  scale=-math.log(ROPE_THETA) / nfreq), reads=[b1], writes=[b2])
        S_.op("pool", lambda: nc.gpsimd.iota(pos[:], pattern=[[1, S]], base=0, channel_multiplier=0,
                                             allow_small_or_imprecise_dtypes=True), writes=[b3])
        S_.op("dve", lambda: nc.vector.tensor_scalar(out=ang[:], in0=pos[:], scalar1=inv[:, 0:1], scalar2=None,
                                                     op0=ALU.mult), reads=[b3, b2], writes=[b4])
        for which, dr_t in ((0, dr_sin), (1, dr_cos)):
            shift = 0.0 if which == 0 else math.pi / 2
            S_.op("dve", lambda: nc.vector.tensor_scalar(out=ki[:], in0=ang[:], scalar1=shift,
                                                         scalar2=1.0 / (2 * math.pi), op0=ALU.add, op1=ALU.mult),
                  reads=[b4], writes=[b5])
            S_.op("dve", lambda: nc.vector.tensor_copy(out=kf[:], in_=ki[:]), reads=[b5], writes=[b6])
            S_.op("dve", lambda: nc.vector.scalar_tensor_tensor(out=res[:], in0=kf[:], scalar=-2 * math.pi,
                                                                in1=ang[:], op0=ALU.mult, op1=ALU.add),
                  reads=[b6, b4], writes=[b7])
            S_.op("dve", lambda: nc.vector.tensor_scalar(out=res[:], in0=res[:], scalar1=shift, scalar2=3.14159,
                                                         op0=ALU.add, op1=ALU.min), reads=[b7], writes=[b7])
            S_.op("dve", lambda: nc.vector.tensor_scalar(out=res[:], in0=res[:], scalar1=-3.14159, scalar2=None,
                                                         op0=ALU.max), reads=[b7], writes=[b7])
            S_.op("act", lambda: nc.scalar.activation(out=res[:], in_=res[:], func=AF.Sin), reads=[b7], writes=[b7])
            if sign_rows and which == 0:
                for p0 in (0, 64):
                    S_.op("act", lambda p0=p0: nc.scalar.mul(out=res[p0:p0 + 32, :], in_=res[p0:p0 + 32, :], mul=-1.0),
                          reads=[b7], writes=[b7])
            S_.dma("sp", dr_t[:, :], res[:], reads=[b7], writes=[bc])


def _load_w(cx, Wt, Wb, w_ap, kchunks, c0, c1, dst_c0=None, q="pool"):
    if dst_c0 is None:
        dst_c0 = c0
    n = c1 - c0
    for kc in range(kchunks):
        cx.S.dma(q, Wt[:, kc, dst_c0:dst_c0 + n], w_ap[kc * 128:(kc + 1) * 128, c0:c1], writes=[Wb], add=(kc > 0))


def _transpose_to(cx, c, src_t, src_b, nchunk, dst_ap_fn, dst_b, evac="act"):
    nc, S_ = cx.nc, cx.S
    for k0 in range(0, nchunk, 8):
        k1 = min(nchunk, k0 + 8)
        bk, bb = cx.bank()
        pb = bk.bitcast(BF16)
        for k in range(k0, k1):
            S_.op("pe", lambda k=k: nc.tensor.transpose(out=pb[:, (k - k0) * 128:(k - k0 + 1) * 128],
                                                        in_=src_t[:, k * 128:(k + 1) * 128],
                                                        identity=c["ident"][:]),
                  reads=[src_b, c["ident_b"]], writes=[bb], inc=(k == k1 - 1))
        dst = dst_ap_fn(k0, k1)
        if evac == "act":
            S_.op("act", lambda: nc.scalar.copy(out=dst, in_=pb[:, 0:(k1 - k0) * 128]), reads=[bb], writes=[dst_b])
        else:
            S_.op("dve", lambda: nc.vector.tensor_copy(out=dst, in_=pb[:, 0:(k1 - k0) * 128]), reads=[bb],
                  writes=[dst_b])


def _layernorm_store(cx, c, z_t, z_b, lng, lnb, lnpb, dst_ap, dst_b, st6, mv, rs, sb):
    nc, S_ = cx.nc, cx.S
    for hh in range(2):
        S_.op("dve", lambda hh=hh: nc.vector.bn_stats(out=st6[:, hh, :], in_=z_t[:, hh * 512:(hh + 1) * 512]),
              reads=[z_b], writes=[sb])
    S_.op("dve", lambda: nc.vector.bn_aggr(out=mv[:], in_=st6[:].rearrange("p a b -> p (a b)")), reads=[sb], writes=[sb])
    S_.op("act", lambda: nc.scalar.activation(out=rs[:], in_=mv[:, 1:2], func=AF.Sqrt, bias=c["eps"][:], scale=1.0),
          reads=[sb, c["eps_b"]], writes=[sb])
    S_.op("dve", lambda: nc.vector.reciprocal(out=rs[:], in_=rs[:]), reads=[sb], writes=[sb])
    S_.op("dve", lambda: nc.vector.scalar_tensor_tensor(out=z_t[:], in0=z_t[:], scalar=mv[:, 0:1], in1=lng[:],
                                                        op0=ALU.subtract, op1=ALU.mult),
          reads=[z_b, sb, lnpb], writes=[z_b])
    S_.op("dve", lambda: nc.vector.scalar_tensor_tensor(out=z_t[:], in0=z_t[:], scalar=rs[:, 0:1], in1=lnb[:],
                                                        op0=ALU.mult, op1=ALU.add),
          reads=[z_b, sb, lnpb], writes=[z_b])
    S_.dma("sp", dst_ap, z_t[:], reads=[z_b], writes=[dst_b], add=True)


def phase_ret1(cx, c, x_in, x_b, w_in, cos_d, sin_d, tab_b, yn_d, yn_b):
    nc, S_ = cx.nc, cx.S
    lg = [math.log1p(-(2.0 ** (-5.0 - h))) for h in range(RH)]
    with ExitStack() as st:
        W = st.enter_context(nc.sbuf_tensor(_u("r1_W"), [128, 8, 4096], BF16))
        Wb = [Buf() for _ in range(4)]
        for j in range(4):
            _load_w(cx, W, Wb[j], w_in, 8, j * 1024, (j + 1) * 1024)
        dec = st.enter_context(nc.sbuf_tensor(_u("r1_dec"), [128, RH, 128], F32))
        qdec = st.enter_context(nc.sbuf_tensor(_u("r1_qdec"), [128, RH, 128], F32))
        kdec = st.enter_context(nc.sbuf_tensor(_u("r1_kdec"), [128, RH], F32))
        io = st.enter_context(nc.sbuf_tensor(_u("r1_io"), [128, 128], F32))
        io2 = st.enter_context(nc.sbuf_tensor(_u("r1_io2"), [128, 128], F32))
        io3 = st.enter_context(nc.sbuf_tensor(_u("r1_io3"), [128, 1], F32))
        lnsc = st.enter_context(nc.sbuf_tensor(_u("r1_lnsc"), [128, 1], F32))
        bt = Buf()
        S_.op("pool", lambda: nc.gpsimd.iota(io[:], pattern=[[1, 128]], base=0, channel_multiplier=-1,
                                             allow_small_or_imprecise_dtypes=True), writes=[bt])
        S_.op("pool", lambda: nc.gpsimd.iota(io2[:], pattern=[[1, 128]], base=1, channel_multiplier=0,
                                             allow_small_or_imprecise_dtypes=True), writes=[bt])
        S_.op("pool", lambda: nc.gpsimd.iota(io3[:], pattern=[[0, 1]], base=127, channel_multiplier=-1,
                                             allow_small_or_imprecise_dtypes=True), writes=[bt])
        S_.op("pool", lambda: nc.gpsimd.memset(lnsc[:], math.log(1.0 / 16.0)), writes=[bt])
        bdec = Buf()
        for h in range(RH):
            S_.op("act", lambda h=h: nc.scalar.activation(out=dec[:, h, :], in_=io[:], func=AF.Exp, scale=lg[h],
                                                          bias=lnsc[:]), reads=[bt], writes=[bdec])
            S_.op("pool", lambda h=h: nc.gpsimd.affine_select(out=dec[:, h, :], in_=dec[:, h, :],
                                                              pattern=[[1, 128]], compare_op=ALU.is_ge, fill=cx.reg(0.0),
                                                              base=0, channel_multiplier=-1),
                  reads=[bdec], writes=[bdec])
            S_.op("act", lambda h=h: nc.scalar.activation(out=qdec[:, h, :], in_=io2[:], func=AF.Exp, scale=lg[h]),
                  reads=[bt], writes=[bdec])
            S_.op("act", lambda h=h: nc.scalar.activation(out=kdec[:, h:h + 1], in_=io3[:], func=AF.Exp, scale=lg[h],
                                                          bias=lnsc[:]), reads=[bt], writes=[bdec])
        states = [st.enter_context(nc.sbuf_tensor(_u("r1_state"), [128, RH, 2, 512], F32)) for _ in range(NB)]
        states_bf = [st.enter_context(nc.sbuf_tensor(_u("r1_statebf"), [128, RH, 2, 512], BF16)) for _ in range(NB)]
        stbs = [[[Buf() for _ in range(2)] for _ in range(RH)] for _ in range(NB)]
        stbbs = [[[Buf() for _ in range(2)] for _ in range(RH)] for _ in range(NB)]
        xtok = Ring(nc, st, "r1_xtok", [128, D], F32, 4)
        xbr = Ring(nc, st, "r1_xb", [128, D], BF16, 1)
        xTr = Ring(nc, st, "r1_xT", [128, 8, 128], BF16, 2)
        csr = Ring(nc, st, "r1_cs", [128, 2, 128], F32, 4)
        qkr = Ring(nc, st, "r1_qk", [128, RH, 2, 2, 128], BF16, 3)
        t1r = Ring(nc, st, "r1_t1", [128, 2, 128], F32, 2)
        t2r = Ring(nc, st, "r1_t2", [128, 2, 128], F32, 2)
        vr = Ring(nc, st, "r1_v", [128, 2048], BF16, 3)
        kdr = Ring(nc, st, "r1_kd", [128, RH, 256], BF16, 2)
        sTr = Ring(nc, st, "r1_sT", [128, RH, 128], BF16, 2)
        qdr = Ring(nc, st, "r1_qd", [128, RH, 2, 128], BF16, 2)
        tmpr = Ring(nc, st, "r1_tmp", [128, 512], F32, 2)
        ynr = Ring(nc, st, "r1_yn", [128, 2048], BF16, 2)
        st6r = Ring(nc, st, "r1_st6", [128, RH, 6], F32, 2)
        mvr = Ring(nc, st, "r1_mv", [128, RH, 2], F32, 2)
        rsr = Ring(nc, st, "r1_rs", [128, RH], F32, 2)

        items = [(b, n) for n in range(NT) for b in range(NB)]
        pre = {}

        def loads(k):
            b, n = items[k]
            t0 = n * 128
            xt, xtb = xtok.next()
            S_.dma("sp", xt[:], x_in[b, t0:t0 + 128, :], reads=[x_b], writes=[xtb])
            cs, csb = csr.next()
            S_.dma("sp", cs[:, 0, :], cos_d[:, t0:t0 + 128], reads=[tab_b], writes=[csb])
            S_.dma("sp", cs[:, 1, :], sin_d[:, t0:t0 + 128], reads=[tab_b], writes=[csb], add=True)
            pre[k] = (xt, xtb, cs, csb)

        def stageA(k):
            b, n = items[k]
            t0 = n * 128
            xt, xtb, cs, csb = pre.pop(k)
            xb, xbb = xbr.next()
            S_.op("pool", lambda: nc.gpsimd.tensor_copy(out=xb[:], in_=xt[:]), reads=[xtb], writes=[xbb])
            xT, xTb = xTr.next()
            _transpose_to(cx, c, xb, xbb, 8,
                          lambda k0, k1: xT[:, k0:k1, :].rearrange("p a b -> p (a b)"), xTb, evac="dve")
            qk, qkb = qkr.next()
            for h in range(RH):
                bk, bb = cx.bank()
                bv = bk[:].rearrange("p (a b c) -> p a b c", a=2, b=2)
                for qi in range(2):
                    for half in range(2):
                        col = qi * 1024 + h * 256 + half * 128
                        for kc in range(8):
                            S_.op("pe", lambda kc=kc, col=col, qi=qi, half=half: nc.tensor.matmul(
                                bv[:, qi, half, :], lhsT=W[:, kc, col:col + 128], rhs=xT[:, kc, :],
                                start=(kc == 0), stop=(kc == 7)),
                                reads=[Wb[qi], xTb], writes=[bb], inc=(kc == 7 and qi == 1 and half == 1))
                cosb = cs[:, 0:1, :].broadcast_to([128, 2, 128])
                sinb = cs[:, 1:2, :].broadcast_to([128, 2, 128])
                A = bv[:, :, 0, :]
                Bh = bv[:, :, 1, :]
                t1, t1b = t1r.next()
                t2, t2b = t2r.next()
                S_.op("dve", lambda: nc.vector.tensor_tensor(out=t1[:], in0=A, in1=cosb, op=ALU.mult),
                      reads=[bb, csb], writes=[t1b])
                S_.op("dve", lambda: nc.vector.tensor_tensor(out=t2[:], in0=Bh, in1=sinb, op=ALU.mult),
                      reads=[bb, csb], writes=[t2b])
                S_.op("pool", lambda h=h: nc.gpsimd.tensor_tensor(out=qk[:, h, :, 0, :], in0=t1[:], in1=t2[:],
                                                                   op=ALU.subtract),
                      reads=[t1b, t2b], writes=[qkb])
                t1, t1b = t1r.next()
                t2, t2b = t2r.next()
                S_.op("dve", lambda: nc.vector.tensor_tensor(out=t1[:], in0=A, in1=sinb, op=ALU.mult),
                      reads=[bb, csb], writes=[t1b])
                S_.op("dve", lambda: nc.vector.tensor_tensor(out=t2[:], in0=Bh, in1=cosb, op=ALU.mult),
                      reads=[bb, csb], writes=[t2b])
                S_.op("pool", lambda h=h: nc.gpsimd.tensor_tensor(out=qk[:, h, :, 1, :], in0=t1[:], in1=t2[:],
                                                                   op=ALU.add),
                      reads=[t1b, t2b], writes=[qkb])
            v, vb = vr.next()
            for j in range(4):
                bk, bb = cx.bank()
                for kc in range(8):
                    S_.op("pe", lambda kc=kc, j=j: nc.tensor.matmul(
                        bk[:], lhsT=xT[:, kc, :], rhs=W[:, kc, 2048 + j * 512:2048 + (j + 1) * 512],
                        start=(kc == 0), stop=(kc == 7)), reads=[Wb[2 + j // 2], xTb], writes=[bb],
                        inc=(kc == 7))
                S_.op("act", lambda j=j: nc.scalar.copy(out=v[:, j * 512:(j + 1) * 512], in_=bk[:]),
                      reads=[bb], writes=[vb])
            return (qk, qkb, v, vb)

        def stageB(k, stt):
            b, n = items[k]
            t0 = n * 128
            qk, qkb, v, vb = stt
            state, state_bf, stb, stbb = states[b], states_bf[b], stbs[b], stbbs[b]
            bk, bb = cx.bank()
            bs = bk[:].rearrange("p (h i) -> p h i", h=RH)
            for h in range(RH):
                for half in range(2):
                    S_.op("pe", lambda h=h, half=half: nc.tensor.matmul(
                        bs[:, h, :], lhsT=qk[:, h, 1, half, :], rhs=qk[:, h, 0, half, :],
                        start=(half == 0), stop=(half == 1)), reads=[qkb], writes=[bb],
                        inc=(h == RH - 1 and half == 1))
            sT, sTb = sTr.next()
            S_.op("dve", lambda: nc.vector.tensor_tensor(out=sT[:], in0=bs, in1=dec[:], op=ALU.mult),
                  reads=[bb, bdec], writes=[sTb])
            kd, kdb = kdr.next()
            if n < NT - 1:
                bk, bb = cx.bank()
                pb = bk.bitcast(BF16)
                for h in range(RH):
                    for half in range(2):
                        o = (h * 2 + half) * 128
                        S_.op("pe", lambda h=h, half=half, o=o: nc.tensor.transpose(
                            out=pb[:, o:o + 128], in_=qk[:, h, 1, half, :], identity=c["ident"][:]),
                            reads=[qkb, c["ident_b"]], writes=[bb], inc=(h == RH - 1 and half == 1))
                S_.op("dve", lambda: nc.vector.tensor_tensor(
                    out=kd[:], in0=pb[:, 0:1024].rearrange("p (h d) -> p h d", h=RH),
                    in1=kdec[:].unsqueeze(2).broadcast_to([128, RH, 256]), op=ALU.mult),
                    reads=[bb, bdec], writes=[kdb])
            qd, qdb = qdr.next()
            if n > 0:
                S_.op("pool", lambda: nc.gpsimd.tensor_tensor(
                    out=qd[:], in0=qk[:, :, 0, :, :],
                    in1=qdec[:].unsqueeze(2).broadcast_to([128, RH, 2, 128]), op=ALU.mult),
                    reads=[qkb, bdec], writes=[qdb])
            yn, ynb = ynr.next()
            st6, st6b = st6r.next()
            mv, mvb = mvr.next()
            rs, rsb = rsr.next()
            pos = []
            for h in range(RH):
                po, pob = cx.bank()
                pos.append((po, pob))
                S_.op("pe", lambda h=h, po=po: nc.tensor.matmul(po[:], lhsT=sT[:, h, :],
                                                                rhs=v[:, h * 512:(h + 1) * 512],
                                                                start=True, stop=(n == 0)),
                      reads=[sTb, vb], writes=[pob], inc=(n == 0))
                if n > 0:
                    for half in range(2):
                        S_.op("pe", lambda h=h, half=half, po=po: nc.tensor.matmul(
                            po[:], lhsT=qd[:, h, half, :], rhs=state_bf[:, h, half, :],
                            start=False, stop=(half == 1)),
                            reads=[qdb, stbb[h][half]], writes=[pob], inc=(half == 1))
                S_.op("dve", lambda h=h, po=po: nc.vector.bn_stats(out=st6[:, h, :], in_=po[:]),
                      reads=[pob], writes=[st6b])
                S_.op("dve", lambda h=h: nc.vector.bn_aggr(out=mv[:, h, :], in_=st6[:, h, :]),
                      reads=[st6b], writes=[mvb])
                S_.op("act", lambda h=h: nc.scalar.activation(out=rs[:, h:h + 1], in_=mv[:, h, 1:2], func=AF.Sqrt,
                                                              bias=c["eps"][:], scale=1.0),
                      reads=[mvb, c["eps_b"]], writes=[rsb])
                S_.op("dve", lambda h=h: nc.vector.reciprocal(out=rs[:, h:h + 1], in_=rs[:, h:h + 1]),
                      reads=[rsb], writes=[rsb])
                S_.op("dve", lambda h=h, po=po: nc.vector.tensor_scalar(
                    out=yn[:, h * 512:(h + 1) * 512], in0=po[:], scalar1=mv[:, h, 0:1], scalar2=rs[:, h:h + 1],
                    op0=ALU.subtract, op1=ALU.mult), reads=[pob, mvb, rsb], writes=[ynb])
                if n < NT - 1:
                    for half in range(2):
                        pu, pub = cx.bank()
                        S_.op("pe", lambda h=h, half=half, pu=pu: nc.tensor.matmul(
                            pu[:], lhsT=kd[:, h, half * 128:(half + 1) * 128], rhs=v[:, h * 512:(h + 1) * 512],
                            start=True, stop=True), reads=[kdb, vb], writes=[pub])
                        if n == 0:
                            S_.op("dve", lambda h=h, half=half, pu=pu: nc.vector.tensor_copy(
                                out=state[:, h, half, :], in_=pu[:]), reads=[pub], writes=[stb[h][half]])
                        else:
                            S_.op("dve", lambda h=h, half=half, pu=pu: nc.vector.scalar_tensor_tensor(
                                out=state[:, h, half, :], in0=state[:, h, half, :],
                                scalar=math.exp(lg[h] * 128), in1=pu[:], op0=ALU.mult, op1=ALU.add),
                                reads=[pub, stb[h][half]], writes=[stb[h][half]])
                        S_.op("act", lambda h=h, half=half: nc.scalar.copy(
                            out=state_bf[:, h, half, :], in_=state[:, h, half, :]),
                            reads=[stb[h][half]], writes=[stbb[h][half]])
            S_.dma("sp", yn_d[b, t0:t0 + 128, :], yn[:], reads=[ynb], writes=[yn_b], add=True)


        loads(0)
        loads(1)
        nxt = stageA(0)
        for k in range(len(items)):
            cur = nxt
            if k + 2 < len(items):
                loads(k + 2)
            if k + 1 < len(items):
                nxt = stageA(k + 1)
            stageB(k, cur)


def phase_ret2(cx, c, x_in, x_b, w_in, gn_g, w_out, ln_g, ln_b, yn_d, yn_b, x1_d, x1_b, side=None):
    nc, S_ = cx.nc, cx.S
    with ExitStack() as st:
        Wg = st.enter_context(nc.sbuf_tensor(_u("r2_Wg"), [128, 8, 2048], BF16))
        Wo = st.enter_context(nc.sbuf_tensor(_u("r2_Wo"), [128, 16, 1024], BF16))
        Wgb, Wob = [Buf(), Buf()], [Buf(), Buf()]
        for j in range(2):
            _load_w(cx, Wg, Wgb[j], w_in, 8, 4096 + j * 1024, 4096 + (j + 1) * 1024, dst_c0=j * 1024)
        for j in range(2):
            _load_w(cx, Wo, Wob[j], w_out, 16, j * 512, (j + 1) * 512)
        gng = st.enter_context(nc.sbuf_tensor(_u("r2_gng"), [128, 2048], F32))
        lng = st.enter_context(nc.sbuf_tensor(_u("r2_lng"), [128, D], F32))
        lnb = st.enter_context(nc.sbuf_tensor(_u("r2_lnb"), [128, D], F32))
        pb_ = Buf()
        S_.dma("sp", gng[:], gn_g[0:1, :].partition_broadcast(128), writes=[pb_])
        S_.dma("sp", lng[:], ln_g[0:1, :].partition_broadcast(128), writes=[pb_], add=True)
        S_.dma("sp", lnb[:], ln_b[0:1, :].partition_broadcast(128), writes=[pb_], add=True)
        xtok = Ring(nc, st, "r2_xtok", [128, D], F32, 4)
        xbr = Ring(nc, st, "r2_xb", [128, D], BF16, 2)
        xTr = Ring(nc, st, "r2_xT", [128, 8, 128], BF16, 2)
        ynr = Ring(nc, st, "r2_yn", [128, 2048], BF16, 4)
        sgr = Ring(nc, st, "r2_sg", [128, 2048], F32, 2)
        yr = Ring(nc, st, "r2_y", [128, 2048], BF16, 3)
        yTr = Ring(nc, st, "r2_yT", [128, 16, 128], BF16, 2)
        zr = Ring(nc, st, "r2_z", [128, D], F32, 2)
        st6r = Ring(nc, st, "r2_st6", [128, 2, 6], F32, 2)
        mvr = Ring(nc, st, "r2_mv", [128, 2], F32, 2)
        rsr = Ring(nc, st, "r2_rs", [128, 1], F32, 2)
        items = [(b, n) for b in range(NB) for n in range(NT)]
        pre = {}

        def loads(k):
            b, n = items[k]
            t0 = n * 128
            xt, xtb = xtok.next()
            S_.dma("sp", xt[:], x_in[b, t0:t0 + 128, :], reads=[x_b], writes=[xtb])
            yn, ynb = ynr.next()
            S_.dma("sp", yn[:], yn_d[b, t0:t0 + 128, :], reads=[yn_b], writes=[ynb])
            pre[k] = (xt, xtb, yn, ynb)

        def stage1(k):
            xt, xtb, yn, ynb = pre.pop(k)
            xb, xbb = xbr.next()
            S_.op("pool", lambda: nc.gpsimd.tensor_copy(out=xb[:], in_=xt[:]), reads=[xtb], writes=[xbb])
            xT, xTb = xTr.next()
            _transpose_to(cx, c, xb, xbb, 8,
                          lambda k0, k1: xT[:, k0:k1, :].rearrange("p a b -> p (a b)"), xTb, evac="dve")
            sg, sgb = sgr.next()
            for j in range(4):
                bk, bb = cx.bank()
                for kc in range(8):
                    S_.op("pe", lambda kc=kc, j=j, bk=bk: nc.tensor.matmul(
                        bk[:], lhsT=xT[:, kc, :], rhs=Wg[:, kc, j * 512:(j + 1) * 512],
                        start=(kc == 0), stop=(kc == 7)), reads=[Wgb[j // 2], xTb], writes=[bb], inc=(kc == 7))
                S_.op("act", lambda j=j, bk=bk: nc.scalar.activation(out=sg[:, j * 512:(j + 1) * 512], in_=bk[:],
                                                                     func=AF.Silu), reads=[bb], writes=[sgb])
            S_.op("pool", lambda: nc.gpsimd.tensor_tensor(out=sg[:], in0=sg[:], in1=gng[:], op=ALU.mult),
                  reads=[sgb, pb_], writes=[sgb])
            y, yb = yr.next()
            S_.op("dve", lambda: nc.vector.tensor_tensor(out=y[:], in0=sg[:], in1=yn[:], op=ALU.mult),
                  reads=[sgb, ynb], writes=[yb])
            return (xt, xtb, y, yb)

        def stage2(k, stt):
            b, n = items[k]
            t0 = n * 128
            xt, xtb, y, yb = stt
            yT, yTb = yTr.next()
            _transpose_to(cx, c, y, yb, 16,
                          lambda k0, k1: yT[:, k0:k1, :].rearrange("p a b -> p (a b)"), yTb)
            z, zb = zr.next()
            for hh in range(2):
                bk, bb = cx.bank()
                for kc in range(16):
                    S_.op("pe", lambda kc=kc, hh=hh, bk=bk: nc.tensor.matmul(
                        bk[:], lhsT=yT[:, kc, :], rhs=Wo[:, kc, hh * 512:(hh + 1) * 512],
                        start=(kc == 0), stop=(kc == 15)), reads=[Wob[hh], yTb], writes=[bb], inc=(kc == 15))
                S_.op("dve", lambda hh=hh, bk=bk: nc.vector.scalar_tensor_tensor(
                    out=z[:, hh * 512:(hh + 1) * 512], in0=xt[:, hh * 512:(hh + 1) * 512], scalar=DN_ALPHA,
                    in1=bk[:], op0=ALU.mult, op1=ALU.add), reads=[bb, xtb], writes=[zb])
            st6, sb = st6r.next()
            mv, _ = mvr.next()
            rs, _ = rsr.next()
            _layernorm_store(cx, c, z, zb, lng, lnb, pb_, x1_d[b, t0:t0 + 128, :], x1_b, st6, mv, rs, sb)

        loads(0)
        loads(1)
        nxt = stage1(0)
        for k in range(len(items)):
            cur = nxt
            if k + 2 < len(items):
                loads(k + 2)
            if k + 1 < len(items):
                nxt = stage1(k + 1)
            stage2(k, cur)
            if side is not None:
                side(k, st)


def _load_w3(cx, dst_ap, w_ap, r0, nk, c0, c1, wb, add=False, q="pool"):
    src = w_ap[r0:r0 + nk * 128, c0:c1].rearrange("(k p) c -> p k c", p=128)
    cx.S.dma(q, dst_ap, src, writes=[wb], add=add)


def phase_ffn(cx, c, x_d, x_b, w_ins, w_outs, F, router, ln_g, ln_b, dst_d, dst_b, T=1024):
    nc, S_ = cx.nc, cx.S
    ne = int(os.environ.get('DBG_NE', len(w_ins)))
    moe = router is not None and not os.environ.get('DBG_NOROUTER')
    ntt = T // 128
    groups = []
    f0 = 0
    while f0 < F:
        g = min(512, F - f0)
        groups.append((f0, g))
        f0 += g
    with ExitStack() as st:
        lng = st.enter_context(nc.sbuf_tensor(_u("f_lng"), [128, D], F32))
        lnb = st.enter_context(nc.sbuf_tensor(_u("f_lnb"), [128, D], F32))
        pb_ = Buf()
        S_.dma("sp", lng[:], ln_g.partition_broadcast(128), writes=[pb_])
        S_.dma("sp", lnb[:], ln_b.partition_broadcast(128), writes=[pb_], add=True)
        if moe:
            wr = st.enter_context(nc.sbuf_tensor(_u("f_wr"), [128, 8, NE], F32))
            wrb = Buf()
            S_.dma("sp", wr[:], router.rearrange("(k p) e -> p k e", p=128), writes=[wrb])
            ident32 = st.enter_context(nc.sbuf_tensor(_u("f_id32"), [128, 128], F32))
            idb = Buf()
            S_.op("pool", lambda: nc.gpsimd.tensor_copy(out=ident32[:], in_=c["ident"][:]), reads=[c["ident_b"]],
                  writes=[idb])
            gate = st.enter_context(nc.sbuf_tensor(_u("f_gate"), [128, ntt, NE], F32))
            gateb = Buf()
            xT32r = Ring(nc, st, "f_xT32", [128, 8, 128], F32, 2)
            lgr = Ring(nc, st, "f_lg", [128, NE], F32, 2)
            topr = Ring(nc, st, "f_top", [128, 8], F32, 2)
            ssr = Ring(nc, st, "f_ss", [128, 1], F32, 2)
            sgr = Ring(nc, st, "f_sg", [128, NE], F32, 2)
        acc = st.enter_context(nc.sbuf_tensor(_u("f_acc"), [128, ntt, D], F32))
        accb = [Buf() for _ in range(ntt)]
        nxT = 1 if moe else 2
        xTs = [st.enter_context(nc.sbuf_tensor(_u("f_xT"), [128, 8, T], BF16)) for _ in range(nxT)]
        xTbs = [[Buf() for _ in range(ntt)] for _ in range(nxT)]
        xtok = Ring(nc, st, "f_xtok", [128, D], F32, 4)
        xbr = Ring(nc, st, "f_xb", [128, D], BF16, 2)
        Wir = Ring(nc, st, "f_Wi", [128, 8, 2, 512], BF16, 2)
        Wor = Ring(nc, st, "f_Wo", [128, 4, D], BF16, 2)
        hTr = Ring(nc, st, "f_hT", [128, 4, T], BF16, 2)
        sar = Ring(nc, st, "f_sa", [128, 512], F32, 3)
        st6r = Ring(nc, st, "f_st6", [128, 2, 6], F32, 2)
        mvr = Ring(nc, st, "f_mv", [128, 2], F32, 2)
        rsr = Ring(nc, st, "f_rs", [128, 1], F32, 2)

        work = [(e, gi) for e in range(ne) for gi in range(len(groups))]
        wpre = {}

        def wload(k):
            e, gi = work[k % len(work)]
            f0, g = groups[gi]
            Wi, Wib = Wir.next()
            Wo, Wob = Wor.next()
            _load_w3(cx, Wi[:, :, 0, 0:g], w_ins[e], 0, 8, f0, f0 + g, Wib)
            _load_w3(cx, Wi[:, :, 1, 0:g], w_ins[e], 0, 8, F + f0, F + f0 + g, Wib, add=True)
            _load_w3(cx, Wo[:, 0:g // 128, :], w_outs[e], f0, g // 128, 0, D, Wob)
            wpre[k] = (Wi, Wib, Wo, Wob)

        nmac = NB * S // T

        def early(m):
            b = (m * T) // S
            s0 = (m * T) % S
            xT, xTb = xTs[m % nxT], xTbs[m % nxT]
            for tt in range(ntt):
                xt, xtb = xtok.next()
                S_.dma("sp", xt[:], x_d[b, s0 + tt * 128:s0 + (tt + 1) * 128, :], reads=[x_b], writes=[xtb])
                xb, xbb = xbr.next()
                S_.op("act", lambda xb=xb, xt=xt: nc.scalar.copy(out=xb[:], in_=xt[:]), reads=[xtb], writes=[xbb])
                _transpose_to(cx, c, xb, xbb, 8,
                              lambda k0, k1, tt=tt, xT=xT: xT[:, k0:k1, tt * 128:(tt + 1) * 128], xTb[tt])

        wk = 0
        wload(0)
        if not moe:
            early(0)
        for m in range(nmac):
            b = (m * T) // S
            s0 = (m * T) % S
            xT, xTb = xTs[m % nxT], xTbs[m % nxT]
            for tt in range(ntt):
                xt, xtb = xtok.next()
                S_.dma("sp", xt[:], x_d[b, s0 + tt * 128:s0 + (tt + 1) * 128, :], reads=[x_b], writes=[xtb])
                S_.op("act", lambda tt=tt, xt=xt: nc.scalar.mul(out=acc[:, tt, :], in_=xt[:], mul=DN_ALPHA),
                      reads=[xtb], writes=[accb[tt]])
                if not moe:
                    pass
                else:
                    xT32, xT32b = xT32r.next()
                    for k0 in (0, 4):
                        bk, bb = cx.bank()
                        for k in range(k0, k0 + 4):
                            S_.op("pe", lambda k=k, k0=k0, bk=bk, xt=xt: nc.tensor.transpose(
                                out=bk[:, (k - k0) * 128:(k - k0 + 1) * 128], in_=xt[:, k * 128:(k + 1) * 128],
                                identity=ident32[:]), reads=[xtb, idb], writes=[bb], inc=(k == k0 + 3))
                        S_.op("act", lambda k0=k0, bk=bk, xT32=xT32: nc.scalar.copy(
                            out=xT32[:, k0:k0 + 4, :], in_=bk[:].rearrange("p (a b) -> p a b", a=4)),
                            reads=[bb], writes=[xT32b])
                        S_.op("pool", lambda k0=k0, xT32=xT32, tt=tt: nc.gpsimd.tensor_copy(
                            out=xT[:, k0:k0 + 4, tt * 128:(tt + 1) * 128],
                            in_=xT32[:, k0:k0 + 4, :]), reads=[xT32b], writes=[xTb[tt]])
                    if DBG_R < 2:
                        continue
                    bk, bb = cx.bank()
                    for kc in range(8):
                        S_.op("pe", lambda kc=kc, bk=bk, xT32=xT32: nc.tensor.matmul(
                            bk[:, 0:NE], lhsT=xT32[:, kc, :], rhs=wr[:, kc, :], start=(kc == 0), stop=(kc == 7)),
                            reads=[xT32b, wrb], writes=[bb], inc=(kc == 7))
                    lg_, lgb = lgr.next()
                    top, topb = topr.next()
                    ss, ssb = ssr.next()
                    sg, sgb = sgr.next()
                    S_.op("dve", lambda bk=bk, lg_=lg_: nc.vector.tensor_copy(out=lg_[:], in_=bk[:, 0:NE]),
                          reads=[bb], writes=[lgb])
                    if DBG_R < 3:
                        continue
                    S_.op("dve", lambda lg_=lg_, top=top: nc.vector.max(out=top[:], in_=lg_[:]), reads=[lgb],
                          writes=[topb])
                    S_.op("dve", lambda top=top, ss=ss: nc.vector.tensor_tensor(out=ss[:], in0=top[:, 0:1],
                                                                               in1=top[:, 1:2], op=ALU.add),
                          reads=[topb], writes=[ssb])
                    S_.op("dve", lambda lg_=lg_, ss=ss, sg=sg: nc.vector.tensor_scalar(
                        out=sg[:], in0=lg_[:], scalar1=2.0, scalar2=ss[:, 0:1], op0=ALU.mult, op1=ALU.subtract),
                        reads=[lgb, ssb], writes=[sgb])
                    S_.op("act", lambda sg=sg: nc.scalar.activation(out=sg[:], in_=sg[:], func=AF.Sigmoid),
                          reads=[sgb], writes=[sgb])
                    S_.op("dve", lambda lg_=lg_, top=top, sg=sg, tt=tt: nc.vector.scalar_tensor_tensor(
                        out=gate[:, tt, :], in0=lg_[:], scalar=top[:, 1:2], in1=sg[:], op0=ALU.is_ge, op1=ALU.mult),
                        reads=[lgb, topb, sgb], writes=[gateb])
            for e in range(ne):
                for gi, (f0, g) in enumerate(groups):
                    nfc = g // 128
                    if (not moe) and e == 0 and gi == 1 and m + 1 < nmac:
                        early(m + 1)
                    if wk + 1 < nmac * len(work):
                        wload(wk + 1)
                    Wi, Wib, Wo, Wob = wpre.pop(wk)
                    wk += 1
                    hT, hTb = hTr.next()
                    for ts in range(T // 512):
                        tsl = slice(ts * 512, (ts + 1) * 512)
                        xdeps = xTb[ts * 4:(ts + 1) * 4]
                        for fc in range(nfc):
                            pa, pab = cx.bank()
                            pbk, pbb = cx.bank()
                            for ab, (pp, ppb) in enumerate(((pa, pab), (pbk, pbb))):
                                for kc in range(8):
                                    S_.op("pe", lambda kc=kc, ab=ab, pp=pp, fc=fc, Wi=Wi: nc.tensor.matmul(
                                        pp[:], lhsT=Wi[:, kc, ab, fc * 128:(fc + 1) * 128], rhs=xT[:, kc, tsl],
                                        start=(kc == 0), stop=(kc == 7)), reads=[Wib] + xdeps, writes=[ppb],
                                        inc=(kc == 7))
                            sa, sab = sar.next()
                            S_.op("act", lambda sa=sa, pa=pa: nc.scalar.activation(out=sa[:], in_=pa[:],
                                                                                   func=AF.Silu),
                                  reads=[pab], writes=[sab])
                            S_.op("dve", lambda sa=sa, pbk=pbk, hT=hT, fc=fc: nc.vector.tensor_tensor(
                                out=hT[:, fc, tsl], in0=sa[:], in1=pbk[:], op=ALU.mult), reads=[sab, pbb],
                                writes=[hTb])
                    for tt in range(ntt):
                        for hh in range(2):
                            po, pob = cx.bank()
                            for fc in range(nfc):
                                S_.op("pe", lambda fc=fc, po=po, hT=hT, Wo=Wo, tt=tt, hh=hh: nc.tensor.matmul(
                                    po[:], lhsT=hT[:, fc, tt * 128:(tt + 1) * 128],
                                    rhs=Wo[:, fc, hh * 512:(hh + 1) * 512], start=(fc == 0), stop=(fc == nfc - 1)),
                                    reads=[hTb, Wob], writes=[pob], inc=(fc == nfc - 1))
                            asl = acc[:, tt, hh * 512:(hh + 1) * 512]
                            if moe and DBG_R >= 4:
                                S_.op("dve", lambda po=po, asl=asl, tt=tt, e=e: nc.vector.scalar_tensor_tensor(
                                    out=asl, in0=po[:], scalar=gate[:, tt, e:e + 1], in1=asl, op0=ALU.mult,
                                    op1=ALU.add), reads=[pob, gateb, accb[tt]], writes=[accb[tt]])
                            else:
                                S_.op("dve", lambda po=po, asl=asl: nc.vector.tensor_tensor(
                                    out=asl, in0=po[:], in1=asl, op=ALU.add), reads=[pob, accb[tt]],
                                    writes=[accb[tt]])
            for tt in range(ntt):
                st6, sb = st6r.next()
                mv, _ = mvr.next()
                rs, _ = rsr.next()
                _layernorm_store(cx, c, acc[:, tt, :], accb[tt], lng, lnb, pb_,
                                 dst_d[b, s0 + tt * 128:s0 + (tt + 1) * 128, :], dst_b, st6, mv, rs, sb)


def _nsa_pieces(w_q, w_kv):
    pieces = []
    for i in range(4):
        pieces.append(("q", w_q, i * 256, 256, False, True, i))
    pieces.append(("kc", w_kv, 0, 256, False, True, 0))
    pieces.append(("vc", w_kv, 256, 256, False, False, 0))
    for i in range(2):
        pieces.append(("ks", w_kv, 512 + i * 128, 128, True, True, i))
    for i in range(2):
        pieces.append(("kw", w_kv, 1024 + i * 128, 128, True, True, i))
    return pieces


def _nsa_prep_rings(nc, st):
    return (Ring(nc, st, "n0_Wr", [128, 8, 256], BF16, 2), Ring(nc, st, "n0_Wp", [128, 8, 256], BF16, 2),
            Ring(nc, st, "n0_Ws", [128, 8, 256], BF16, 2))


def _nsa_prep_piece(cx, pieces, pi, rings, wsw_d, wswb):
    nc, S_ = cx.nc, cx.S
    Wrr, Wpr, Wsr = rings
    kind, wsrc, c0, ncr, dup, rot, idx = pieces[pi]
    Wr, Wrb = Wrr.next()
    _load_w3(cx, Wr[:, :, 0:ncr], wsrc, 0, 8, c0, c0 + ncr, Wrb)
    if dup:
        Wp, Wpb = Wpr.next()
        for gg in range(2):
            S_.op("pool", lambda Wp=Wp, Wr=Wr, gg=gg: nc.gpsimd.tensor_copy(
                out=Wp[:, :, gg * 128:(gg + 1) * 128].rearrange("p k (t d) -> p k t d", t=2),
                in_=Wr[:, :, gg * 64:(gg + 1) * 64].unsqueeze(2).broadcast_to([128, 8, 2, 64])),
                reads=[Wrb], writes=[Wpb])
    else:
        Wp, Wpb = Wr, Wrb
    S_.dma("sp", wsw_d[2 * pi], Wp[:], reads=[Wpb], writes=[wswb], add=True)
    if rot:
        Ws, Wsb = Wsr.next()
        wp5 = Wp[:].rearrange("p k (h t d) -> p k h t d", t=2, d=32)
        ws5 = Ws[:].rearrange("p k (h t d) -> p k h t d", t=2, d=32)
        for k0 in (0, 4):
            S_.op("pool", lambda ws5=ws5, wp5=wp5, k0=k0: nc.gpsimd.tensor_copy(
                out=ws5[:, k0:k0 + 4, :, 0, :], in_=wp5[:, k0:k0 + 4, :, 1, :]), reads=[Wpb], writes=[Wsb])
            S_.op("pool", lambda ws5=ws5, wp5=wp5, k0=k0: nc.gpsimd.tensor_copy(
                out=ws5[:, k0:k0 + 4, :, 1, :], in_=wp5[:, k0:k0 + 4, :, 0, :]), reads=[Wpb], writes=[Wsb])
        S_.dma("sp", wsw_d[2 * pi + 1], Ws[:], reads=[Wsb], writes=[wswb], add=True)


SLOT_R = [0, 2, 1, 3]


def phase_nsa(cx, c, x2_d, x2_b, w_kv, ck_pe, ck_w1, ck_w2, cv_pe, cv_w1, cv_w2, w_q, w_out, ln_g, ln_b,
              cos_d, sin_d, tab_b, x3_d, x3_b, wsw_d, dbg_kc=None, dbg_vc=None, dbg_b=None, wswb=None):
    nc, S_ = cx.nc, cx.S
    ident = c["ident"]
    idb = c["ident_b"]
    with ExitStack() as st:
        QT2 = st.enter_context(nc.sbuf_tensor(_u("n_QT2"), [128, 8, S], BF16))
        KS = st.enter_context(nc.sbuf_tensor(_u("n_KS"), [128, NG, S], BF16))
        KW = st.enter_context(nc.sbuf_tensor(_u("n_KW"), [128, NG, S], BF16))
        Vaug = st.enter_context(nc.sbuf_tensor(_u("n_Vaug"), [128, NT, 2, NG, 65], BF16))
        G3 = st.enter_context(nc.sbuf_tensor(_u("n_G3"), [128, NT, NG, 4, 3], F32))
        kcT = st.enter_context(nc.sbuf_tensor(_u("n_kcT"), [128, NG, 128], BF16))
        vcaug = st.enter_context(nc.sbuf_tensor(_u("n_vcaug"), [128, NG, 97], BF16))
        Emat = st.enter_context(nc.sbuf_tensor(_u("n_E"), [96, S], BF16))
        tri4 = st.enter_context(nc.sbuf_tensor(_u("n_tri4"), [128, 2, 128], BF16))
        tri4b = st.enter_context(nc.sbuf_tensor(_u("n_tri4b"), [128, 2, 128], BF16))
        cmask = st.enter_context(nc.sbuf_tensor(_u("n_cmask"), [128, NT, 128], BF16))
        M1 = st.enter_context(nc.sbuf_tensor(_u("n_M1"), [128, NT, 32], F32))
        M2 = st.enter_context(nc.sbuf_tensor(_u("n_M2"), [128, NT, 32], F32))
        VAL = st.enter_context(nc.sbuf_tensor(_u("n_VAL"), [128, NT, 32], F32))
        ovt = st.enter_context(nc.sbuf_tensor(_u("n_ov"), [128, 32], F32))
        QTb, KSb, KWb, Vb, G3b, kcTb, vcb, cb, Wob, pb_ = [Buf() for _ in range(10)]
        P = "pool"
        S_.op(P, lambda: nc.gpsimd.memset(Emat[:], 1.0), writes=[cb])
        for p0 in (0, 64):
            S_.op(P, lambda p0=p0: nc.gpsimd.affine_select(out=Emat[p0:p0 + 32], in_=Emat[p0:p0 + 32], pattern=[[1, S]],
                                                           compare_op=ALU.is_ge, fill=cx.reg(0.0), base=0,
                                                           channel_multiplier=-64), reads=[cb], writes=[cb])
            S_.op(P, lambda p0=p0: nc.gpsimd.affine_select(out=Emat[p0:p0 + 32], in_=Emat[p0:p0 + 32], pattern=[[-1, S]],
                                                           compare_op=ALU.is_ge, fill=cx.reg(0.0), base=63,
                                                           channel_multiplier=64), reads=[cb], writes=[cb])
        S_.op(P, lambda: nc.gpsimd.memset(tri4[:], 0.0), writes=[cb])
        S_.op(P, lambda: nc.gpsimd.affine_select(out=tri4[:], in_=tri4[:], pattern=[[0, 2], [1, 128]],
                                                 compare_op=ALU.is_ge, fill=cx.reg(NEG), base=0, channel_multiplier=-1),
              reads=[cb], writes=[cb])
        S_.op(P, lambda: nc.gpsimd.memset(tri4b[:], 0.0), writes=[cb])
        S_.op(P, lambda: nc.gpsimd.affine_select(out=tri4b[:], in_=tri4b[:], pattern=[[0, 2], [-1, 128]],
                                                 compare_op=ALU.is_ge, fill=cx.reg(NEG), base=-1, channel_multiplier=1),
              reads=[cb], writes=[cb])
        S_.op(P, lambda: nc.gpsimd.memset(cmask[:], 1.0), writes=[cb])
        S_.op(P, lambda: nc.gpsimd.affine_select(out=cmask[:], in_=cmask[:], pattern=[[128, NT], [1, 128]],
                                                 compare_op=ALU.is_ge, fill=cx.reg(0.0), base=-31, channel_multiplier=-16),
              reads=[cb], writes=[cb])
        S_.op(P, lambda: nc.gpsimd.memset(VAL[:], 1.0), writes=[cb])
        S_.op(P, lambda: nc.gpsimd.memset(M1[:], 0.0), writes=[cb])
        for half in range(2):
            ps = slice(half * 64, (half + 1) * 64)
            S_.op(P, lambda ps=ps, half=half: nc.gpsimd.affine_select(
                out=VAL[ps], in_=VAL[ps], pattern=[[2, NT], [-1, 32]], compare_op=ALU.is_ge, fill=cx.reg(0.0), base=half,
                channel_multiplier=0), reads=[cb], writes=[cb])
            for off in (0, 1):
                S_.op(P, lambda ps=ps, half=half, off=off: nc.gpsimd.affine_select(
                    out=M1[ps], in_=M1[ps], pattern=[[-2, NT], [1, 32]], compare_op=ALU.not_equal, fill=cx.reg(1.0),
                    base=-half + off, channel_multiplier=0), reads=[cb], writes=[cb])
        S_.op(P, lambda: nc.gpsimd.memset(M1[:, :, 0:1], 1.0), reads=[cb], writes=[cb])
        S_.op(P, lambda: nc.gpsimd.tensor_tensor(out=M1[:], in0=M1[:], in1=VAL[:], op=ALU.mult), reads=[cb], writes=[cb])
        S_.op("dve", lambda: nc.vector.tensor_scalar(out=M2[:], in0=VAL[:], scalar1=-1.0, scalar2=1e30, op0=ALU.add,
                                                     op1=ALU.mult), reads=[cb], writes=[cb])
        S_.op("dve", lambda: nc.vector.scalar_tensor_tensor(out=M2[:], in0=M1[:], scalar=1e9, in1=M2[:], op0=ALU.mult,
                                                            op1=ALU.add), reads=[cb], writes=[cb])
        S_.op("dve", lambda: nc.vector.tensor_tensor(out=M1[:], in0=VAL[:], in1=M1[:], op=ALU.subtract), reads=[cb],
              writes=[cb])
        S_.op(P, lambda: nc.gpsimd.iota(ovt[:], pattern=[[-64, 32]], base=0, channel_multiplier=16,
                                        allow_small_or_imprecise_dtypes=True), reads=[cb], writes=[cb])
        with ExitStack() as st2:
            lo = st2.enter_context(nc.sbuf_tensor(_u("n_lo"), [128, 32], F32))
            S_.op("dve", lambda: nc.vector.tensor_scalar(out=lo[:], in0=ovt[:], scalar1=0.0, scalar2=None, op0=ALU.max),
                  reads=[cb], writes=[cb])
            S_.op("dve", lambda: nc.vector.tensor_scalar(out=ovt[:], in0=ovt[:], scalar1=32.0, scalar2=64.0,
                                                         op0=ALU.add, op1=ALU.min), reads=[cb], writes=[cb])
            S_.op("dve", lambda: nc.vector.tensor_tensor(out=ovt[:], in0=ovt[:], in1=lo[:], op=ALU.subtract),
                  reads=[cb], writes=[cb])
            S_.op("dve", lambda: nc.vector.tensor_scalar(out=ovt[:], in0=ovt[:], scalar1=0.0, scalar2=1.0 / 16.0,
                                                         op0=ALU.max, op1=ALU.mult), reads=[cb], writes=[cb])
            for g in range(NG):
                S_.op("dve", lambda g=g: nc.vector.tensor_copy(out=vcaug[:, g, 64:96], in_=ovt[:]), reads=[cb],
                      writes=[vcb])
            S_.op("dve", lambda: nc.vector.memset(vcaug[:, :, 96:97], 1.0), reads=[cb], writes=[vcb])
            S_.op("dve", lambda: nc.vector.memset(Vaug[:, :, :, :, 64:65], 1.0), writes=[Vb])
            S_.fence()

        pieces = _nsa_pieces(w_q, w_kv)
        if wswb is None:
            wswb = Buf()
            with ExitStack() as s0:
                rings = _nsa_prep_rings(nc, s0)
                for pi in range(len(pieces)):
                    _nsa_prep_piece(cx, pieces, pi, rings, wsw_d, wswb)
                S_.fence()

        for b in range(NB):
            with ExitStack() as sa:
                kcmpT = sa.enter_context(nc.sbuf_tensor(_u("n_kcmpT"), [128, 2, S], BF16))
                vcmpT = sa.enter_context(nc.sbuf_tensor(_u("n_vcmpT"), [128, 2, S], BF16))
                kcb, vcmb = Buf(), Buf()
                with ExitStack() as s1:
                    xtok = Ring(nc, s1, "n1_xtok", [128, D], F32, 2)
                    xbr = Ring(nc, s1, "n1_xb", [128, D], BF16, 2)
                    xTr = Ring(nc, s1, "n1_xT", [128, 8, 512], BF16, 2)
                    csr = Ring(nc, s1, "n1_cs", [128, 2, 512], F32, 1)
                    Wpr = Ring(nc, s1, "n1_Wp", [128, 8, 256], BF16, 3)
                    Wsr = Ring(nc, s1, "n1_Ws", [128, 8, 256], BF16, 3)
                    Wvr = Ring(nc, s1, "n1_Wv", [128, 8, 560], BF16, 1)
                    t1r = Ring(nc, s1, "n1_t1", [128, 512], F32, 2)
                    t2r = Ring(nc, s1, "n1_t2", [128, 512], F32, 2)
                    for blk in range(S // 512):
                        tb0 = blk * 512
                        cs, csb = csr.next()
                        S_.dma("sp", cs[:, 0, :], cos_d[:, tb0:tb0 + 512], reads=[tab_b], writes=[csb])
                        S_.dma("sp", cs[:, 1, :], sin_d[:, tb0:tb0 + 512], reads=[tab_b], writes=[csb], add=True)
                        xT, xTb = xTr.next()
                        for tt in range(4):
                            xt, xtb = xtok.next()
                            S_.dma("sp", xt[:], x2_d[b, tb0 + tt * 128:tb0 + (tt + 1) * 128, :], reads=[x2_b],
                                   writes=[xtb])
                            xb, xbb = xbr.next()
                            S_.op("pool", lambda xb=xb, xt=xt: nc.gpsimd.tensor_copy(out=xb[:], in_=xt[:]),
                                  reads=[xtb], writes=[xbb])
                            _transpose_to(cx, c, xb, xbb, 8,
                                          lambda k0, k1, tt=tt, xT=xT: xT[:, k0:k1, tt * 128:(tt + 1) * 128], xTb)
                        for pi, (kind, wsrc, c0, ncr, dup, rot, idx) in enumerate(pieces):
                            Wp, Wpb = Wpr.next()
                            S_.dma("sp", Wp[:], wsw_d[2 * pi], reads=[wswb], writes=[Wpb])
                            if rot:
                                Ws, Wsb = Wsr.next()
                                S_.dma("sp", Ws[:], wsw_d[2 * pi + 1], reads=[wswb], writes=[Wsb])
                            for ch in range(2):
                                pa, pab = cx.bank()
                                for kc in range(8):
                                    S_.op("pe", lambda kc=kc, ch=ch, Wp=Wp, pa=pa, xT=xT: nc.tensor.matmul(
                                        pa[:], lhsT=Wp[:, kc, ch * 128:(ch + 1) * 128], rhs=xT[:, kc, :],
                                        start=(kc == 0), stop=(kc == 7)), reads=[Wpb, xTb], writes=[pab],
                                        inc=(kc == 7))
                                if kind == "q":
                                    dst, dstb = QT2[:, idx * 2 + ch, tb0:tb0 + 512], QTb
                                elif kind == "kc":
                                    dst, dstb = kcmpT[:, ch, tb0:tb0 + 512], kcb
                                elif kind == "vc":
                                    dst, dstb = vcmpT[:, ch, tb0:tb0 + 512], vcmb
                                elif kind == "ks":
                                    dst, dstb = KS[:, idx * 2 + ch, tb0:tb0 + 512], KSb
                                else:
                                    dst, dstb = KW[:, idx * 2 + ch, tb0:tb0 + 512], KWb
                                if not rot:
                                    S_.op("act", lambda dst=dst, pa=pa: nc.scalar.copy(out=dst, in_=pa[:]),
                                          reads=[pab], writes=[dstb])
                                    continue
                                pb2, pbb2 = cx.bank()
                                for kc in range(8):
                                    S_.op("pe", lambda kc=kc, ch=ch, Ws=Ws, pb2=pb2, xT=xT: nc.tensor.matmul(
                                        pb2[:], lhsT=Ws[:, kc, ch * 128:(ch + 1) * 128], rhs=xT[:, kc, :],
                                        start=(kc == 0), stop=(kc == 7)), reads=[Wsb, xTb], writes=[pbb2],
                                        inc=(kc == 7))
                                t1, t1b = t1r.next()
                                t2, t2b = t2r.next()
                                S_.op("dve", lambda t1=t1, pa=pa, cs=cs: nc.vector.tensor_tensor(
                                    out=t1[:], in0=pa[:], in1=cs[:, 0, :], op=ALU.mult), reads=[pab, csb],
                                    writes=[t1b])
                                S_.op("dve", lambda t2=t2, pb2=pb2, cs=cs: nc.vector.tensor_tensor(
                                    out=t2[:], in0=pb2[:], in1=cs[:, 1, :], op=ALU.mult), reads=[pbb2, csb],
                                    writes=[t2b])
                                S_.op("pool", lambda dst=dst, t1=t1, t2=t2: nc.gpsimd.tensor_tensor(
                                    out=dst, in0=t1[:], in1=t2[:], op=ALU.add), reads=[t1b, t2b], writes=[dstb])
                        Wv, Wvb = Wvr.next()
                        _load_w3(cx, Wv[:, :, 0:256], w_kv, 0, 8, 768, 1024, Wvb)
                        _load_w3(cx, Wv[:, :, 256:512], w_kv, 0, 8, 1280, 1536, Wvb, add=True)
                        _load_w3(cx, Wv[:, :, 512:560], w_q, 0, 8, 1024, 1072, Wvb, add=True)
                        for tt in range(4):
                            ti = blk * 4 + tt
                            pv, pvb = cx.bank()
                            pg, pgb = cx.bank()
                            for kc in range(8):
                                S_.op("pe", lambda kc=kc, tt=tt, pv=pv, xT=xT, Wv=Wv: nc.tensor.matmul(
                                    pv[:], lhsT=xT[:, kc, tt * 128:(tt + 1) * 128], rhs=Wv[:, kc, 0:512],
                                    start=(kc == 0), stop=(kc == 7)), reads=[Wvb, xTb], writes=[pvb], inc=(kc == 7))
                            for kc in range(8):
                                S_.op("pe", lambda kc=kc, tt=tt, pg=pg, xT=xT, Wv=Wv: nc.tensor.matmul(
                                    pg[:, 0:48], lhsT=xT[:, kc, tt * 128:(tt + 1) * 128], rhs=Wv[:, kc, 512:560],
                                    start=(kc == 0), stop=(kc == 7)), reads=[Wvb, xTb], writes=[pgb], inc=(kc == 7))
                            S_.op("act", lambda ti=ti, pv=pv: nc.scalar.copy(
                                out=Vaug[:, ti, :, :, 0:64],
                                in_=pv[:].rearrange("p (a g d) -> p a g d", a=2, g=NG)), reads=[pvb], writes=[Vb])
                            for gg in range(NG):
                                S_.op("act", lambda ti=ti, pg=pg, gg=gg: nc.scalar.activation(
                                    out=G3[:, ti, gg].rearrange("p (a b) c -> p a b c", a=2),
                                    in_=pg[:, gg * 12:(gg + 1) * 12].rearrange("p (b a c) -> p a b c", b=2, a=2),
                                    func=AF.Sigmoid), reads=[pgb], writes=[G3b])
                S_.fence()
                with ExitStack() as s2:
                    W1 = [s2.enter_context(nc.sbuf_tensor(_u("n2_W1_%d" % i), [128, 32, 256], BF16)) for i in range(2)]
                    W2 = [s2.enter_context(nc.sbuf_tensor(_u("n2_W2_%d" % i), [128, 2, 128], BF16)) for i in range(2)]
                    pet = [s2.enter_context(nc.sbuf_tensor(_u("n2_pe_%d" % i), [32, 64], BF16)) for i in range(2)]
                    peT = [s2.enter_context(nc.sbuf_tensor(_u("n2_peT_%d" % i), [128, 32], BF16)) for i in range(2)]
                    bias = [s2.enter_context(nc.sbuf_tensor(_u("n2_bias_%d" % i), [128, 2], F32)) for i in range(2)]
                    wb2 = [Buf(), Buf()]
                    hx = Ring(nc, s2, "n2_hx", [128, 2, 127], F32, 2)
                    hu = Ring(nc, s2, "n2_hu", [128, 2, 127], F32, 2)
                    hs = Ring(nc, s2, "n2_hs", [128, 2, 127], F32, 2)
                    hb = Ring(nc, s2, "n2_hb", [128, 2, 127], BF16, 2)
                    for i, (pe_, w1_, w2_) in enumerate(((ck_pe, ck_w1, ck_w2), (cv_pe, cv_w1, cv_w2))):
                        src = w1_.rearrange("(l d) h -> d l h", d=64)
                        S_.dma("pool", W1[i][0:64], src, writes=[wb2[i]])
                        S_.dma("pool", W1[i][64:128], src, writes=[wb2[i]], add=True)
                        src2 = w2_.rearrange("(k p) d -> p k d", p=128)
                        S_.dma("pool", W2[i][:, :, 0:64], src2, writes=[wb2[i]], add=True)
                        S_.dma("pool", W2[i][:, :, 64:128], src2, writes=[wb2[i]], add=True)
                        S_.dma("pool", pet[i][:], pe_, writes=[wb2[i]], add=True)
                        bk, bb = cx.bank()
                        pb = bk.bitcast(BF16)
                        S_.op("pe", lambda i=i, pb=pb: nc.tensor.transpose(out=pb[0:64, 0:32], in_=pet[i][:],
                                                                           identity=ident[0:32, 0:32]),
                              reads=[wb2[i], idb], writes=[bb])
                        S_.op("act", lambda i=i, pb=pb: nc.scalar.copy(out=peT[i][0:64, :], in_=pb[0:64, 0:32]),
                              reads=[bb], writes=[wb2[i]])
                        bk, bb = cx.bank()
                        for hc in range(2):
                            for l in range(32):
                                S_.op("pe", lambda i=i, hc=hc, l=l, bk=bk: nc.tensor.matmul(
                                    bk[:, hc:hc + 1], lhsT=W1[i][0:64, l, hc * 128:(hc + 1) * 128],
                                    rhs=peT[i][0:64, l:l + 1], start=(l == 0), stop=(l == 31)),
                                    reads=[wb2[i]], writes=[bb], inc=(l == 31))
                        S_.op("act", lambda i=i, bk=bk: nc.scalar.copy(out=bias[i][:], in_=bk[:, 0:2]), reads=[bb],
                              writes=[wb2[i]])
                    for g in range(NG):
                        p0 = (g % 2) * 64
                        cc = g // 2
                        for i, (srcT, srcb) in enumerate(((kcmpT, kcb), (vcmpT, vcmb))):
                            bk, bb = cx.bank()
                            bv = bk[:, 0:256].rearrange("p (a n) -> p a n", a=2)
                            for hc in range(2):
                                for l in range(32):
                                    S_.op("pe", lambda i=i, hc=hc, l=l, bv=bv, srcT=srcT: nc.tensor.matmul(
                                        bv[:, hc, 0:127], lhsT=W1[i][p0:p0 + 64, l, hc * 128:(hc + 1) * 128],
                                        rhs=srcT[p0:p0 + 64, cc, l:l + 16 * 126 + 1:16], start=(l == 0),
                                        stop=(l == 31)), reads=[wb2[i], srcb], writes=[bb], inc=(l == 31))
                            x_, xb_ = hx.next()
                            u_, ub_ = hu.next()
                            s_, sb_ = hs.next()
                            h_, hb_ = hb.next()
                            for hc in range(2):
                                S_.op("act", lambda hc=hc, x_=x_, bv=bv, i=i: nc.scalar.activation(
                                    out=x_[:, hc, :], in_=bv[:, hc, 0:127], func=AF.Identity,
                                    bias=bias[i][:, hc:hc + 1], scale=1.0), reads=[bb, wb2[i]], writes=[xb_])
                            S_.op("dve", lambda x_=x_, u_=u_: nc.vector.tensor_tensor(out=u_[:], in0=x_[:], in1=x_[:],
                                                                                      op=ALU.mult),
                                  reads=[xb_], writes=[ub_])
                            S_.op("dve", lambda u_=u_: nc.vector.tensor_scalar(out=u_[:], in0=u_[:], scalar1=0.044715,
                                                                               scalar2=1.0, op0=ALU.mult,
                                                                               op1=ALU.add), reads=[ub_], writes=[ub_])
                            S_.op("dve", lambda x_=x_, u_=u_: nc.vector.tensor_tensor(out=u_[:], in0=u_[:], in1=x_[:],
                                                                                      op=ALU.mult),
                                  reads=[xb_, ub_], writes=[ub_])
                            S_.op("act", lambda u_=u_, s_=s_: nc.scalar.activation(
                                out=s_[:], in_=u_[:], func=AF.Sigmoid, scale=2.0 * math.sqrt(2.0 / math.pi)),
                                reads=[ub_], writes=[sb_])
                            S_.op("dve", lambda x_=x_, s_=s_, h_=h_: nc.vector.tensor_tensor(
                                out=h_[:], in0=x_[:], in1=s_[:], op=ALU.mult), reads=[xb_, sb_], writes=[hb_])
                            bk2, bb2 = cx.bank()
                            if i == 0:
                                for hc in range(2):
                                    S_.op("pe", lambda hc=hc, bk2=bk2, h_=h_: nc.tensor.matmul(
                                        bk2[:, 0:127], lhsT=W2[0][:, hc, :], rhs=h_[:, hc, :], start=(hc == 0),
                                        stop=(hc == 1)), reads=[wb2[0], hb_], writes=[bb2], inc=(hc == 1))
                                S_.op("act", lambda g=g, bk2=bk2: nc.scalar.copy(out=kcT[:, g, 0:127],
                                                                                 in_=bk2[:, 0:127]),
                                      reads=[bb2], writes=[kcTb])
                            else:
                                for hc in range(2):
                                    S_.op("pe", lambda hc=hc, bk2=bk2, h_=h_: nc.tensor.matmul(
                                        bk2[0:127, 0:64], lhsT=h_[:, hc, :], rhs=W2[1][:, hc, 0:64], start=(hc == 0),
                                        stop=(hc == 1)), reads=[wb2[1], hb_], writes=[bb2], inc=(hc == 1))
                                S_.op("act", lambda g=g, bk2=bk2: nc.scalar.copy(out=vcaug[0:127, g, 0:64],
                                                                                 in_=bk2[0:127, 0:64]),
                                      reads=[bb2], writes=[vcb])
                    if dbg_kc is not None:
                        S_.dma("sp", dbg_kc[b], kcT[:], reads=[kcTb], writes=[dbg_b], add=True)
                        S_.dma("sp", dbg_vc[b], vcaug[:], reads=[vcb], writes=[dbg_b], add=True)
                S_.fence()
            if os.environ.get("DBG_NOATT"):
                continue
            with ExitStack() as sb_:
                Wo = sb_.enter_context(nc.sbuf_tensor(_u("n_Wo"), [128, 8, D], BF16))
                lng = sb_.enter_context(nc.sbuf_tensor(_u("n_lng"), [128, D], F32))
                lnb = sb_.enter_context(nc.sbuf_tensor(_u("n_lnb"), [128, D], F32))
                Wob, pb_ = Buf(), Buf()
                S_.dma("sp", lng[:], ln_g.partition_broadcast(128), writes=[pb_])
                S_.dma("sp", lnb[:], ln_b.partition_broadcast(128), writes=[pb_], add=True)
                _load_w3(cx, Wo[:, :, :], w_out, 0, 8, 0, D, Wob)
                xtok = Ring(nc, sb_, "nb_xtok", [128, D], F32, 3)
                Or = Ring(nc, sb_, "nb_O", [128, D], F32, 4)
                Obr = Ring(nc, sb_, "nb_Ob", [128, D], BF16, 2)
                OTr = Ring(nc, sb_, "nb_OT", [128, 8, 128], BF16, 2)
                zr = Ring(nc, sb_, "nb_z", [128, D], F32, 2)
                ecr = Ring(nc, sb_, "nb_ec", [128, 4, 128], BF16, 3)
                basr = Ring(nc, sb_, "nb_bas", [128, 4 * 97], F32, 3)
                pTr = Ring(nc, sb_, "nb_pT", [128, 4, 128], BF16, 5)
                rzr = Ring(nc, sb_, "nb_rz", [128, 4], F32, 10)
                facr = Ring(nc, sb_, "nb_fac", [128, 4], F32, 10)
                impr = Ring(nc, sb_, "nb_imp", [128, 32], F32, 3)
                topr = Ring(nc, sb_, "nb_top", [128, 8], F32, 2)
                selr = Ring(nc, sb_, "nb_sel", [128, 32], F32, 2)
                selbr = Ring(nc, sb_, "nb_selb", [128, 96], BF16, 2)
                sT4r = Ring(nc, sb_, "nb_sT4", [96, 2, 128], BF16, 4)
                st6r = Ring(nc, sb_, "nb_st6", [128, 2, 6], F32, 2)
                mvr = Ring(nc, sb_, "nb_mv", [128, 2], F32, 2)
                rsr = Ring(nc, sb_, "nb_rs", [128, 1], F32, 2)
                old_rot = cx.rot
                cx.rot = [2, 3]
                cx.bi = -1
                cx.pi = -1
                it = 0
                pre = {}

                def xload(qt):
                    xt, xtb = xtok.next()
                    S_.dma("sp", xt[:], x2_d[b, qt * 128:(qt + 1) * 128, :], reads=[x2_b], writes=[xtb])
                    pre[qt] = (xt, xtb)

                accs = ((cx.banks[0], cx.bbuf[0]), (cx.banks[1], cx.bbuf[1]))

                def scores(bks, rq, g, Kt, Kb, ksl, nk, last):
                    for hb2 in range(2):
                        p0 = hb2 * 64
                        bk, bb = bks[hb2]
                        S_.op("pe", lambda hb2=hb2, p0=p0, bk=bk: nc.tensor.matmul(
                            bk[0:nk, 0:256], lhsT=Kt[p0:p0 + 64, g, ksl], rhs=rq[hb2],
                            start=True, stop=last), reads=[Kb, QTb], writes=[bb], inc=last)

                def bias_mm(bks, lhs_fn, rhs_fn, rd, last):
                    for hb2 in range(2):
                        bk, bb = bks[hb2]
                        S_.op("pe", lambda hb2=hb2, bk=bk: nc.tensor.matmul(
                            bk[:, 0:256], lhsT=lhs_fn(hb2), rhs=rhs_fn(hb2), start=False, stop=last),
                            reads=rd, writes=[bb], inc=last)

                def exp_to(dst, dstb, bks, nk):
                    dbl = bks[2]
                    S_.op("act", lambda: nc.scalar.activation(
                        out=dst[0:nk].rearrange("p (b s) i -> p b (s i)", b=2),
                        in_=dbl[0:nk, :].rearrange("p (b c) -> p b c", b=2)[:, :, 0:256], func=AF.Exp, scale=0.125),
                        reads=[bks[0][1], bks[1][1]], writes=[dstb])

                def combine(qt, g, Og, Ob, acc, accb, ncol, br, first):
                    av = acc[:, 0:4 * ncol].rearrange("p (s c) -> p s c", s=4)
                    rz, rzb = rzr.next()
                    fac, facb = facr.next()
                    S_.op("dve", lambda: nc.vector.tensor_scalar(out=rz[:], in0=av[:, :, ncol - 1],
                                                                 scalar1=1e-30, scalar2=None, op0=ALU.max),
                          reads=[accb], writes=[rzb])
                    S_.op("dve", lambda: nc.vector.reciprocal(out=rz[:], in_=rz[:]), reads=[rzb], writes=[rzb])
                    S_.op("dve", lambda: nc.vector.tensor_tensor(out=fac[:], in0=rz[:],
                                                                 in1=G3[:, qt, g, :, br], op=ALU.mult),
                          reads=[rzb, G3b], writes=[facb])
                    for s_i in range(4):
                        osl = Og[:, s_i // 2, s_i % 2, :]
                        if first:
                            S_.op("dve", lambda s_i=s_i, osl=osl: nc.vector.tensor_scalar(
                                out=osl, in0=av[:, s_i, 0:64], scalar1=fac[:, s_i:s_i + 1], scalar2=None,
                                op0=ALU.mult), reads=[accb, facb], writes=[Ob])
                        else:
                            S_.op("dve", lambda s_i=s_i, osl=osl: nc.vector.scalar_tensor_tensor(
                                out=osl, in0=av[:, s_i, 0:64], scalar=fac[:, s_i:s_i + 1], in1=osl,
                                op0=ALU.mult, op1=ALU.add), reads=[accb, facb, Ob], writes=[Ob])
                    return rz, rzb, av

                def stageA1(qt, g, O, Ob):
                    qsl = slice(qt * 128, (qt + 1) * 128)
                    rq = [QT2[0:64, 2 * g:2 * g + 2, qsl], QT2[64:128, 2 * g:2 * g + 2, qsl]]
                    Og = O[:, g * 256:(g + 1) * 256].rearrange("p (a b d) -> p b a d", a=2, b=2)
                    bks = cx.bank_pair()
                    scores(bks, rq, g, kcT, kcTb, slice(0, 127), 127, True)
                    ec, ecb = ecr.next()
                    exp_to(ec, ecb, bks, 127)
                    S_.op("dve", lambda: nc.vector.tensor_tensor(
                        out=ec[0:127], in0=ec[0:127],
                        in1=cmask[0:127, qt:qt + 1, :].broadcast_to([127, 4, 128]), op=ALU.mult),
                        reads=[ecb, cb], writes=[ecb])
                    return dict(qt=qt, g=g, rq=rq, Og=Og, Ob=Ob, ec=ec, ecb=ecb)

                def stageA2(stt):
                    qt, g, Og, Ob, ec, ecb = (stt[k_] for k_ in ("qt", "g", "Og", "Ob", "ec", "ecb"))
                    pr2 = cx.bank_pair()
                    ba, bab = pr2[0]
                    for s_i in range(4):
                        S_.op("pe", lambda s_i=s_i: nc.tensor.matmul(
                            ba[:, s_i * 97:(s_i + 1) * 97], lhsT=ec[0:127, s_i, :], rhs=vcaug[0:127, g, :],
                            start=True, stop=True), reads=[ecb, vcb], writes=[bab], inc=(s_i == 3))
                    bas, basb = basr.next()
                    S_.op("dve", lambda: nc.vector.tensor_copy(out=bas[:], in_=ba[:, 0:4 * 97]), reads=[bab],
                          writes=[basb])
                    ba, bab = bas, basb
                    rz, rzb, av = combine(qt, g, Og, Ob, ba, bab, 97, 0, True)
                    imp, impb = impr.next()
                    for s_i in range(4):
                        if s_i == 0:
                            S_.op("dve", lambda: nc.vector.tensor_scalar(
                                out=imp[:], in0=av[:, 0, 64:96], scalar1=rz[:, 0:1], scalar2=None, op0=ALU.mult),
                                reads=[bab, rzb], writes=[impb])
                        else:
                            S_.op("dve", lambda s_i=s_i: nc.vector.scalar_tensor_tensor(
                                out=imp[:], in0=av[:, s_i, 64:96], scalar=rz[:, s_i:s_i + 1], in1=imp[:],
                                op0=ALU.mult, op1=ALU.add), reads=[bab, rzb, impb], writes=[impb])
                    S_.op("dve", lambda: nc.vector.tensor_tensor(out=imp[:], in0=imp[:], in1=M1[:, qt, :],
                                                                 op=ALU.mult), reads=[impb, cb], writes=[impb])
                    S_.op("dve", lambda: nc.vector.tensor_tensor(out=imp[:], in0=imp[:], in1=M2[:, qt, :],
                                                                 op=ALU.add), reads=[impb, cb], writes=[impb])
                    top, topb = topr.next()
                    S_.op("dve", lambda: nc.vector.max(out=top[:], in_=imp[:]), reads=[impb], writes=[topb])
                    sel, selb_ = selr.next()
                    S_.op("dve", lambda: nc.vector.scalar_tensor_tensor(
                        out=sel[:], in0=imp[:], scalar=top[:, 7:8], in1=VAL[:, qt, :], op0=ALU.is_ge,
                        op1=ALU.mult), reads=[impb, topb, cb], writes=[selb_])
                    selbf, selbfb = selbr.next()
                    S_.op("pool", lambda: nc.gpsimd.memset(selbf[:, 32:64], 0.0), writes=[selbfb])
                    for c0 in (0, 64):
                        S_.op("dve", lambda c0=c0: nc.vector.tensor_scalar(
                            out=selbf[:, c0:c0 + 32], in0=sel[:], scalar1=-1.0, scalar2=-NEG, op0=ALU.add,
                            op1=ALU.mult), reads=[selb_], writes=[selbfb])
                    stt["selbf"], stt["selbfb"] = selbf, selbfb

                def stageA3(stt):
                    selbf, selbfb = stt["selbf"], stt["selbfb"]
                    pr2 = cx.bank_pair()
                    bt, btb = pr2[0]
                    ptb = bt.bitcast(BF16)
                    S_.op("pe", lambda: nc.tensor.transpose(out=ptb[0:96, 0:128], in_=selbf[:], identity=ident[:]),
                          reads=[selbfb, idb], writes=[btb])
                    sT4, sT4b = sT4r.next()
                    S_.op("act", lambda: nc.scalar.copy(
                        out=sT4[:], in_=ptb[0:96, 0:128].unsqueeze(1).broadcast_to([96, 2, 128])),
                        reads=[btb], writes=[sT4b])
                    stt["sT4"], stt["sT4b"] = sT4, sT4b

                def stageB(stt, hooks):
                    qt, g, rq, Og, Ob, sT4, sT4b = (stt[k] for k in ("qt", "g", "rq", "Og", "Ob", "sT4", "sT4b"))
                    kts_w = [kt for kt in range(qt - 4, qt + 1) if kt >= 0]
                    steps = [("w", kt) for kt in kts_w] + [("s", kt) for kt in range(qt + 1)]

                    def emit_scores(step):
                        kind, kt = step
                        ksl = slice(kt * 128, (kt + 1) * 128)
                        bks = cx.bank_pair()
                        if kind == "w":
                            edge = (kt == qt) or (kt == qt - 4)
                            scores(bks, rq, g, KW, KWb, ksl, 128, not edge)
                            if edge:
                                tri = tri4 if kt == qt else tri4b
                                bias_mm(bks, lambda hb2: ident[:],
                                        lambda hb2, tri=tri: tri[:].rearrange("p s i -> p (s i)"), [cb, idb], True)
                        else:
                            scores(bks, rq, g, KS, KSb, ksl, 128, False)
                            bias_mm(bks, lambda hb2, ksl=ksl: Emat[hb2 * 64:hb2 * 64 + 32, ksl],
                                    lambda hb2: sT4[hb2 * 64:hb2 * 64 + 32].rearrange("p s i -> p (s i)"),
                                    [cb, sT4b], kt != qt)
                            if kt == qt:
                                bias_mm(bks, lambda hb2: ident[:], lambda hb2: tri4[:].rearrange("p s i -> p (s i)"),
                                        [cb, idb], True)
                        return bks

                    def emit_rest(step, bks):
                        kind, kt = step
                        acc, accb = accs[1] if kind == "w" else accs[0]
                        vi = 1 if kind == "w" else 0
                        first_kt = kts_w[0] if kind == "w" else 0
                        pT, pTb = pTr.next()
                        exp_to(pT, pTb, bks, 128)
                        for s_i in range(4):
                            S_.op("pe", lambda s_i=s_i, pT=pT, kt=kt, acc=acc: nc.tensor.matmul(
                                acc[:, s_i * 65:(s_i + 1) * 65], lhsT=pT[:, s_i, :], rhs=Vaug[:, kt, vi, g, :],
                                start=(kt == first_kt and s_i == 0), stop=(kt == qt), skip_group_check=True),
                                reads=[pTb, Vb], writes=[accb], inc=(s_i == 3))
                        if kt == qt:
                            combine(qt, g, Og, Ob, acc, accb, 65, 2 if kind == "w" else 1, False)

                    LOOK = 1
                    n = len(steps)
                    if len(hooks) == 4:
                        cand = [0, 1, max(2, n // 3), max(3, (2 * n) // 3)]
                    else:
                        cand = [0, max(1, n // 3), max(2, (2 * n) // 3)]
                    for ci in range(1, len(cand)):
                        cand[ci] = max(cand[ci], cand[ci - 1] + 1)
                    hookpos = {p: hi for hi, p in enumerate(cand)}
                    done = 0
                    pend = [emit_scores(steps[i]) for i in range(min(LOOK, n))]
                    for i, step in enumerate(steps):
                        if i + LOOK < n:
                            pend.append(emit_scores(steps[i + LOOK]))
                        emit_rest(step, pend.pop(0))
                        if i in hookpos and hookpos[i] == done and done < len(hooks):
                            hooks[done]()
                            done += 1
                    while done < len(hooks):
                        hooks[done]()
                        done += 1

                def tail(qt, O, Ob, xt, xtb):
                    cx.pi = (cx.pi + 1) % len(cx.prot)
                    cx.rot = [2 * cx.prot[cx.pi], 2 * cx.prot[cx.pi] + 1]
                    cx.bi = -1
                    Obf, Obfb = Obr.next()
                    S_.op("pool", lambda Obf=Obf, O=O: nc.gpsimd.tensor_copy(out=Obf[:], in_=O[:]), reads=[Ob],
                          writes=[Obfb])
                    OT, OTb = OTr.next()
                    _transpose_to(cx, c, Obf, Obfb, 8, lambda k0, k1, OT=OT: OT[:, k0:k1, :].rearrange("p a b -> p (a b)"),
                                  OTb)
                    z, zb = zr.next()
                    for hh in range(2):
                        bk, bb = cx.bank()
                        for kc in range(8):
                            S_.op("pe", lambda kc=kc, hh=hh, bk=bk, OT=OT: nc.tensor.matmul(
                                bk[:], lhsT=OT[:, kc, :], rhs=Wo[:, kc, hh * 512:(hh + 1) * 512], start=(kc == 0),
                                stop=(kc == 7)), reads=[Wob, OTb], writes=[bb], inc=(kc == 7))
                        S_.op("dve", lambda hh=hh, bk=bk, z=z, xt=xt: nc.vector.scalar_tensor_tensor(
                            out=z[:, hh * 512:(hh + 1) * 512], in0=xt[:, hh * 512:(hh + 1) * 512], scalar=DN_ALPHA,
                            in1=bk[:], op0=ALU.mult, op1=ALU.add), reads=[bb, xtb], writes=[zb])
                    st6, sb6 = st6r.next()
                    mv, _ = mvr.next()
                    rs, _ = rsr.next()
                    _layernorm_store(cx, c, z, zb, lng, lnb, pb_, x3_d[b, qt * 128:(qt + 1) * 128, :], x3_b, st6, mv,
                                     rs, sb6)

                items = [(qt, g) for qt in range(NT) for g in range(NG)]
                Otiles = {}

                def get_O(qt):
                    if qt not in Otiles:
                        Otiles[qt] = Or.next()
                    return Otiles[qt]

                xload(0)
                xload(1)
                ALOOK = 2

                def fullA(q2, g2):
                    stt = stageA1(q2, g2, *get_O(q2))
                    stageA2(stt)
                    stageA3(stt)
                    return stt

                stq = [fullA(q2, g2) for (q2, g2) in items[:ALOOK]]
                pending_tail = [None]
                for k, (qt, g) in enumerate(items):
                    hooks = []
                    if k + ALOOK < len(items):
                        q2, g2 = items[k + ALOOK]
                        box = {}

                        def h1(q2=q2, g2=g2, box=box):
                            box["s"] = stageA1(q2, g2, *get_O(q2))

                        def h2(box=box):
                            stageA2(box["s"])

                        def h3(box=box):
                            stageA3(box["s"])
                            stq.append(box["s"])

                        hooks = [h1, h2, h3]
                    if pending_tail[0] is not None:
                        if len(hooks) == 3:
                            hooks = [hooks[0], pending_tail[0], hooks[1], hooks[2]]
                        else:
                            hooks = hooks + [pending_tail[0]]
                        pending_tail[0] = None
                    stageB(stq.pop(0), hooks)
                    if g == NG - 1:
                        def tl(qt=qt):
                            if qt + 2 < NT:
                                xload(qt + 2)
                            xt, xtb = pre.pop(qt)
                            O, Ob = Otiles.pop(qt)
                            tail(qt, O, Ob, xt, xtb)
                        pending_tail[0] = tl
                if pending_tail[0] is not None:
                    pending_tail[0]()
                    pending_tail[0] = None
                cx.rot = old_rot
                cx.bi = -1
            S_.fence()


CAP = int(os.environ.get('DBG_CAP', 1152))
NSLOT = NE * CAP


def _dma_ind(S_, nc, breg, out, out_off, in_, in_off, reads, writes, add=True):
    i = S_.dnext
    S_.dnext = (S_.dnext + 1) % S_.NDMA
    if S_.dcnt[i] > 0:
        S_._wait("pool", (i, S_.dcnt[i]))
    S_._deps("pool", reads, writes, add=add)
    S_.dcnt[i] += 16
    ev = (i, S_.dcnt[i])
    nc.gpsimd.indirect_dma_start(out=out, out_offset=out_off, in_=in_, in_offset=in_off, bounds_check=breg,
                                 oob_is_err=False).then_inc(S_.dsem[i], 16)
    S_.ninstr += 1
    for b in writes:
        if add:
            b.w.append(ev)
        else:
            b.w = [ev]
        b.r = []
    for b in reads:
        b.r.append(ev)


def phase_moe_sparse(cx, c, x_d, x_b, w_ins, w_outs, router, ln_g, ln_b, dst_d, dst_b, xe_d, ye_d, flag_d,
                     dbg_sl=None):
    nc, S_ = cx.nc, cx.S
    F = DFFE
    NTT = NB * S // 128
    xe_b, ye_b, flag_b = Buf(), Buf(), Buf()
    with ExitStack() as st:
        G12 = st.enter_context(nc.sbuf_tensor(_u("ms_G12"), [128, NTT, 2], F32))
        SL = st.enter_context(nc.sbuf_tensor(_u("ms_SL"), [128, NTT, 2], I32))
        SLt = [[st.enter_context(nc.sbuf_tensor(_u("ms_SLt"), [128, 1], I32)) for _ in range(2)] for _ in range(NTT)]
        G12b, SLb = Buf(), Buf()
        with ExitStack() as s1:
            wr = s1.enter_context(nc.sbuf_tensor(_u("ms_wr"), [128, 8, NE], F32))
            ident32 = s1.enter_context(nc.sbuf_tensor(_u("ms_id32"), [128, 128], F32))
            Ltri = s1.enter_context(nc.sbuf_tensor(_u("ms_Ltri"), [128, 128], BF16))
            eC = s1.enter_context(nc.sbuf_tensor(_u("ms_eC"), [128, NE], F32))
            cum = s1.enter_context(nc.sbuf_tensor(_u("ms_cum"), [128, NE], F32))
            ovfacc = s1.enter_context(nc.sbuf_tensor(_u("ms_ovf"), [128, 1], F32))
            ovfb16 = s1.enter_context(nc.sbuf_tensor(_u("ms_ovfb"), [128, 1], BF16))
            flag_sb = s1.enter_context(nc.sbuf_tensor(_u("ms_flag"), [1, 1], I32))
            wrb, idb32, cb, cumb, ovfb = Buf(), Buf(), Buf(), Buf(), Buf()
            S_.dma("sp", wr[:], router.rearrange("(k p) e -> p k e", p=128), writes=[wrb])
            S_.op("pool", lambda: nc.gpsimd.tensor_copy(out=ident32[:], in_=c["ident"][:]), reads=[c["ident_b"]],
                  writes=[idb32])
            S_.op("pool", lambda: nc.gpsimd.affine_select(out=Ltri[:], in_=c["ones"][:], pattern=[[1, 128]],
                                                          compare_op=ALU.is_ge, fill=cx.reg(0.0), base=-1,
                                                          channel_multiplier=-1), reads=[c["ones_b"]], writes=[cb])
            S_.op("pool", lambda: nc.gpsimd.iota(eC[:], pattern=[[CAP, NE]], base=0, channel_multiplier=0,
                                                 allow_small_or_imprecise_dtypes=True), writes=[cb])
            S_.op("pool", lambda: nc.gpsimd.memset(cum[:], 0.0), writes=[cumb])
            S_.op("pool", lambda: nc.gpsimd.memset(ovfacc[:], 0.0), writes=[ovfb])
            xtok = Ring(nc, s1, "ms_xtok", [128, D], F32, 3)
            xbr = Ring(nc, s1, "ms_xb", [128, D], BF16, 3)
            xT32r = Ring(nc, s1, "ms_xT32", [128, 8, 128], F32, 2)
            lgr = Ring(nc, s1, "ms_lg", [128, NE], F32, 2)
            topr = Ring(nc, s1, "ms_top", [128, 8], F32, 2)
            mkr = Ring(nc, s1, "ms_mk", [128, NE], F32, 2)
            mkbr = Ring(nc, s1, "ms_mkb", [128, NE], BF16, 2)
            cumbr = Ring(nc, s1, "ms_cumb", [128, NE], BF16, 2)
            p2r = Ring(nc, s1, "ms_p2", [128, NE], F32, 2)
            ger = Ring(nc, s1, "ms_ge", [128, NE], F32, 2)
            tmr = Ring(nc, s1, "ms_tm", [128, NE], F32, 4)
            slfr = Ring(nc, s1, "ms_slf", [128, 2], F32, 2)
            dr = Ring(nc, s1, "ms_d", [128, 1], F32, 2)
            pre = {}

            def xload(j):
                b, t0 = (j * 128) // S, (j * 128) % S
                xt, xtb = xtok.next()
                S_.dma("sp", xt[:], x_d[b, t0:t0 + 128, :], reads=[x_b], writes=[xtb])
                pre[j] = (xt, xtb)

            xload(0)
            for j in range(NTT):
                if j + 1 < NTT:
                    xload(j + 1)
                xt, xtb = pre.pop(j)
                xb, xbb = xbr.next()
                S_.op("act", lambda xb=xb, xt=xt: nc.scalar.copy(out=xb[:], in_=xt[:]), reads=[xtb], writes=[xbb])
                xT32, xT32b = xT32r.next()
                for k0 in (0, 4):
                    bk, bb = cx.bank()
                    for k in range(k0, k0 + 4):
                        S_.op("pe", lambda k=k, k0=k0, bk=bk, xt=xt: nc.tensor.transpose(
                            out=bk[:, (k - k0) * 128:(k - k0 + 1) * 128], in_=xt[:, k * 128:(k + 1) * 128],
                            identity=ident32[:]), reads=[xtb, idb32], writes=[bb], inc=(k == k0 + 3))
                    S_.op("act", lambda k0=k0, bk=bk, xT32=xT32: nc.scalar.copy(
                        out=xT32[:, k0:k0 + 4, :], in_=bk[:].rearrange("p (a b) -> p a b", a=4)),
                        reads=[bb], writes=[xT32b])
                bk, bb = cx.bank()
                for kc in range(8):
                    S_.op("pe", lambda kc=kc, bk=bk, xT32=xT32: nc.tensor.matmul(
                        bk[:, 0:NE], lhsT=xT32[:, kc, :], rhs=wr[:, kc, :], start=(kc == 0), stop=(kc == 7)),
                        reads=[xT32b, wrb], writes=[bb], inc=(kc == 7))
                lg_, lgb = lgr.next()
                top, topb = topr.next()
                S_.op("dve", lambda bk=bk, lg_=lg_: nc.vector.tensor_copy(out=lg_[:], in_=bk[:, 0:NE]), reads=[bb],
                      writes=[lgb])
                S_.op("dve", lambda lg_=lg_, top=top: nc.vector.max(out=top[:], in_=lg_[:]), reads=[lgb],
                      writes=[topb])
                d_, db_ = dr.next()
                S_.op("dve", lambda top=top, d_=d_: nc.vector.tensor_tensor(out=d_[:], in0=top[:, 0:1],
                                                                           in1=top[:, 1:2], op=ALU.subtract),
                      reads=[topb], writes=[db_])
                S_.op("act", lambda d_=d_, j=j: nc.scalar.activation(out=G12[:, j, 0:1], in_=d_[:], func=AF.Sigmoid),
                      reads=[db_], writes=[G12b])
                S_.op("act", lambda d_=d_, j=j: nc.scalar.activation(out=G12[:, j, 1:2], in_=d_[:], func=AF.Sigmoid,
                                                                    scale=-1.0), reads=[db_], writes=[G12b])
                mk, mkb = mkr.next()
                S_.op("dve", lambda mk=mk, lg_=lg_, top=top: nc.vector.tensor_scalar(
                    out=mk[:], in0=lg_[:], scalar1=top[:, 1:2], scalar2=None, op0=ALU.is_ge), reads=[lgb, topb],
                    writes=[mkb])
                mkb16, mkb16b = mkbr.next()
                S_.op("dve", lambda mk=mk, mkb16=mkb16: nc.vector.tensor_copy(out=mkb16[:], in_=mk[:]), reads=[mkb],
                      writes=[mkb16b])
                cumb16, cumb16b = cumbr.next()
                S_.op("dve", lambda cumb16=cumb16: nc.vector.tensor_copy(out=cumb16[:], in_=cum[:]), reads=[cumb],
                      writes=[cumb16b])
                bp, bpb = cx.bank()
                S_.op("pe", lambda bp=bp, mkb16=mkb16: nc.tensor.matmul(bp[:, 0:NE], lhsT=Ltri[:], rhs=mkb16[:],
                                                                         start=True, stop=False),
                      reads=[cb, mkb16b], writes=[bpb], inc=False)
                S_.op("pe", lambda bp=bp, cumb16=cumb16: nc.tensor.matmul(bp[:, 0:NE], lhsT=c["ones"][:],
                                                                           rhs=cumb16[:], start=False, stop=True),
                      reads=[c["ones_b"], cumb16b], writes=[bpb])
                S_.op("dve", lambda mk=mk: nc.vector.tensor_tensor(out=cum[:], in0=cum[:], in1=mk[:], op=ALU.add),
                      reads=[mkb, cumb, cumb16b], writes=[cumb])
                ge, geb = ger.next()
                p2, p2b = p2r.next()
                S_.op("dve", lambda ge=ge, bp=bp: nc.vector.tensor_scalar(out=ge[:], in0=bp[:, 0:NE],
                                                                          scalar1=float(CAP), scalar2=None,
                                                                          op0=ALU.is_ge), reads=[bpb], writes=[geb])
                S_.op("dve", lambda p2=p2, bp=bp: nc.vector.tensor_tensor(out=p2[:], in0=bp[:, 0:NE], in1=eC[:],
                                                                          op=ALU.add), reads=[bpb, cb], writes=[p2b])
                S_.op("dve", lambda p2=p2, ge=ge: nc.vector.scalar_tensor_tensor(
                    out=p2[:], in0=ge[:], scalar=1.0e6, in1=p2[:], op0=ALU.mult, op1=ALU.add), reads=[geb, p2b],
                    writes=[p2b])
                tm, tmb = tmr.next()
                S_.op("dve", lambda tm=tm, ge=ge, mk=mk: nc.vector.tensor_tensor(out=tm[:], in0=ge[:], in1=mk[:],
                                                                                 op=ALU.mult), reads=[geb, mkb],
                      writes=[tmb])
                tm2, tm2b = tmr.next()
                S_.op("dve", lambda tm=tm, tm2=tm2: nc.vector.tensor_reduce(out=tm2[:, 0:1], in_=tm[:],
                                                                           axis=mybir.AxisListType.X, op=ALU.add),
                      reads=[tmb], writes=[tm2b])
                S_.op("dve", lambda tm2=tm2: nc.vector.tensor_tensor(out=ovfacc[:], in0=ovfacc[:], in1=tm2[:, 0:1],
                                                                     op=ALU.add), reads=[tm2b, ovfb], writes=[ovfb])
                slf, slfb = slfr.next()
                for ci in range(2):
                    tm, tmb = tmr.next()
                    S_.op("dve", lambda tm=tm, lg_=lg_, top=top, p2=p2, ci=ci: nc.vector.scalar_tensor_tensor(
                        out=tm[:], in0=lg_[:], scalar=top[:, ci:ci + 1], in1=p2[:], op0=ALU.is_equal, op1=ALU.mult),
                        reads=[lgb, topb, p2b], writes=[tmb])
                    S_.op("dve", lambda tm=tm, slf=slf, ci=ci: nc.vector.tensor_reduce(
                        out=slf[:, ci:ci + 1], in_=tm[:], axis=mybir.AxisListType.X, op=ALU.add), reads=[tmb],
                        writes=[slfb])
                S_.op("dve", lambda slf=slf, j=j: nc.vector.tensor_copy(out=SL[:, j, :], in_=slf[:]), reads=[slfb],
                      writes=[SLb])
                for ci in range(2):
                    S_.op("dve", lambda slf=slf, j=j, ci=ci: nc.vector.tensor_copy(out=SLt[j][ci][:],
                                                                                  in_=slf[:, ci:ci + 1]),
                          reads=[slfb], writes=[SLb])
                for ci in range(2):
                    _dma_ind(S_, nc, cx.reg(NSLOT - 1), xe_d[:, :], bass.IndirectOffsetOnAxis(ap=SLt[j][ci][:, :], axis=0), xb[:, :],
                             None, reads=[xbb, SLb], writes=[xe_b])
            S_.op("dve", lambda: nc.vector.tensor_copy(out=ovfb16[:], in_=ovfacc[:]), reads=[ovfb], writes=[ovfb])
            bk, bb = cx.bank()
            S_.op("pe", lambda: nc.tensor.matmul(bk[0:1, 0:1], lhsT=ovfb16[:, 0:1], rhs=c["ones"][:, 0:1], start=True,
                                                 stop=True), reads=[ovfb, c["ones_b"]], writes=[bb])
            S_.op("dve", lambda: nc.vector.tensor_copy(out=flag_sb[:], in_=bk[0:1, 0:1]), reads=[bb], writes=[ovfb])
            S_.dma("sp", flag_d[:, :], flag_sb[:], reads=[ovfb], writes=[flag_b])
            if dbg_sl is not None:
                S_.dma("sp", dbg_sl[:, :, :], SL[:], reads=[SLb], writes=[flag_b], add=True)
            S_.fence()
        with ExitStack() as s2:
            T = CAP
            ntt = T // 128
            tblocks = []
            t0 = 0
            while t0 < T:
                tn = min(512, T - t0)
                tblocks.append((t0, tn))
                t0 += tn
            groups = [(f0, 512) for f0 in range(0, F, 512)]
            acc = s2.enter_context(nc.sbuf_tensor(_u("me_acc"), [128, ntt, D], F32))
            accb = [Buf() for _ in range(ntt)]
            xTs = [s2.enter_context(nc.sbuf_tensor(_u("me_xT"), [128, 8, T], BF16)) for _ in range(2)]
            xTbs = [[Buf() for _ in range(ntt)] for _ in range(2)]
            xer = Ring(nc, s2, "me_xe", [128, D], BF16, 3)
            Wir = Ring(nc, s2, "me_Wi", [128, 8, 2, 512], BF16, 2)
            Wor = Ring(nc, s2, "me_Wo", [128, 4, D], BF16, 2)
            hTr = Ring(nc, s2, "me_hT", [128, 4, T], BF16, 2)
            sar = Ring(nc, s2, "me_sa", [128, 512], F32, 3)
            work = [(e, gi) for e in range(NE) for gi in range(len(groups))]
            wpre = {}

            def wload(k):
                e, gi = work[k]
                f0, g = groups[gi]
                Wi, Wib = Wir.next()
                Wo, Wob = Wor.next()
                _load_w3(cx, Wi[:, :, 0, 0:g], w_ins[e], 0, 8, f0, f0 + g, Wib)
                _load_w3(cx, Wi[:, :, 1, 0:g], w_ins[e], 0, 8, F + f0, F + f0 + g, Wib, add=True)
                _load_w3(cx, Wo[:, 0:g // 128, :], w_outs[e], f0, g // 128, 0, D, Wob)
                wpre[k] = (Wi, Wib, Wo, Wob)

            def load_xT(e):
                xT, xTb = xTs[e % 2], xTbs[e % 2]
                for tt in range(ntt):
                    xe, xeb = xer.next()
                    S_.dma("sp", xe[:], xe_d[e * CAP + tt * 128:e * CAP + (tt + 1) * 128, :], reads=[xe_b],
                           writes=[xeb])
                    _transpose_to(cx, c, xe, xeb, 8,
                                  lambda k0, k1, tt=tt, xT=xT: xT[:, k0:k1, tt * 128:(tt + 1) * 128], xTb[tt])

            wk = 0
            wload(0)
            load_xT(0)
            for e in range(NE):
                xT, xTb = xTs[e % 2], xTbs[e % 2]
                for gi, (f0, g) in enumerate(groups):
                    nfc = g // 128
                    if gi == 1 and e + 1 < NE:
                        load_xT(e + 1)
                    if wk + 1 < len(work):
                        wload(wk + 1)
                    Wi, Wib, Wo, Wob = wpre.pop(wk)
                    wk += 1
                    hT, hTb = hTr.next()
                    for (tb0, tn) in tblocks:
                        tsl = slice(tb0, tb0 + tn)
                        xdeps = xTb[tb0 // 128:(tb0 + tn) // 128]
                        for fc in range(nfc):
                            pa, pab = cx.bank()
                            pbk, pbb = cx.bank()
                            for ab, (pp, ppb) in enumerate(((pa, pab), (pbk, pbb))):
                                for kc in range(8):
                                    S_.op("pe", lambda kc=kc, ab=ab, pp=pp, fc=fc, Wi=Wi: nc.tensor.matmul(
                                        pp[:, 0:tn], lhsT=Wi[:, kc, ab, fc * 128:(fc + 1) * 128], rhs=xT[:, kc, tsl],
                                        start=(kc == 0), stop=(kc == 7)), reads=[Wib] + xdeps, writes=[ppb],
                                        inc=(kc == 7))
                            sa, sab = sar.next()
                            S_.op("act", lambda sa=sa, pa=pa: nc.scalar.activation(out=sa[:, 0:tn], in_=pa[:, 0:tn],
                                                                                   func=AF.Silu),
                                  reads=[pab], writes=[sab])
                            S_.op("dve", lambda sa=sa, pbk=pbk, hT=hT, fc=fc: nc.vector.tensor_tensor(
                                out=hT[:, fc, tsl], in0=sa[:, 0:tn], in1=pbk[:, 0:tn], op=ALU.mult), reads=[sab, pbb],
                                writes=[hTb])
                    for tt in range(ntt):
                        for hh in range(2):
                            po, pob = cx.bank()
                            for fc in range(nfc):
                                S_.op("pe", lambda fc=fc, po=po, hT=hT, Wo=Wo, tt=tt, hh=hh: nc.tensor.matmul(
                                    po[:], lhsT=hT[:, fc, tt * 128:(tt + 1) * 128],
                                    rhs=Wo[:, fc, hh * 512:(hh + 1) * 512], start=(fc == 0), stop=(fc == nfc - 1)),
                                    reads=[hTb, Wob], writes=[pob], inc=(fc == nfc - 1))
                            asl = acc[:, tt, hh * 512:(hh + 1) * 512]
                            if gi == 0:
                                S_.op("act", lambda po=po, asl=asl: nc.scalar.copy(out=asl, in_=po[:]),
                                      reads=[pob], writes=[accb[tt]])
                            else:
                                S_.op("dve", lambda po=po, asl=asl: nc.vector.tensor_tensor(
                                    out=asl, in0=po[:], in1=asl, op=ALU.add), reads=[pob, accb[tt]],
                                    writes=[accb[tt]])
                for tt in range(ntt):
                    S_.dma("sp", ye_d[e * CAP + tt * 128:e * CAP + (tt + 1) * 128, :], acc[:, tt, :],
                           reads=[accb[tt]], writes=[ye_b], add=True)
            S_.fence()
        with ExitStack() as s3:
            lng = s3.enter_context(nc.sbuf_tensor(_u("mc_lng"), [128, D], F32))
            lnb = s3.enter_context(nc.sbuf_tensor(_u("mc_lnb"), [128, D], F32))
            pb_ = Buf()
            S_.dma("sp", lng[:], ln_g.partition_broadcast(128), writes=[pb_])
            S_.dma("sp", lnb[:], ln_b.partition_broadcast(128), writes=[pb_], add=True)
            xtok = Ring(nc, s3, "mc_xtok", [128, D], F32, 3)
            y1r = Ring(nc, s3, "mc_y1", [128, D], F32, 3)
            y2r = Ring(nc, s3, "mc_y2", [128, D], F32, 3)
            st6r = Ring(nc, s3, "mc_st6", [128, 2, 6], F32, 2)
            mvr = Ring(nc, s3, "mc_mv", [128, 2], F32, 2)
            rsr = Ring(nc, s3, "mc_rs", [128, 1], F32, 2)
            pre = {}

            def cload(j):
                b, t0 = (j * 128) // S, (j * 128) % S
                xt, xtb = xtok.next()
                S_.dma("sp", xt[:], x_d[b, t0:t0 + 128, :], reads=[x_b], writes=[xtb])
                y1, y1b = y1r.next()
                y2, y2b = y2r.next()
                _dma_ind(S_, nc, cx.reg(NSLOT - 1), y1[:, :], None, ye_d[:, :], bass.IndirectOffsetOnAxis(ap=SLt[j][0][:, :], axis=0),
                         reads=[ye_b, SLb], writes=[y1b], add=False)
                _dma_ind(S_, nc, cx.reg(NSLOT - 1), y2[:, :], None, ye_d[:, :], bass.IndirectOffsetOnAxis(ap=SLt[j][1][:, :], axis=0),
                         reads=[ye_b, SLb], writes=[y2b], add=False)
                pre[j] = (xt, xtb, y1, y1b, y2, y2b)

            cload(0)
            for j in range(NTT):
                if j + 1 < NTT:
                    cload(j + 1)
                xt, xtb, y1, y1b, y2, y2b = pre.pop(j)
                b, t0 = (j * 128) // S, (j * 128) % S
                S_.op("act", lambda xt=xt: nc.scalar.mul(out=xt[:], in_=xt[:], mul=DN_ALPHA), reads=[xtb],
                      writes=[xtb])
                S_.op("dve", lambda xt=xt, y1=y1, j=j: nc.vector.scalar_tensor_tensor(
                    out=xt[:], in0=y1[:], scalar=G12[:, j, 0:1], in1=xt[:], op0=ALU.mult, op1=ALU.add),
                    reads=[y1b, G12b, xtb], writes=[xtb])
                S_.op("dve", lambda xt=xt, y2=y2, j=j: nc.vector.scalar_tensor_tensor(
                    out=xt[:], in0=y2[:], scalar=G12[:, j, 1:2], in1=xt[:], op0=ALU.mult, op1=ALU.add),
                    reads=[y2b, G12b, xtb], writes=[xtb])
                st6, sb = st6r.next()
                mv, _ = mvr.next()
                rs, _ = rsr.next()
                _layernorm_store(cx, c, xt, xtb, lng, lnb, pb_, dst_d[b, t0:t0 + 128, :], dst_b, st6, mv, rs, sb)
    return flag_b


LAST_INPUTS = []


def build_program(phases=("ret1", "ret2", "ffn", "moe"), dbg=False):
    nc = bass.Bass("TRN2", target_bir_lowering=False)
    dt = {}
    del LAST_INPUTS[:]

    def inp(name, shape):
        dt[name] = nc.dram_tensor(name, list(shape), F32, kind="ExternalInput").ap()
        LAST_INPUTS.append(name)
        return dt[name]

    x = inp("x", [NB, S, D])
    ret_w_in = inp("ret_w_in", [1, D, 6144])
    ret_gn_g = inp("ret_gn_g", [1, 2048])
    ret_w_out = inp("ret_w_out", [1, 2048, D])
    nsa_w_kv = inp("nsa_w_kv", [D, 1536])
    cmp_k_pe = inp("cmp_k_pe", [32, 64])
    cmp_k_w1 = inp("cmp_k_w1", [2048, 256])
    cmp_k_w2 = inp("cmp_k_w2", [256, 64])
    cmp_v_pe = inp("cmp_v_pe", [32, 64])
    cmp_v_w1 = inp("cmp_v_w1", [2048, 256])
    cmp_v_w2 = inp("cmp_v_w2", [256, 64])
    nsa_w_q = inp("nsa_w_q", [1, D, 1072])
    nsa_w_out = inp("nsa_w_out", [1, D, D])
    ffn_w_in = inp("ffn_w_in", [1, D, 2 * DFF])
    ffn_w_out = inp("ffn_w_out", [1, DFF, D])
    moe_router = inp("moe_router", [1, D, NE])
    moe_w_in = inp("moe_w_in", [1, NE, D, 2 * DFFE])
    moe_w_out = inp("moe_w_out", [1, NE, DFFE, D])
    ln_g = inp("ln_g", [2, 2, D])
    ln_b = inp("ln_b", [2, 2, D])
    okind = "ExternalOutput" if dbg else "Internal"

    def scratch(name, shape, dtype=F32, src_phase=None):
        if dbg and src_phase is not None and src_phase not in phases:
            LAST_INPUTS.append(name)
            return nc.dram_tensor(name, list(shape), dtype, kind="ExternalInput").ap()
        return nc.dram_tensor(name, list(shape), dtype, kind=okind).ap()

    cos0 = scratch("cos0", [128, S])
    sin0 = scratch("sin0", [128, S])
    yn_d = scratch("yn_d", [NB, S, 2048], BF16)
    cos1 = scratch("cos1", [128, S])
    sin1 = scratch("sin1", [128, S])
    wsw_d = nc.dram_tensor("wsw_d", [20, 128, 8, 256], BF16, kind="Internal").ap()
    dbg_kc = dbg_vc = None
    if dbg:
        dbg_kc = nc.dram_tensor("dbg_kc", [NB, 128, NG, 128], BF16, kind="ExternalOutput").ap()
        dbg_vc = nc.dram_tensor("dbg_vc", [NB, 128, NG, 97], BF16, kind="ExternalOutput").ap()
    x1_d = scratch("x1", [NB, S, D], src_phase="ret2")
    x2_d = scratch("x2", [NB, S, D], src_phase="ffn")
    x3_d = scratch("x3", [NB, S, D], src_phase="nsa")
    out_d = nc.dram_tensor("out", [NB, S, D], F32, kind="ExternalOutput").ap()
    with ExitStack() as st:
        cx = Ctx(nc, st)
        c = _consts(cx, st, None)
        xb_, tab0_b, yn_b, x1_b, x2_b, x3_b, out_b = [Buf() for _ in range(7)]
        nsa_wswb = None
        tab1_b = Buf()
        tab1_done = False
        if "ret1" in phases:
            _rope_tables(cx, c, cos0, sin0, 128, 128, tab0_b)
            if "nsa" in phases:
                _rope_tables(cx, c, cos1, sin1, 32, 32, tab1_b, sign_rows=True)
                tab1_done = True
            cx.S.fence()
            phase_ret1(cx, c, x, xb_, ret_w_in[0], cos0, sin0, tab0_b, yn_d, yn_b)
            cx.S.fence()
        if "ret2" in phases:
            side = None
            if "nsa" in phases:
                pcs = _nsa_pieces(nsa_w_q[0], nsa_w_kv)
                prep = {"rings": None, "next": 0}
                wswb_pre = Buf()

                def side(k, st_):
                    if prep["rings"] is None:
                        prep["rings"] = _nsa_prep_rings(nc, st_)
                    if k % 3 == 2 and prep["next"] < len(pcs):
                        _nsa_prep_piece(cx, pcs, prep["next"], prep["rings"], wsw_d, wswb_pre)
                        prep["next"] += 1
            phase_ret2(cx, c, x, xb_, ret_w_in[0], ret_gn_g, ret_w_out[0], ln_g[0, 0:1, :], ln_b[0, 0:1, :],
                       yn_d, yn_b, x1_d, x1_b, side=side)
            if side is not None:
                assert prep["next"] == len(pcs)
                nsa_wswb = wswb_pre
            cx.S.fence()
        if "ffn" in phases:
            phase_ffn(cx, c, x1_d, x1_b, [ffn_w_in[0]], [ffn_w_out[0]], DFF, None, ln_g[0, 1:2, :], ln_b[0, 1:2, :],
                      x2_d, x2_b)
            cx.S.fence()
        if "nsa" in phases:
            dbgb = Buf()
            if not tab1_done:
                _rope_tables(cx, c, cos1, sin1, 32, 32, tab1_b, sign_rows=True)
                cx.S.fence()
            phase_nsa(cx, c, x2_d, x2_b, nsa_w_kv, cmp_k_pe, cmp_k_w1, cmp_k_w2, cmp_v_pe, cmp_v_w1, cmp_v_w2,
                      nsa_w_q[0], nsa_w_out[0], ln_g[1, 0:1, :], ln_b[1, 0:1, :], cos1, sin1, tab1_b, x3_d, x3_b,
                      wsw_d, dbg_kc, dbg_vc, dbgb, wswb=nsa_wswb)
            cx.S.fence()
        if "moe" in phases and os.environ.get("DBG_DENSE_MOE"):
            phase_ffn(cx, c, x3_d, x3_b, [moe_w_in[0, e] for e in range(NE)], [moe_w_out[0, e] for e in range(NE)],
                      DFFE, moe_router[0], ln_g[1, 1:2, :], ln_b[1, 1:2, :], out_d, out_b)
            cx.S.fence()
        elif "moe" in phases:
            xe_d = nc.dram_tensor("xe_d", [NSLOT, D], BF16, kind="Internal").ap()
            ye_d = nc.dram_tensor("ye_d", [NSLOT, D], F32, kind="Internal").ap()
            flag_d = nc.dram_tensor("flag_d", [1, 1], I32, kind=okind).ap()
            dbg_sl = nc.dram_tensor("dbg_sl", [128, NB * S // 128, 2], I32, kind="ExternalOutput").ap() if dbg else None
            flag_b = phase_moe_sparse(cx, c, x3_d, x3_b, [moe_w_in[0, e] for e in range(NE)],
                                      [moe_w_out[0, e] for e in range(NE)], moe_router[0], ln_g[1, 1:2, :],
                                      ln_b[1, 1:2, :], out_d, out_b, xe_d, ye_d, flag_d, dbg_sl)
            cx.S.fence()
            Sd = cx.S
            engs = OrderedEngineSet([mybir.EngineType.PE, mybir.EngineType.DVE, mybir.EngineType.Activation,
                                     mybir.EngineType.Pool, mybir.EngineType.SP])
            regs = nc.alloc_registers("ovf_flag", engs)
            for reg in regs:
                nc.reg_load(reg, flag_d[0:1, 0:1])
            cnt0 = dict(Sd.cnt)
            dcnt0 = list(Sd.dcnt)
            with nc.If(nc.snap(regs) > 0):
                phase_ffn(cx, c, x3_d, x3_b, [moe_w_in[0, e] for e in range(NE)],
                          [moe_w_out[0, e] for e in range(NE)], DFFE, moe_router[0], ln_g[1, 1:2, :],
                          ln_b[1, 1:2, :], out_d, out_b)
                Sd.fence()
            with nc.Else():
                for k in ("pe", "dve", "act", "pool"):
                    dlt = Sd.cnt[k] - cnt0[k]
                    if dlt > 0:
                        Sd.eng[k].sem_inc(Sd.sem[k], dlt)
                for i in range(Sd.NDMA):
                    dlt = Sd.dcnt[i] - dcnt0[i]
                    if dlt > 0:
                        nc.sync.sem_inc(Sd.dsem[i], dlt)
            cx.S.fence()
        cx.S.finish([x1_b, x2_b, x3_b, out_b, yn_b, tab0_b], "sp")
        print("instructions:", cx.S.ninstr)
    return nc


_PROG = {}


def kernel(**inputs):
    if "nc" not in _PROG:
        _PROG["nc"] = build_program(phases=("ret1", "ret2", "ffn", "nsa", "moe"), dbg=False)
        _PROG["names"] = list(LAST_INPUTS)
    nc = _PROG["nc"]
    names = _PROG["names"]
    x = np.ascontiguousarray(np.asarray(inputs["x"], dtype=np.float32))
    shared = {k: np.ascontiguousarray(np.asarray(inputs[k], dtype=np.float32)) for k in names if k != "x"}
    in_maps = []
    for ci in range(NCORES):
        m = dict(shared)
        m["x"] = np.ascontiguousarray(x[ci * NB:(ci + 1) * NB])
        in_maps.append(m)
    res = run_bass_kernel_spmd(nc, in_maps, core_ids=list(range(NCORES)))
    out = np.concatenate([np.asarray(r["out"], dtype=np.float32) for r in res.results], axis=0)
    return out
```
